# Optimizing a Trainium2 kernel written in Bass

```python
import math
import jax
import jax.numpy as jnp
from jax import lax
import numpy as np

D_MODEL = 1024
BATCH = 2
SEQ = 16384
DEPTH = 1

D_MIX = D_MODEL
HEAD_DIM = 64
D_ATTN = D_MIX // 2
D_CONV = D_MIX - D_ATTN
N_HEADS = D_ATTN // HEAD_DIM
N_KV = 2
HPG = N_HEADS // N_KV
KV_DIM = N_KV * HEAD_DIM
N_MIX_GROUPS = D_MIX // HEAD_DIM
CONV_WIDTH = 3
CMP_LEN = 32
CMP_STRIDE = 16
CMP_HIDDEN = 256
SLC_LEN = 64
N_SELECT = 16
WINDOW = 512
Q_BLOCK = 128
N_BUCKETS = 32
MAX_DISTANCE = 128
N_GROUPS = 8
EXPERTS_PER_GROUP = 8
N_EXPERTS = N_GROUPS * EXPERTS_PER_GROUP
TOP_K = 2
D_EXPERT = D_MODEL // 2
MOE_BLOCK = 128
FORCED_SCORE = 1e4
EPS = 1e-6
N_IN = D_ATTN + 6 * KV_DIM + 3 * N_HEADS + 3 * D_CONV

kernel_name = 'hybrid_nsa_shortconv_hmoe'


def rms_norm(x, w):
    xf = x.astype(jnp.float32)
    y = xf * lax.rsqrt(jnp.mean(xf * xf, axis=-1, keepdims=True) + EPS)
    return (y * w.astype(jnp.float32)).astype(x.dtype)


def t5_bucket(dist):
    n = jnp.maximum(dist, 0)
    max_exact = N_BUCKETS // 2
    nf = jnp.maximum(n, 1).astype(jnp.float32)
    large = max_exact + (jnp.log(nf / max_exact) / math.log(MAX_DISTANCE / max_exact)
                         * (N_BUCKETS - max_exact)).astype(jnp.int32)
    large = jnp.minimum(large, N_BUCKETS - 1)
    return jnp.where(n < max_exact, n, large)


def masked_softmax(logits, mask):
    lf = jnp.where(mask, logits.astype(jnp.float32), -jnp.inf)
    m = jnp.max(lf, axis=-1, keepdims=True)
    m = jnp.where(jnp.isfinite(m), m, 0.0)
    e = jnp.where(mask, jnp.exp(lf - m), 0.0)
    return e / jnp.maximum(jnp.sum(e, axis=-1, keepdims=True), 1e-30)


def compress(kv, pos_emb, w1, w2):
    b, s, g, dh = kv.shape
    nc = (s - CMP_LEN) // CMP_STRIDE + 1
    idx = jnp.arange(nc)[:, None] * CMP_STRIDE + jnp.arange(CMP_LEN)[None, :]
    blocks = kv[:, idx] + pos_emb[None, None, :, None, :]
    blocks = jnp.moveaxis(blocks, 3, 2).reshape(b, nc, g, CMP_LEN * dh)
    return jax.nn.silu(blocks @ w1) @ w2


def nsa_attention(q, gates, k_cmp, v_cmp, k_slc, v_slc, k_win, v_win, rel_bias):
    b, s = q.shape[0], q.shape[1]
    nc = k_cmp.shape[1]
    ns = s // SLC_LEN
    n_sel = min(N_SELECT, ns)
    nqb = s // Q_BLOCK
    scale = HEAD_DIM ** -0.5
    bias_gh = rel_bias.T.reshape(N_KV, HPG, N_BUCKETS)

    c_start = jnp.arange(nc) * CMP_STRIDE
    c_end = c_start + CMP_LEN - 1
    s_start = jnp.arange(ns) * SLC_LEN
    overlap = ((c_start[:, None] <= s_start[None, :] + SLC_LEN - 1)
               & (c_end[:, None] >= s_start[None, :])).astype(jnp.float32)

    ks_blocks = k_slc.reshape(b, ns, SLC_LEN, N_KV, HEAD_DIM).transpose(0, 3, 1, 2, 4)
    vs_blocks = v_slc.reshape(b, ns, SLC_LEN, N_KV, HEAD_DIM).transpose(0, 3, 1, 2, 4)
    pad = ((0, 0), (WINDOW, 0), (0, 0), (0, 0))
    kw_pad = jnp.pad(k_win, pad)
    vw_pad = jnp.pad(v_win, pad)

    q_all = q.reshape(b, nqb, Q_BLOCK, N_KV, HPG, HEAD_DIM).swapaxes(0, 1)
    g_all = gates.reshape(b, nqb, Q_BLOCK, N_KV, HPG, 3).swapaxes(0, 1)
    b_ix = jnp.arange(b)[:, None, None, None]
    g_ix = jnp.arange(N_KV)[None, :, None, None]
    g_ix5 = jnp.arange(N_KV)[None, :, None, None, None]
    h_ix5 = jnp.arange(HPG)[None, None, :, None, None]
    blk = jnp.arange(ns)

    def block_fn(args):
        j, qb, gb = args
        s0 = j * Q_BLOCK
        pos = s0 + jnp.arange(Q_BLOCK)

        dist_c = pos[:, None] - c_end[None, :]
        lc = jnp.einsum('bqghd,bngd->bghqn', qb, k_cmp) * scale + bias_gh[:, :, t5_bucket(dist_c)]
        p_cmp = masked_softmax(lc, dist_c >= 0)
        o_cmp = jnp.einsum('bghqn,bngd->bqghd', p_cmp.astype(v_cmp.dtype), v_cmp)

        imp = jnp.einsum('bghqn,ns->bgqs', p_cmp, overlap)
        cur = pos // SLC_LEN
        valid = blk[None, :] <= cur[:, None]
        forced = (blk[None, :] == 0) | (blk[None, :] == cur[:, None]) | (blk[None, :] == cur[:, None] - 1)
        score = jnp.where(forced, FORCED_SCORE, jnp.where(valid, imp, -1.0))
        top_val, top_idx = lax.top_k(score, n_sel)
        sel_ok = top_val >= 0.0
        ks = ks_blocks[b_ix, g_ix, top_idx].reshape(b, N_KV, Q_BLOCK, n_sel * SLC_LEN, HEAD_DIM)
        vs = vs_blocks[b_ix, g_ix, top_idx].reshape(b, N_KV, Q_BLOCK, n_sel * SLC_LEN, HEAD_DIM)
        kpos = (top_idx[..., None] * SLC_LEN + jnp.arange(SLC_LEN)).reshape(b, N_KV, Q_BLOCK, -1)
        dist_s = pos[None, None, :, None] - kpos
        mask_s = jnp.repeat(sel_ok, SLC_LEN, axis=-1) & (dist_s >= 0)
        ls = (jnp.einsum('bqghd,bgqkd->bghqk', qb, ks) * scale
              + bias_gh[g_ix5, h_ix5, t5_bucket(dist_s)[:, :, None]])
        p_s = masked_softmax(ls, mask_s[:, :, None])
        o_slc = jnp.einsum('bghqk,bgqkd->bqghd', p_s.astype(vs.dtype), vs)

        kw = lax.dynamic_slice_in_dim(kw_pad, s0, WINDOW + Q_BLOCK, axis=1)
        vw = lax.dynamic_slice_in_dim(vw_pad, s0, WINDOW + Q_BLOCK, axis=1)
        kpos_w = s0 - WINDOW + jnp.arange(WINDOW + Q_BLOCK)
        dist_w = pos[:, None] - kpos_w[None, :]
        mask_w = (dist_w >= 0) & (dist_w < WINDOW) & (kpos_w[None, :] >= 0)
        lw = jnp.einsum('bqghd,bkgd->bghqk', qb, kw) * scale + bias_gh[:, :, t5_bucket(dist_w)]
        p_w = masked_softmax(lw, mask_w)
        o_win = jnp.einsum('bghqk,bkgd->bqghd', p_w.astype(vw.dtype), vw)

        out = gb[..., 0:1] * o_cmp + gb[..., 1:2] * o_slc + gb[..., 2:3] * o_win
        return out.reshape(b, Q_BLOCK, D_ATTN)

    outs = lax.map(block_fn, (jnp.arange(nqb), q_all, g_all))
    return outs.swapaxes(0, 1).reshape(b, s, D_ATTN)


def short_conv(u, w):
    return lax.conv_general_dilated(u, w[:, None, :], window_strides=(1,),
                                    padding=[(CONV_WIDTH - 1, 0)],
                                    dimension_numbers=('NWC', 'WIO', 'NWC'),
                                    feature_group_count=u.shape[-1])


def hier_moe(h, w_group, b_group, w_expert, b_expert, w1, w3, w2):
    b, s, d = h.shape
    t = b * s
    xt = h.reshape(t, d)
    g_prob = jax.nn.softmax((xt @ w_group + b_group).astype(jnp.float32), axis=-1)
    g_top_p, g_top = lax.top_k(g_prob, 1)
    e_logits = (xt @ w_expert + b_expert).astype(jnp.float32).reshape(t, N_GROUPS, EXPERTS_PER_GROUP)
    e_in = jnp.take_along_axis(e_logits, g_top[:, :, None], axis=1)[:, 0]
    e_top_p, e_top = lax.top_k(jax.nn.softmax(e_in, axis=-1), TOP_K)
    e_top_p = e_top_p / jnp.sum(e_top_p, axis=-1, keepdims=True)
    weights = (g_top_p * e_top_p).reshape(-1)
    expert_ids = (g_top * EXPERTS_PER_GROUP + e_top).reshape(-1)
    token_ids = jnp.repeat(jnp.arange(t), TOP_K)

    a = t * TOP_K
    order = jnp.argsort(expert_ids)
    e_sorted = expert_ids[order]
    tok_sorted = token_ids[order]
    w_sorted = weights[order]
    counts = jax.ops.segment_sum(jnp.ones((a,), jnp.int32), expert_ids, num_segments=N_EXPERTS)
    padded = (counts + MOE_BLOCK - 1) // MOE_BLOCK * MOE_BLOCK
    start = jnp.cumsum(counts) - counts
    pad_end = jnp.cumsum(padded)
    pad_start = pad_end - padded
    dest = pad_start[e_sorted] + (jnp.arange(a) - start[e_sorted])
    n_blocks = (a + N_EXPERTS * (MOE_BLOCK - 1)) // MOE_BLOCK + 1
    rows = n_blocks * MOE_BLOCK
    x_disp = jnp.zeros((rows, d), xt.dtype).at[dest].set(xt[tok_sorted])
    blk_expert = jnp.minimum(jnp.searchsorted(pad_end, jnp.arange(n_blocks) * MOE_BLOCK, side='right'),
                             N_EXPERTS - 1)

    def expert_block(args):
        xb, e = args
        return (jax.nn.silu(xb @ w1[e]) * (xb @ w3[e])) @ w2[e]

    y_disp = lax.map(expert_block, (x_disp.reshape(n_blocks, MOE_BLOCK, d), blk_expert)).reshape(rows, d)
    y = jax.ops.segment_sum(y_disp[dest] * w_sorted[:, None].astype(y_disp.dtype), tok_sorted,
                            num_segments=t)
    return y.reshape(b, s, d)


def setup_inputs(seed: int = 0) -> dict:
    key = jax.random.key(seed)
    ks = jax.random.split(key, 26)
    f32 = jnp.float32

    def nrm(k, shape, scale):
        return jax.random.normal(k, shape, f32) * scale

    def gain(k, shape):
        return 1.0 + 0.02 * jax.random.normal(k, shape, f32)

    L = DEPTH
    return {
        'x': nrm(ks[0], (BATCH, SEQ, D_MODEL), 1.0),
        'c': nrm(ks[1], (BATCH, D_MODEL), 1.0),
        'w_ada': nrm(ks[2], (L, D_MODEL, 6 * D_MODEL), 0.5 * D_MODEL ** -0.5),
        'b_ada': nrm(ks[3], (L, 6 * D_MODEL), 0.01),
        'norm1_w': gain(ks[4], (L, D_MODEL)),
        'w_in': nrm(ks[5], (L, D_MODEL, N_IN), D_MODEL ** -0.5),
        'q_norm_w': gain(ks[6], (L, HEAD_DIM)),
        'k_norm_w': gain(ks[7], (L, 3, HEAD_DIM)),
        'cmp_pos_k': nrm(ks[8], (L, CMP_LEN, HEAD_DIM), 0.1),
        'cmp_pos_v': nrm(ks[9], (L, CMP_LEN, HEAD_DIM), 0.1),
        'cmp_k_w1': nrm(ks[10], (L, CMP_LEN * HEAD_DIM, CMP_HIDDEN), (CMP_LEN * HEAD_DIM) ** -0.5),
        'cmp_k_w2': nrm(ks[11], (L, CMP_HIDDEN, HEAD_DIM), CMP_HIDDEN ** -0.5),
        'cmp_v_w1': nrm(ks[12], (L, CMP_LEN * HEAD_DIM, CMP_HIDDEN), (CMP_LEN * HEAD_DIM) ** -0.5),
        'cmp_v_w2': nrm(ks[13], (L, CMP_HIDDEN, HEAD_DIM), CMP_HIDDEN ** -0.5),
        'conv_w': nrm(ks[14], (L, CONV_WIDTH, D_CONV), CONV_WIDTH ** -0.5),
        'out_norm_w': gain(ks[15], (L, D_MIX)),
        'w_out': nrm(ks[16], (L, D_MIX, D_MODEL), D_MIX ** -0.5),
        'rel_bias': nrm(ks[17], (N_BUCKETS, N_HEADS), 0.5),
        'norm2_w': gain(ks[18], (L, D_MODEL)),
        'w_group': nrm(ks[19], (L, D_MODEL, N_GROUPS), D_MODEL ** -0.5),
        'b_group': nrm(ks[20], (L, N_GROUPS), 0.01),
        'w_expert': nrm(ks[21], (L, D_MODEL, N_EXPERTS), D_MODEL ** -0.5),
        'b_expert': nrm(ks[22], (L, N_EXPERTS), 0.01),
        'w1': nrm(ks[23], (L, N_EXPERTS, D_MODEL, D_EXPERT), D_MODEL ** -0.5),
        'w3': nrm(ks[24], (L, N_EXPERTS, D_MODEL, D_EXPERT), D_MODEL ** -0.5),
        'w2': nrm(ks[25], (L, N_EXPERTS, D_EXPERT, D_MODEL), D_EXPERT ** -0.5),
    }


def reference(x, c, w_ada, b_ada, norm1_w, w_in, q_norm_w, k_norm_w, cmp_pos_k, cmp_pos_v,
              cmp_k_w1, cmp_k_w2, cmp_v_w1, cmp_v_w2, conv_w, out_norm_w, w_out, rel_bias,
              norm2_w, w_group, b_group, w_expert, b_expert, w1, w3, w2):
    b, s, _ = x.shape
    cond = jax.nn.silu(c)
    splits = [D_ATTN, D_ATTN + 6 * KV_DIM, D_ATTN + 6 * KV_DIM + 3 * N_HEADS,
              D_ATTN + 6 * KV_DIM + 3 * N_HEADS + D_CONV,
              D_ATTN + 6 * KV_DIM + 3 * N_HEADS + 2 * D_CONV]
    for l in range(DEPTH):
        mod = cond @ w_ada[l] + b_ada[l]
        sh1, sc1, g1, sh2, sc2, g2 = jnp.split(mod[:, None, :], 6, axis=-1)

        h = rms_norm(x, norm1_w[l]) * (1 + sc1) + sh1
        proj = h @ w_in[l]
        q, kvs, gate_logits, b_gate, c_gate, u = jnp.split(proj, splits, axis=-1)
        q = rms_norm(q.reshape(b, s, N_HEADS, HEAD_DIM), q_norm_w[l])
        kv = kvs.reshape(b, s, 6, N_KV, HEAD_DIM)
        k_c = rms_norm(compress(kv[:, :, 0], cmp_pos_k[l], cmp_k_w1[l], cmp_k_w2[l]), k_norm_w[l, 0])
        v_c = compress(kv[:, :, 1], cmp_pos_v[l], cmp_v_w1[l], cmp_v_w2[l])
        k_s = rms_norm(kv[:, :, 2], k_norm_w[l, 1])
        k_w = rms_norm(kv[:, :, 4], k_norm_w[l, 2])
        gates = jax.nn.sigmoid(gate_logits.reshape(b, s, N_HEADS, 3))
        o_attn = nsa_attention(q, gates, k_c, v_c, k_s, kv[:, :, 3], k_w, kv[:, :, 5], rel_bias)
        y_conv = b_gate * short_conv(c_gate * u, conv_w[l])
        mix = jnp.concatenate([o_attn, y_conv], axis=-1).reshape(b, s, N_MIX_GROUPS, HEAD_DIM)
        mix = rms_norm(mix, out_norm_w[l].reshape(N_MIX_GROUPS, HEAD_DIM)).reshape(b, s, D_MIX)
        x = x + g1 * (mix @ w_out[l])

        h2 = rms_norm(x, norm2_w[l]) * (1 + sc2) + sh2
        x = x + g2 * hier_moe(h2, w_group[l], b_group[l], w_expert[l], b_expert[l], w1[l], w3[l], w2[l])
    return x
```

```python
import math
import threading
from contextlib import ExitStack

import numpy as np
import concourse.bass as bass
import concourse.mybir as mybir
from concourse.bass_utils import run_bass_kernel_spmd

F32 = mybir.dt.float32
BF16 = mybir.dt.bfloat16
I32 = mybir.dt.int32
AF = mybir.ActivationFunctionType
ALU = mybir.AluOpType
AX = mybir.AxisListType
NEG = -30000.0
EPS = 1e-6
SAME_ENGINE_SYNC = True


class Buf:
    __slots__ = ("w", "r", "ds", "name")

    def __init__(self, name=""):
        self.w = []
        self.r = {}
        self.ds = {}
        self.name = name


class _Task:
    def __init__(self, fn):
        self.fn = fn
        self.go = threading.Event()
        self.back = threading.Event()
        self.done = False
        self.exc = None
        self.th = threading.Thread(target=self._run, daemon=True)
        self.th.start()

    def _run(self):
        self.go.wait()
        self.go.clear()
        _TL.task = self
        try:
            self.fn()
        except BaseException as e:
            self.exc = e
        self.done = True
        self.back.set()

    def step(self):
        self.go.set()
        self.back.wait()
        self.back.clear()
        if self.exc is not None:
            raise self.exc


_TL = threading.local()


def weave_yield():
    t = getattr(_TL, "task", None)
    if t is not None:
        t.back.set()
        t.go.wait()
        t.go.clear()


def weave(fns, width):
    it = iter(fns)
    active = []
    while True:
        while len(active) < width:
            f_ = next(it, None)
            if f_ is None:
                break
            active.append(_Task(f_))
        if not active:
            break
        for t in list(active):
            t.step()
            if t.done:
                active.remove(t)


def weave_slots(make_body, nslots, nitems):
    bodies = [make_body(k) for k in range(nslots)]
    done = [False] * nitems

    def task(i):
        while i >= nslots and not done[i - nslots]:
            weave_yield()
        bodies[i % nslots](i)
        done[i] = True

    weave([(lambda i=i: task(i)) for i in range(nitems)], nslots)


class Sched:
    def __init__(self, nc, es):
        self.nc = nc
        self.es = es
        self.E = {"pe": nc.tensor, "act": nc.scalar, "dve": nc.vector, "pool": nc.gpsimd, "sp": nc.sync}
        self.sem = {k: es.enter_context(nc.semaphore("cs_" + k)) for k in ("pe", "act", "dve", "pool")}
        self.cnt = {k: 0 for k in self.sem}
        self.seen = {k: {} for k in self.E}
        self.dbufs = []
        self.free_dsems = {"sw": [], "hw": []}
        self.bar = es.enter_context(nc.semaphore("bar"))
        self.barc = 0
        self.nd = 0

    def _wait(self, e, ev):
        if ev is None:
            return
        sem, val, owner = ev
        if owner == e and (e == "pe" or not SAME_ENGINE_SYNC):
            return
        if self.seen[e].get(sem, 0) >= val:
            return
        self.E[e].wait_ge(sem, val)
        self.seen[e][sem] = val

    def _deps(self, e, r, w):
        for b in r:
            for ev in b.w:
                self._wait(e, ev)
        for b in w:
            for ev in b.w:
                self._wait(e, ev)
            for ev in b.r.values():
                self._wait(e, ev)

    def _rec(self, ev, r, w):
        for b in r:
            b.r[ev[0]] = ev
        for b in w:
            if ev[2] == "dma":
                b.w = [o for o in b.w if o[2] == "dma" and o[0] != ev[0]] + [ev]
            else:
                b.w = [ev]
            b.r = {}

    def op(self, e, fn, r=(), w=()):
        self._deps(e, r, w)
        ins = fn(self.E[e])
        self.cnt[e] += 1
        ins.then_inc(self.sem[e], 1)
        self._rec((self.sem[e], self.cnt[e], e), r, w)
        weave_yield()

    def dma(self, q, fn, owner, r=(), w=()):
        self._deps(q, r, w)
        kind = "sw" if q == "pool" else "hw"
        if kind not in owner.ds:
            if self.free_dsems[kind]:
                owner.ds[kind] = list(self.free_dsems[kind].pop())
            else:
                self.nd += 1
                owner.ds[kind] = [self.es.enter_context(self.nc.semaphore("ds%d" % self.nd)), 0]
            self.dbufs.append((owner, kind))
        ins = fn(self.E[q])
        d = owner.ds[kind]
        d[1] += 16
        ins.then_inc(d[0], 16)
        self._rec((d[0], d[1], "dma"), r, w)
        weave_yield()

    def barrier(self):
        for k in self.sem:
            self._wait("sp", (self.sem[k], self.cnt[k], k))
        for b, kind in self.dbufs:
            self._wait("sp", (b.ds[kind][0], b.ds[kind][1], "dma"))
        for b, kind in self.dbufs:
            self.free_dsems[kind].append(tuple(b.ds.pop(kind)))
        self.dbufs = []
        self.barc += 1
        self.E["sp"].sem_inc(self.bar, 1)
        for k in self.sem:
            self.E[k].wait_ge(self.bar, self.barc)


def t5_bucket_np(dist):
    n = np.maximum(dist, 0)
    nf = np.maximum(n, 1).astype(np.float32)
    large = 16 + (np.log(nf / np.float32(16)) / np.float32(math.log(8.0)) * np.float32(16)).astype(np.int32)
    large = np.minimum(large, 31)
    return np.where(n < 16, n, large)


def oh_table(dists, lo_valid=0, hi_valid=None):
    L = len(dists)
    t = np.zeros((33, L), np.float32)
    valid = dists >= lo_valid
    if hi_valid is not None:
        valid &= dists < hi_valid
    bk = t5_bucket_np(dists)
    for i in range(L):
        if valid[i]:
            t[bk[i], i] += 1.0
            t[31, i] -= 1.0
        else:
            t[32, i] = NEG
    return t


def build(S, C):
    NB = S // 128
    NOWN = NB // 4
    NCMP = S // 16 - 1
    NSEL = S // 64
    NCH = (NSEL + 63) // 64
    NCT = (NCMP + 127) // 128
    NBLK = (NOWN * 256 + 64 * 127) // 128
    NROWS = NBLK * 128

    nc = bass.Bass("TRN2", target_bir_lowering=False)

    def din(name, shape, dt=F32):
        return nc.dram_tensor(name, list(shape), dt, kind="ExternalInput").ap()

    def dscr(name, shape, dt):
        return nc.dram_tensor(name, list(shape), dt).ap()

    x_all = din("x_all", [S, 1024])
    x_own = din("x_own", [NOWN * 128, 1024])
    x_prev = din("x_prev", [NOWN * 2, 1024])
    c_lay = din("c_lay", [128, 8])
    w_ada = din("w_ada", [1024, 6144])
    b_ada = din("b_ada", [1, 6144])
    norm1_w = din("norm1_w", [1, 1024])
    w_in = din("w_in", [1024, 2840])
    q_norm_w = din("q_norm_w", [1, 64])
    k_norm_w = din("k_norm_w", [1, 192])
    cmp_pos_k = din("cmp_pos_k", [128, 16, 1])
    cmp_pos_v = din("cmp_pos_v", [128, 16, 1])
    cmp_k_w1 = din("cmp_k_w1", [2048, 256])
    cmp_k_w2 = din("cmp_k_w2", [256, 64])
    cmp_v_w1 = din("cmp_v_w1", [2048, 256])
    cmp_v_w2 = din("cmp_v_w2", [256, 64])
    conv_wl = din("conv_wl", [128, 12])
    onw_c = din("onw_c", [128, 4])
    onw_a = din("onw_a", [1, 512])
    w_out = din("w_out", [1024, 1024])
    rel_bias = din("rel_bias", [32, 8])
    norm2_w = din("norm2_w", [1, 1024])
    w_rt = din("w_rt", [1024, 72])
    b_rt = din("b_rt", [1, 72])
    w1 = din("w1", [64 * 128, 4096])
    w3 = din("w3", [64 * 128, 4096])
    w2 = din("w2", [64 * 128, 4096])
    c_blk = din("c_blk", [128, NBLK])
    c_pidx = din("c_pidx", [128, 1])
    c_ident = din("c_ident", [128, 128])
    c_anti = din("c_anti", [128, 128])
    c_anti48 = din("c_anti48", [48, 48])
    c_bd = din("c_bd", [128, 128])
    c_triu = din("c_triu", [128, 128])
    c_erow = din("c_erow", [128, 4096])
    c_ohs = din("c_ohs", [33, 768])
    c_ohw = din("c_ohw", [33, 1152])
    c_ohq = din("c_ohq", [33, 880])
    c_ohk = din("c_ohk", [33, 4224])
    c_hi = din("c_hi", [128, 9])
    c_lo = din("c_lo", [128, 9])
    c_pad = din("c_pad", [128, 1])
    out = nc.dram_tensor("out", [NOWN * 128, 1024], F32, kind="ExternalOutput").ap()

    qT_d = dscr("qT_d", [NOWN, 128, 512], BF16)
    gates_d = dscr("gates_d", [NOWN, 128, 24], F32)
    mixc_d = dscr("mixc_d", [NOWN, 128, 512], BF16)
    mixa_d = dscr("mixa_d", [NOWN, 128, 512], BF16)
    x1_d = dscr("x1_d", [NOWN * 128, 1024], F32)
    fs_d = dscr("fs_d", [8, 768], BF16)
    fw_d = dscr("fw_d", [8, 1152], BF16)
    fq_d = dscr("fq_d", [8, 880], F32)
    fk_d = dscr("fk_d", [8, 4224], BF16)
    xdisp_d = dscr("xdisp_d", [NROWS, 1024], BF16)
    ydisp_d = dscr("ydisp_d", [NROWS, 1024], BF16)
    D_qT, D_gates, D_mixc, D_mixa, D_x1 = Buf(), Buf(), Buf(), Buf(), Buf()
    D_fs, D_fw, D_fq, D_fk, D_xd, D_yd = Buf(), Buf(), Buf(), Buf(), Buf(), Buf()

    with ExitStack() as es:
        sc = Sched(nc, es)
        op, dma = sc.op, sc.dma

        def sb(es_, name, shape, dt=F32):
            return es_.enter_context(nc.sbuf_tensor(name, list(shape), dt))

        tpb = es.enter_context(nc.psum_tensor("tpb", [128, 1024], BF16))
        pj = es.enter_context(nc.psum_tensor("pj", [128, 1024], F32))
        st = [es.enter_context(nc.psum_tensor("st%d" % i, [128, 512], F32)) for i in range(2)]
        oa = [es.enter_context(nc.psum_tensor("oa%d" % i, [128, 512], F32)) for i in range(2)]
        ms = es.enter_context(nc.psum_tensor("ms", [128, 512], F32))
        B_tpb, B_pjA, B_pjB, B_ms = Buf(), Buf(), Buf(), Buf()
        B_st = [Buf(), Buf()]
        B_oa = [Buf(), Buf()]

        identb = sb(es, "identb", [128, 128], BF16)
        antib = sb(es, "antib", [128, 128], BF16)
        identf = sb(es, "identf", [128, 128])
        ones1 = sb(es, "ones1", [1, 128])
        epsc = sb(es, "epsc", [128, 1])
        B_cst = Buf()
        dma("pool", lambda e: e.dma_start(out=identb[:], in_=c_ident), B_cst, w=[B_cst])
        dma("pool", lambda e: e.dma_start(out=antib[:], in_=c_anti), B_cst, w=[B_cst])
        dma("sp", lambda e: e.dma_start(out=identf[:], in_=c_ident), B_cst, w=[B_cst])
        B_ones = Buf()
        op("dve", lambda e: e.memset(ones1[:], 1.0), w=[B_ones])
        op("dve", lambda e: e.memset(epsc[:], EPS), w=[B_ones])
        qwr = sb(es, "qwr", [128, 64])
        kwr = sb(es, "kwr", [128, 192])
        onar = sb(es, "onar", [128, 512])
        brt = sb(es, "brt", [128, 72])
        B_qwr, B_kwr, B_c2 = Buf(), Buf(), Buf()
        esu = ExitStack()
        zt = sb(esu, "zt", [128, 1024], BF16)
        B_zt = Buf()
        op("pool", lambda e: e.memset(zt[:], 0.0), w=[B_zt])
        for k in range(NROWS // 128):
            dma("sp", lambda e, k=k: e.dma_start(out=xdisp_d[k * 128:(k + 1) * 128, :], in_=zt[:]), B_zt, r=[B_zt], w=[])
        D_xd.w = [(B_zt.ds["hw"][0], B_zt.ds["hw"][1], "dma")]

        def bcast_row(dst_ps, dbuf, row_ap, n):
            op("pe", lambda e: e.matmul(dst_ps, lhsT=ones1[0:1, :], rhs=row_ap, start=True, stop=True),
               r=[B_ones, B_row], w=[dbuf])

        def rstd_from_ss(ss_ap, bufs, inv_n):
            op("act", lambda e: e.activation(out=ss_ap, in_=ss_ap, func=AF.Ln, scale=inv_n, bias=epsc[0:ss_ap.shape[0], :]),
               r=bufs + [B_ones], w=bufs)
            op("act", lambda e: e.activation(out=ss_ap, in_=ss_ap, func=AF.Exp, scale=-0.5), r=bufs, w=bufs)

        rows = sb(esu, "rows", [1, 1024 + 1024 + 64 + 192 + 512 + 72])
        B_row = Buf()
        R_N1, R_N2, R_QW, R_KW, R_ONA, R_BRT = 0, 1024, 2048, 2112, 2304, 2816
        for src, off, n in ((norm1_w, R_N1, 1024), (norm2_w, R_N2, 1024), (q_norm_w, R_QW, 64),
                            (k_norm_w, R_KW, 192), (onw_a, R_ONA, 512), (b_rt, R_BRT, 72)):
            dma("sp", lambda e, src=src, off=off, n=n: e.dma_start(out=rows[0:1, off:off + n], in_=src), B_row, w=[B_row])

        cond = sb(esu, "cond", [128, 8])
        condr = sb(esu, "condr", [128, 8, 128])
        B_cond = Buf()
        dma("sp", lambda e: e.dma_start(out=cond[:], in_=c_lay), B_cond, w=[B_cond])
        ctmp = sb(esu, "ctmp", [128, 8])
        B_ctmp = Buf()
        op("act", lambda e: e.activation(out=ctmp[:], in_=cond[:], func=AF.Exp, scale=-1.0), r=[B_cond], w=[B_ctmp])
        op("dve", lambda e: e.tensor_scalar_add(out=ctmp[:], in0=ctmp[:], scalar1=1.0), r=[B_ctmp], w=[B_ctmp])
        op("dve", lambda e: e.reciprocal(out=ctmp[:], in_=ctmp[:]), r=[B_ctmp], w=[B_ctmp])
        op("dve", lambda e: e.tensor_mul(out=cond[:], in0=cond[:], in1=ctmp[:]), r=[B_ctmp, B_cond], w=[B_cond])
        B_condr = Buf()
        op("dve", lambda e: e.tensor_copy(out=condr[:], in_=cond[:].unsqueeze(2).to_broadcast([128, 8, 128])),
           r=[B_cond], w=[B_condr])
        w_ada_v = w_ada.rearrange("(kt p) n -> p kt n", p=128)
        MODS = sb(esu, "MODS", [128, 6, 1024])
        B_mods = Buf()

        def compute_mods():
            with ExitStack() as es2:
                was = [sb(es2, "wa%d" % q, [128, 8, 512]) for q in range(2)]
                brows = [sb(es2, "brow%d" % q, [1, 512]) for q in range(2)]
                B_was, B_brows = [Buf(), Buf()], [Buf(), Buf()]
                for ch in range(12):
                    c0 = ch * 512
                    wa, brow, B_wa, B_brow = was[ch % 2], brows[ch % 2], B_was[ch % 2], B_brows[ch % 2]
                    dma("sp", lambda e: e.dma_start(out=wa[:], in_=w_ada_v[:, :, c0:c0 + 512]), B_wa, w=[B_wa])
                    dma("sp", lambda e: e.dma_start(out=brow[:], in_=b_ada[0:1, c0:c0 + 512]), B_brow, w=[B_brow])
                    for kt in range(8):
                        op("pe", lambda e, kt=kt: e.matmul(pj[:, 0:512], lhsT=condr[:, kt, :], rhs=wa[:, kt, :],
                                                           start=(kt == 0), stop=False),
                           r=[B_condr, B_wa], w=[B_pjA])
                    op("pe", lambda e: e.matmul(pj[:, 0:512], lhsT=ones1[0:1, :], rhs=brow[:], start=False, stop=True),
                       r=[B_ones, B_brow], w=[B_pjA])
                    op("act", lambda e: e.activation(out=MODS[:, ch // 2, (ch % 2) * 512:(ch % 2) * 512 + 512],
                                                     in_=pj[:, 0:512], func=AF.Copy), r=[B_pjA], w=[B_mods])
                for (slot, roff) in ((1, R_N1), (4, R_N2)):
                    for hh in range(2):
                        bcast_row(pj[:, 0:512], B_pjA, rows[0:1, roff + hh * 512: roff + hh * 512 + 512], 512)
                        seg = MODS[:, slot, hh * 512:(hh + 1) * 512]
                        op("dve", lambda e, seg=seg: e.scalar_tensor_tensor(out=seg, in0=seg, scalar=1.0, in1=pj[:, 0:512],
                                                                            op0=ALU.add, op1=ALU.mult),
                           r=[B_pjA, B_mods], w=[B_mods])

        compute_mods()
        mods_d = dscr("mods_d", [128, 6144], F32)
        D_mods = Buf()
        dma("sp", lambda e: e.dma_start(out=mods_d, in_=MODS[:].rearrange("p k n -> p (k n)")), B_mods, r=[B_mods], w=[D_mods])
        bcast_row(ms[:, 0:64], B_ms, rows[0:1, R_QW:R_QW + 64], 64)
        op("act", lambda e: e.activation(out=qwr[:], in_=ms[:, 0:64], func=AF.Copy, scale=0.125), r=[B_ms], w=[B_qwr])
        bcast_row(ms[:, 0:192], B_ms, rows[0:1, R_KW:R_KW + 192], 192)
        op("act", lambda e: e.activation(out=kwr[:], in_=ms[:, 0:192], func=AF.Copy), r=[B_ms], w=[B_kwr])
        bcast_row(ms[:, 0:512], B_ms, rows[0:1, R_ONA:R_ONA + 512], 512)
        op("act", lambda e: e.activation(out=onar[:], in_=ms[:, 0:512], func=AF.Copy), r=[B_ms], w=[B_kwr])
        bcast_row(ms[:, 0:72], B_ms, rows[0:1, R_BRT:R_BRT + 72], 72)
        op("act", lambda e: e.activation(out=brt[:], in_=ms[:, 0:72], func=AF.Copy), r=[B_ms], w=[B_c2])
        sc.barrier()
        esu.close()

        def load_mod(es_, name, k):
            t = sb(es_, name, [128, 1024])
            dma("sp", lambda e: e.dma_start(out=t[:], in_=mods_d[:, k * 1024:(k + 1) * 1024]), B_mods, r=[D_mods], w=[B_mods])
            return t[:]

        eKV = ExitStack()
        KA = [sb(eKV, "KA%d" % g, [128, S], BF16) for g in range(2)]
        VS = sb(eKV, "VS", [128, NB, 2, 65], BF16)
        KC = sb(eKV, "KC", [128, NCT * 128], BF16)
        VC = sb(eKV, "VC", [128, NCT, 2, 65], BF16)
        kw_d = dscr("kw_d", [NB, 128, 128], BF16)
        vw_d = dscr("vw_d", [NB, 128, 130], BF16)
        D_kw = [Buf() for _ in range(NB)]
        D_vw = [Buf() for _ in range(NB)]
        B_KA = [[Buf() for _ in range(NB)] for _ in range(2)]
        B_VS = [Buf() for _ in range(NB)]
        B_KC, B_VC = Buf(), Buf()
        B_init = Buf()
        op("pool", lambda e: e.memset(VS[:, :, :, 64:65], 1.0), w=B_VS)
        op("pool", lambda e: e.memset(VC[:], 0.0), w=[B_VC])
        op("pool", lambda e: e.memset(VC[:, :, :, 64:65], 1.0), w=[B_VC])
        op("pool", lambda e: e.memset(KC[:], 0.0), w=[B_KC])
        for per in range(S // 4096 if S >= 4096 else 1):
            n = min(4096, S)
            dma("pool", lambda e, per=per, n=n: e.dma_start(out=KA[0][64:128, per * 4096:per * 4096 + n], in_=c_erow[64:128, 0:n]),
                B_init, w=[B_init] + B_KA[0])
            dma("pool", lambda e, per=per, n=n: e.dma_start(out=KA[1][0:64, per * 4096:per * 4096 + n], in_=c_erow[0:64, 0:n]),
                B_init, w=[B_init] + B_KA[1])
        eA = ExitStack()
        SH1 = load_mod(eA, "SH1", 0)
        A1 = load_mod(eA, "A1", 1)

        def rms_mod(xt_ap, hb_ap, A, Bm, npart, bufs_x, bufs_h, scr, ss, B_scr):
            op("act", lambda e: e.activation(out=scr[0:npart, :], in_=xt_ap, func=AF.Square, accum_out=ss[0:npart, :]),
               r=bufs_x, w=[B_scr])
            rstd_from_ss(ss[0:npart, :], [B_scr], 1.0 / 1024)
            op("dve", lambda e: e.scalar_tensor_tensor(out=scr[0:npart, :], in0=xt_ap, scalar=ss[0:npart, 0:1],
                                                       in1=A[0:npart, :], op0=ALU.mult, op1=ALU.mult),
               r=bufs_x + [B_scr, B_mods], w=[B_scr])
            op("pool", lambda e: e.tensor_tensor(out=hb_ap, in0=scr[0:npart, :], in1=Bm[0:npart, :], op=ALU.add),
               r=[B_scr, B_mods], w=bufs_h)

        w_in_v = w_in.rearrange("(kt p) n -> p kt n", p=128)

        with ExitStack() as e0:
            wq = sb(e0, "wq", [128, 8, 512], BF16)
            wg = sb(e0, "wg", [128, 8, 24], BF16)
            wc = sb(e0, "wc", [128, 8, 1536], BF16)
            B_w0 = Buf()
            for kt in range(8):
                dma("pool", lambda e, kt=kt: e.dma_start(out=wq[:, kt, :], in_=w_in_v[:, kt, 0:512]), B_w0, w=[B_w0])
                dma("pool", lambda e, kt=kt: e.dma_start(out=wg[:, kt, :], in_=w_in_v[:, kt, 1280:1304]), B_w0, w=[B_w0])
                dma("pool", lambda e, kt=kt: e.dma_start(out=wc[:, kt, :], in_=w_in_v[:, kt, 1304:2840]), B_w0, w=[B_w0])
            convw = sb(e0, "convw", [128, 12])
            onwc = sb(e0, "onwc", [128, 4])
            bdm = sb(e0, "bdm", [128, 128])
            padf = sb(e0, "padf", [128, 1])
            B_c0 = Buf()
            for t_, s_ in ((convw, conv_wl), (onwc, onw_c), (bdm, c_bd), (padf, c_pad)):
                dma("sp", lambda e, t_=t_, s_=s_: e.dma_start(out=t_[:], in_=s_), B_c0, w=[B_c0])

            def make_a0(sl):
                xt = sb(e0, "xt0_%d" % sl, [128, 1024])
                xp = sb(e0, "xp0_%d" % sl, [2, 1024])
                scr = sb(e0, "scr0_%d" % sl, [128, 1024])
                ss = sb(e0, "ss0_%d" % sl, [128, 1])
                hb = sb(e0, "hb0_%d" % sl, [128, 1024], BF16)
                hbp = sb(e0, "hbp0_%d" % sl, [2, 1024], BF16)
                hT = sb(e0, "hT0_%d" % sl, [128, 8, 130], BF16)
                qf = sb(e0, "qf_%d" % sl, [128, 512])
                qsq = sb(e0, "qsq_%d" % sl, [128, 512])
                qss = sb(e0, "qss_%d" % sl, [128, 8])
                qp = sb(e0, "qp_%d" % sl, [128, 512], BF16)
                qTs = sb(e0, "qTs_%d" % sl, [128, 512], BF16)
                gts = sb(e0, "gts_%d" % sl, [128, 24])
                uT = sb(e0, "uT_%d" % sl, [128, 130])
                cu = sb(e0, "cu_%d" % sl, [128, 130])
                yc = sb(e0, "yc_%d" % sl, [128, 128])
                ysq = sb(e0, "ysq_%d" % sl, [128, 128])
                yrs = sb(e0, "yrs_%d" % sl, [128, 128])
                mixc = sb(e0, "mixc_%d" % sl, [128, 4, 128], BF16)
                B_xt, B_xp, B_scr, B_hb, B_hbp, B_hT = Buf(), Buf(), Buf(), Buf(), Buf(), Buf()
                B_qf, B_qsq, B_qss, B_qp, B_qTs, B_gts = Buf(), Buf(), Buf(), Buf(), Buf(), Buf()
                B_uT, B_cu, B_yc, B_ysq, B_yrs, B_mixc = Buf(), Buf(), Buf(), Buf(), Buf(), Buf()
                TP, B_TP = (tpb[:], B_tpb) if sl == 0 else (oa[0][:].bitcast(BF16), B_oa[0])
                PQ, B_PQ = (pj[:, 0:512], B_pjA) if sl == 0 else (pj[:, 512:1024], B_pjB)
                MS, B_MS = (ms[:], B_ms) if sl == 0 else (oa[1][:], B_oa[1])
                CV, B_CV = st[sl], B_st[sl]

                def body(i):
                    dma("sp", lambda e: e.dma_start(out=xt[:], in_=x_own[i * 128:(i + 1) * 128, :]), B_xt, w=[B_xt])
                    dma("sp", lambda e: e.dma_start(out=xp[:], in_=x_prev[i * 2:(i + 1) * 2, :]), B_xp, w=[B_xp])
                    rms_mod(xt[:], hb[:], A1, SH1, 128, [B_xt], [B_hb], scr, ss, B_scr)
                    rms_mod(xp[:], hbp[:], A1, SH1, 2, [B_xp], [B_hbp], scr, ss, B_scr)
                    for kt in range(8):
                        op("pe", lambda e, kt=kt: e.transpose(out=TP[:, kt * 128:(kt + 1) * 128], in_=hb[:, kt * 128:(kt + 1) * 128],
                                                              identity=identb[:]), r=[B_hb, B_cst], w=[B_TP])
                    op("act", lambda e: e.activation(out=hT[:, :, 2:130], in_=TP.rearrange("p (k t) -> p k t", k=8), func=AF.Copy),
                       r=[B_TP], w=[B_hT])
                    for kt in range(8):
                        op("pe", lambda e, kt=kt: e.transpose(out=TP[:, kt * 2:(kt + 1) * 2], in_=hbp[:, kt * 128:(kt + 1) * 128],
                                                              identity=identb[0:2, 0:2]), r=[B_hbp, B_cst], w=[B_TP])
                    op("act", lambda e: e.activation(out=hT[:, :, 0:2], in_=TP[:, 0:16].rearrange("p (k t) -> p k t", k=8), func=AF.Copy),
                       r=[B_TP], w=[B_hT])
                    for kt in range(8):
                        op("pe", lambda e, kt=kt: e.matmul(PQ, lhsT=hT[:, kt, 2:130], rhs=wq[:, kt, :],
                                                           start=(kt == 0), stop=(kt == 7)), r=[B_hT, B_w0], w=[B_PQ])
                    for kt in range(8):
                        op("pe", lambda e, kt=kt: e.matmul(MS[:, 0:24], lhsT=hT[:, kt, 2:130], rhs=wg[:, kt, :],
                                                           start=(kt == 0), stop=(kt == 7)), r=[B_hT, B_w0], w=[B_MS])
                    op("act", lambda e: e.activation(out=gts[:], in_=MS[:, 0:24], func=AF.Exp, scale=-1.0), r=[B_MS], w=[B_gts])
                    op("dve", lambda e: e.tensor_scalar_add(out=gts[:], in0=gts[:], scalar1=1.0), r=[B_gts], w=[B_gts])
                    op("dve", lambda e: e.reciprocal(out=gts[:], in_=gts[:]), r=[B_gts], w=[B_gts])
                    dma("sp", lambda e: e.dma_start(out=gates_d[i], in_=gts[:]), B_gts, r=[B_gts], w=[D_gates])
                    op("act", lambda e: e.activation(out=qf[:], in_=PQ, func=AF.Copy), r=[B_PQ], w=[B_qf])
                    op("pool", lambda e: e.tensor_tensor(out=qsq[:], in0=qf[:], in1=qf[:], op=ALU.mult), r=[B_qf], w=[B_qsq])
                    op("dve", lambda e: e.tensor_reduce(out=qss[:], in_=qsq[:].rearrange("p (h d) -> p h d", d=64), axis=AX.X, op=ALU.add),
                       r=[B_qsq], w=[B_qss])
                    rstd_from_ss(qss[:], [B_qss], 1.0 / 64)
                    op("dve", lambda e: e.tensor_tensor(out=qsq[:].rearrange("p (h d) -> p h d", d=64),
                                                        in0=qf[:].rearrange("p (h d) -> p h d", d=64),
                                                        in1=qss[:].unsqueeze(2).to_broadcast([128, 8, 64]), op=ALU.mult),
                       r=[B_qf, B_qss], w=[B_qsq])
                    op("dve", lambda e: e.tensor_tensor(out=qp[:].rearrange("p (a g d) -> p g a d", a=4, g=2, d=64),
                                                        in0=qsq[:].rearrange("p (g a d) -> p g a d", g=2, a=4, d=64),
                                                        in1=qwr[:].unsqueeze(1).unsqueeze(1).to_broadcast([128, 2, 4, 64]), op=ALU.mult),
                       r=[B_qsq, B_qwr], w=[B_qp])
                    for a in range(4):
                        op("pe", lambda e, a=a: e.transpose(out=TP[:, a * 128:(a + 1) * 128], in_=qp[:, a * 128:(a + 1) * 128],
                                                            identity=identb[:]), r=[B_qp, B_cst], w=[B_TP])
                    op("act", lambda e: e.activation(out=qTs[:], in_=TP[:, 0:512], func=AF.Copy), r=[B_TP], w=[B_qTs])
                    dma("sp", lambda e: e.dma_start(out=qT_d[i], in_=qTs[:]), B_qTs, r=[B_qTs], w=[D_qT])
                    for ct in range(4):
                        ps = CV
                        Bp = B_CV
                        for (k, col0, t0) in ((0, 0, 2), (1, 512, 0), (2, 1024, 0)):
                            nt = 130 - t0
                            for kt in range(8):
                                op("pe", lambda e, kt=kt, k=k, col0=col0, t0=t0, nt=nt: e.matmul(
                                    ps[:, k * 130:k * 130 + nt], lhsT=wc[:, kt, col0 + ct * 128: col0 + ct * 128 + 128],
                                    rhs=hT[:, kt, t0:130], start=(kt == 0), stop=(kt == 7)), r=[B_hT, B_w0], w=[Bp])
                        op("act", lambda e: e.activation(out=uT[:], in_=ps[:, 260:390], func=AF.Copy), r=[Bp], w=[B_uT])
                        op("dve", lambda e: e.tensor_tensor(out=cu[:], in0=ps[:, 130:260], in1=uT[:], op=ALU.mult), r=[Bp, B_uT], w=[B_cu])
                        if i == 0:
                            op("dve", lambda e: e.tensor_scalar(out=cu[:, 0:2], in0=cu[:, 0:2], scalar1=padf[:, 0:1], scalar2=None,
                                                                op0=ALU.mult), r=[B_cu, B_c0], w=[B_cu])
                        op("dve", lambda e: e.tensor_scalar(out=yc[:], in0=cu[:, 0:128], scalar1=convw[:, ct * 3:ct * 3 + 1], scalar2=None,
                                                            op0=ALU.mult), r=[B_cu, B_c0], w=[B_yc])
                        for k in (1, 2):
                            op("dve", lambda e, k=k: e.scalar_tensor_tensor(out=yc[:], in0=cu[:, k:k + 128],
                                                                            scalar=convw[:, ct * 3 + k:ct * 3 + k + 1], in1=yc[:],
                                                                            op0=ALU.mult, op1=ALU.add), r=[B_cu, B_c0, B_yc], w=[B_yc])
                        op("dve", lambda e: e.tensor_tensor(out=yc[:], in0=yc[:], in1=ps[:, 0:128], op=ALU.mult), r=[Bp, B_yc], w=[B_yc])
                        op("pool", lambda e: e.tensor_tensor(out=ysq[:], in0=yc[:], in1=yc[:], op=ALU.mult), r=[B_yc], w=[B_ysq])
                        op("pe", lambda e: e.matmul(MS[:, 0:128], lhsT=bdm[:], rhs=ysq[:], start=True, stop=True), r=[B_c0, B_ysq], w=[B_MS])
                        op("act", lambda e: e.activation(out=yrs[:], in_=MS[:, 0:128], func=AF.Ln, scale=1.0 / 64, bias=epsc[:]),
                           r=[B_MS, B_ones], w=[B_yrs])
                        op("act", lambda e: e.activation(out=yrs[:], in_=yrs[:], func=AF.Exp, scale=-0.5), r=[B_yrs], w=[B_yrs])
                        op("dve", lambda e: e.scalar_tensor_tensor(out=mixc[:, ct, :], in0=yc[:], scalar=onwc[:, ct:ct + 1], in1=yrs[:],
                                                                   op0=ALU.mult, op1=ALU.mult), r=[B_yc, B_yrs, B_c0], w=[B_mixc])
                    dma("sp", lambda e: e.dma_start(out=mixc_d[i], in_=mixc[:].rearrange("p c t -> p (c t)")), B_mixc, r=[B_mixc], w=[D_mixc])
                return body

            weave_slots(make_a0, 2, NOWN)
            sc.barrier()

        with ExitStack() as e1:
            e1a = ExitStack()
            e1_real = e1
            e1 = e1a
            wkv = sb(e1, "wkv", [128, 8, 768], BF16)
            B_wkv = Buf()
            for kt in range(8):
                dma("pool", lambda e, kt=kt: e.dma_start(out=wkv[:, kt, :], in_=w_in_v[:, kt, 512:1280]), B_wkv, w=[B_wkv])
            cw1 = [sb(e1, "cw1_%d" % k, [128, 16, 256], BF16) for k in range(2)]
            cw2 = [sb(e1, "cw2_%d" % k, [128, 2, 64], BF16) for k in range(2)]
            cpos = [sb(e1, "cpos_%d" % k, [128, 16, 1], BF16) for k in range(2)]
            B_cw = Buf()
            for k, (a1_, a2_, ap_) in enumerate(((cmp_k_w1, cmp_k_w2, cmp_pos_k), (cmp_v_w1, cmp_v_w2, cmp_pos_v))):
                dma("pool", lambda e, k=k, a1_=a1_: e.dma_start(out=cw1[k][:], in_=a1_.rearrange("(kt p) n -> p kt n", p=128)), B_cw, w=[B_cw])
                dma("pool", lambda e, k=k, a2_=a2_: e.dma_start(out=cw2[k][:], in_=a2_.rearrange("(kt p) n -> p kt n", p=128)), B_cw, w=[B_cw])
                dma("pool", lambda e, k=k, ap_=ap_: e.dma_start(out=cpos[k][:], in_=ap_), B_cw, w=[B_cw])
            cb1 = [sb(e1, "cb1_%d" % k, [128, 2]) for k in range(2)]
            B_cb1 = Buf()
            for k in range(2):
                for hf in range(2):
                    for kt in range(16):
                        op("pe", lambda e, k=k, hf=hf, kt=kt: e.matmul(ms[:, hf:hf + 1], lhsT=cw1[k][:, kt, hf * 128:(hf + 1) * 128],
                                                                      rhs=cpos[k][:, kt, :], start=(kt == 0), stop=(kt == 15)),
                           r=[B_cw], w=[B_ms])
                op("act", lambda e, k=k: e.activation(out=cb1[k][:], in_=ms[:, 0:2], func=AF.Copy), r=[B_ms], w=[B_cb1])
            KSd = [[[sb(e1, "KS%d%d%d" % (q, k, g), [128, 544], BF16) for g in range(2)] for k in range(2)] for q in range(2)]
            B_KSd = [Buf(), Buf()]
            for q in range(2):
                for k in range(2):
                    for g in range(2):
                        op("pool", lambda e, q=q, k=k, g=g: e.memset(KSd[q][k][g][:], 0.0), w=[B_KSd[q]])
            NSL = 2
            xt = [sb(e1, "xt1_%d" % k, [128, 1024]) for k in range(NSL)]
            scr_ = [sb(e1, "scr1_%d" % k, [128, 1024]) for k in range(NSL)]
            ss_ = [sb(e1, "ss1_%d" % k, [128, 1]) for k in range(NSL)]
            hb_ = [sb(e1, "hb1_%d" % k, [128, 1024], BF16) for k in range(NSL)]
            hT_ = [sb(e1, "hT1_%d" % k, [128, 8, 128], BF16) for k in range(NSL)]
            kvf_ = [sb(e1, "kvf_%d" % k, [128, 768]) for k in range(NSL)]
            ksq_ = [sb(e1, "ksq_%d" % k, [128, 128]) for k in range(NSL)]
            kss_ = [sb(e1, "kss_%d" % k, [128, 2]) for k in range(NSL)]
            knb_ = [sb(e1, "knb_%d" % k, [128, 128], BF16) for k in range(NSL)]
            craw_ = [sb(e1, "craw_%d" % k, [128, 2, 2, 128], BF16) for k in range(NSL)]
            kwst = [sb(e1, "kwst%d" % k, [128, 128], BF16) for k in range(2)]
            vwst = [sb(e1, "vwst%d" % k, [128, 2, 65], BF16) for k in range(2)]
            B_kwst = [Buf(), Buf()]
            B_vwst = [Buf(), Buf()]
            for k in range(2):
                op("pool", lambda e, k=k: e.memset(vwst[k][:, :, 64:65], 1.0), w=[B_vwst[k]])
            B_xt1 = [Buf() for _ in range(NSL)]
            B_scr_ = [Buf() for _ in range(NSL)]
            B_hb_ = [Buf() for _ in range(NSL)]
            B_hT_ = [Buf() for _ in range(NSL)]
            B_kvf_ = [Buf() for _ in range(NSL)]
            B_ksq_ = [Buf() for _ in range(NSL)]
            B_kss_ = [Buf() for _ in range(NSL)]
            B_knb_ = [Buf() for _ in range(NSL)]
            B_craw_ = [Buf() for _ in range(NSL)]
            TPs = [tpb[:], st[0][:].bitcast(BF16)]
            B_TPs = [B_tpb, B_st[0]]
            PAs = [pj[:, 0:512], oa[0][:, 0:512]]
            B_PAs = [B_pjA, B_oa[0]]
            PBs = [pj[:, 512:768], oa[1][:, 0:256]]
            B_PBs = [B_pjB, B_oa[1]]
            tpc = st[1][:].bitcast(BF16)
            B_tpc = B_st[1]
            kssc = sb(e1, "kssc", [128, 2])
            B_kssc = Buf()
            hid = sb(e1, "hid", [128, 2, 32], BF16)
            hidf = sb(e1, "hidf", [128, 32])
            hide = sb(e1, "hide", [128, 32])
            kcf = sb(e1, "kcf", [32, 64])
            kcb = sb(e1, "kcb", [32, 128], BF16)
            B_hid, B_hidf, B_hide, B_kcf, B_kcb = Buf(), Buf(), Buf(), Buf(), Buf()

            def run_interleaved(gens, width):
                active = []
                it = iter(gens)
                while True:
                    while len(active) < width:
                        g_ = next(it, None)
                        if g_ is None:
                            break
                        active.append(g_)
                    if not active:
                        break
                    for g_ in list(active):
                        try:
                            next(g_)
                        except StopIteration:
                            active.remove(g_)

            def proj_block(J, jj):
                j = 4 * J + jj
                sl = j % NSL
                xb_, Bx = xt[sl], B_xt1[sl]
                scr, ss, hb, hT, kvf, ksq, kss, knb, craw = scr_[sl], ss_[sl], hb_[sl], hT_[sl], kvf_[sl], ksq_[sl], kss_[sl], knb_[sl], craw_[sl]
                B_scr1, B_hb1, B_hT1, B_kvf, B_ksq, B_kss, B_knb, B_craw = (B_scr_[sl], B_hb_[sl], B_hT_[sl], B_kvf_[sl], B_ksq_[sl], B_kss_[sl],
                                                                             B_knb_[sl], B_craw_[sl])
                tp, B_tp = TPs[sl], B_TPs[sl]
                KSr = KSd[J % 2]
                B_KS = B_KSd[J % 2]
                dma("sp", lambda e: e.dma_start(out=xb_[:], in_=x_all[j * 128:(j + 1) * 128, :]), Bx, w=[Bx])
                yield
                op("act", lambda e: e.activation(out=scr[:], in_=xb_[:], func=AF.Square, accum_out=ss[:]), r=[Bx], w=[B_scr1])
                yield
                op("act", lambda e: e.activation(out=ss[:], in_=ss[:], func=AF.Ln, scale=1.0 / 1024, bias=epsc[:]), r=[B_scr1, B_ones], w=[B_scr1])
                op("act", lambda e: e.activation(out=ss[:], in_=ss[:], func=AF.Exp, scale=-0.5), r=[B_scr1], w=[B_scr1])
                yield
                op("dve", lambda e: e.scalar_tensor_tensor(out=scr[:], in0=xb_[:], scalar=ss[:, 0:1], in1=A1, op0=ALU.mult, op1=ALU.mult),
                   r=[Bx, B_scr1, B_mods], w=[B_scr1])
                yield
                op("pool", lambda e: e.tensor_tensor(out=hb[:], in0=scr[:], in1=SH1, op=ALU.add), r=[B_scr1, B_mods], w=[B_hb1])
                yield
                for kt in range(8):
                    op("pe", lambda e, kt=kt: e.transpose(out=tp[:, kt * 128:(kt + 1) * 128], in_=hb[:, kt * 128:(kt + 1) * 128],
                                                          identity=identb[:]), r=[B_hb1, B_cst], w=[B_tp])
                yield
                op("act", lambda e: e.activation(out=hT[:].rearrange("p k t -> p (k t)"), in_=tp, func=AF.Copy), r=[B_tp], w=[B_hT1])
                yield
                for (c0, n, pdst, Bp) in ((0, 512, PAs[sl], B_PAs[sl]), (512, 256, PBs[sl], B_PBs[sl])):
                    for kt in range(8):
                        op("pe", lambda e, kt=kt, c0=c0, n=n, pdst=pdst: e.matmul(pdst, lhsT=hT[:, kt, :], rhs=wkv[:, kt, c0:c0 + n],
                                                                                start=(kt == 0), stop=(kt == 7)),
                           r=[B_hT1, B_wkv], w=[Bp])
                yield
                op("act", lambda e: e.activation(out=kvf[:, 0:512], in_=PAs[sl], func=AF.Copy), r=[B_PAs[sl]], w=[B_kvf])
                op("act", lambda e: e.activation(out=kvf[:, 512:768], in_=PBs[sl], func=AF.Copy), r=[B_PBs[sl]], w=[B_kvf])
                yield
                ws2 = j % 2
                op("dve", lambda e: e.tensor_copy(out=VS[:, j, :, 0:64], in_=kvf[:, 384:512].rearrange("p (g d) -> p g d", g=2)),
                   r=[B_kvf], w=[B_VS[j]])
                op("dve", lambda e: e.tensor_copy(out=vwst[ws2][:, :, 0:64], in_=kvf[:, 640:768].rearrange("p (g d) -> p g d", g=2)),
                   r=[B_kvf], w=[B_vwst[ws2]])
                dma("sp", lambda e: e.dma_start(out=vw_d[j], in_=vwst[ws2][:].rearrange("p g d -> p (g d)")), B_vwst[ws2],
                    r=[B_vwst[ws2]], w=[D_vw[j]])
                yield
                for (col0, kw_i, which) in ((256, 1, "slc"), (512, 2, "win")):
                    src = kvf[:, col0:col0 + 128]
                    op("pool", lambda e, src=src: e.tensor_tensor(out=ksq[:], in0=src, in1=src, op=ALU.mult), r=[B_kvf], w=[B_ksq])
                    yield
                    op("dve", lambda e: e.tensor_reduce(out=kss[:], in_=ksq[:].rearrange("p (g d) -> p g d", g=2), axis=AX.X, op=ALU.add),
                       r=[B_ksq], w=[B_kss])
                    yield
                    op("act", lambda e: e.activation(out=kss[:], in_=kss[:], func=AF.Ln, scale=1.0 / 64, bias=epsc[:]), r=[B_kss, B_ones], w=[B_kss])
                    op("act", lambda e: e.activation(out=kss[:], in_=kss[:], func=AF.Exp, scale=-0.5), r=[B_kss], w=[B_kss])
                    yield
                    op("dve", lambda e, src=src: e.tensor_tensor(out=ksq[:].rearrange("p (g d) -> p g d", g=2),
                                                                 in0=src.rearrange("p (g d) -> p g d", g=2),
                                                                 in1=kss[:].unsqueeze(2).to_broadcast([128, 2, 64]), op=ALU.mult),
                       r=[B_kvf, B_kss], w=[B_ksq])
                    op("dve", lambda e, kw_i=kw_i: e.tensor_tensor(out=knb[:].rearrange("p (g d) -> p g d", g=2),
                                                                   in0=ksq[:].rearrange("p (g d) -> p g d", g=2),
                                                                   in1=kwr[:, kw_i * 64:(kw_i + 1) * 64].unsqueeze(1).to_broadcast([128, 2, 64]),
                                                                   op=ALU.mult), r=[B_ksq, B_kwr], w=[B_knb])
                    yield
                    op("pe", lambda e: e.transpose(out=tp[:, 0:128], in_=knb[:], identity=identb[:]), r=[B_knb, B_cst], w=[B_tp])
                    yield
                    if which == "slc":
                        op("act", lambda e: e.activation(out=KA[0][0:64, j * 128:(j + 1) * 128], in_=tp[0:64, 0:128], func=AF.Copy),
                           r=[B_tp], w=[B_KA[0][j]])
                        op("act", lambda e: e.activation(out=KA[1][64:128, j * 128:(j + 1) * 128], in_=tp[64:128, 0:128], func=AF.Copy),
                           r=[B_tp], w=[B_KA[1][j]])
                    else:
                        op("act", lambda e: e.activation(out=kwst[ws2][:], in_=tp[:, 0:128], func=AF.Copy), r=[B_tp], w=[B_kwst[ws2]])
                        dma("sp", lambda e: e.dma_start(out=kw_d[j], in_=kwst[ws2][:]), B_kwst[ws2], r=[B_kwst[ws2]], w=[D_kw[j]])
                    yield
                for k in range(2):
                    base = k * 128
                    op("dve", lambda e, k=k, base=base: e.tensor_copy(out=craw[:, k, 0, :], in_=kvf[:, base:base + 128]), r=[B_kvf], w=[B_craw])
                    op("dve", lambda e, k=k, base=base: e.tensor_copy(out=craw[:, k, 1, 0:64], in_=kvf[:, base + 64:base + 128]), r=[B_kvf], w=[B_craw])
                    op("dve", lambda e, k=k, base=base: e.tensor_copy(out=craw[:, k, 1, 64:128], in_=kvf[:, base:base + 64]), r=[B_kvf], w=[B_craw])
                yield
                for k in range(2):
                    for o in range(2):
                        op("pe", lambda e, k=k, o=o: e.transpose(out=tp[:, (k * 2 + o) * 128:(k * 2 + o + 1) * 128], in_=craw[:, k, o, :],
                                                                identity=identb[:]), r=[B_craw, B_cst], w=[B_tp])
                yield
                c0 = 16 + jj * 128
                for k in range(2):
                    Ta = tp[:, (k * 2) * 128:(k * 2 + 1) * 128]
                    Tb = tp[:, (k * 2 + 1) * 128:(k * 2 + 2) * 128]
                    op("act", lambda e, k=k, Ta=Ta: e.activation(out=KSr[k][0][0:64, c0:c0 + 128], in_=Ta[0:64, :], func=AF.Copy), r=[B_tp], w=[B_KS])
                    op("act", lambda e, k=k, Tb=Tb: e.activation(out=KSr[k][0][64:128, c0 - 1:c0 + 127], in_=Tb[64:128, :], func=AF.Copy), r=[B_tp], w=[B_KS])
                    op("act", lambda e, k=k, Tb=Tb: e.activation(out=KSr[k][1][0:64, c0:c0 + 128], in_=Tb[0:64, :], func=AF.Copy), r=[B_tp], w=[B_KS])
                    op("act", lambda e, k=k, Ta=Ta: e.activation(out=KSr[k][1][64:128, c0 - 1:c0 + 127], in_=Ta[64:128, :], func=AF.Copy), r=[B_tp], w=[B_KS])
                    yield

            def compress_gen(J):
                KSr = KSd[J % 2]
                B_KS = B_KSd[J % 2]
                kss, B_kss = kssc, B_kssc
                n0 = 32 * J - 1
                nlo = max(n0, 0)
                nn = 32 * J + 31 - nlo
                col_lo = 16 * (nlo - n0)
                for k in range(2):
                    for g in range(2):
                        for hf in range(2):
                            for kt in range(16):
                                rhs_ap = bass.AP(KSr[k][g][:].tensor, KSr[k][g][:, col_lo + 2 * kt:col_lo + 2 * kt + 1].offset,
                                                 [[KSr[k][g][:].ap[0][0], 128], [16, nn]])
                                op("pe", lambda e, rhs_ap=rhs_ap, k=k, hf=hf, kt=kt: e.matmul(ms[:, 0:nn], lhsT=cw1[k][:, kt, hf * 128:(hf + 1) * 128],
                                                                                            rhs=rhs_ap, start=(kt == 0), stop=(kt == 15)),
                                   r=[B_KS, B_cw], w=[B_ms])
                            yield
                            op("dve", lambda e, k=k, hf=hf: e.tensor_scalar(out=hidf[:, 0:nn], in0=ms[:, 0:nn], scalar1=cb1[k][:, hf:hf + 1], scalar2=None,
                                                                            op0=ALU.add), r=[B_ms, B_cb1], w=[B_hidf])
                            yield
                            op("act", lambda e: e.activation(out=hide[:, 0:nn], in_=hidf[:, 0:nn], func=AF.Exp, scale=-1.0), r=[B_hidf], w=[B_hide])
                            yield
                            op("dve", lambda e: e.tensor_scalar_add(out=hide[:, 0:nn], in0=hide[:, 0:nn], scalar1=1.0), r=[B_hide], w=[B_hide])
                            op("dve", lambda e: e.reciprocal(out=hide[:, 0:nn], in_=hide[:, 0:nn]), r=[B_hide], w=[B_hide])
                            op("dve", lambda e, hf=hf: e.tensor_tensor(out=hid[:, hf, 0:nn], in0=hidf[:, 0:nn], in1=hide[:, 0:nn], op=ALU.mult),
                               r=[B_hidf, B_hide], w=[B_hid])
                            yield
                        for hf in range(2):
                            op("pe", lambda e, k=k, hf=hf: e.matmul(ms[0:nn, 0:64], lhsT=hid[:, hf, 0:nn], rhs=cw2[k][:, hf, :],
                                                                    start=(hf == 0), stop=(hf == 1)), r=[B_hid, B_cw], w=[B_ms])
                        yield
                        tn, r0 = nlo // 128, nlo % 128
                        if k == 1:
                            op("act", lambda e: e.activation(out=kcb[0:nn, 0:64], in_=ms[0:nn, 0:64], func=AF.Copy), r=[B_ms], w=[B_kcb])
                            n1 = min(nn, 128 - r0)
                            dma("sp", lambda e, g=g, tn=tn, r0=r0, n1=n1: e.dma_start(out=VC[r0:r0 + n1, tn, g, 0:64], in_=kcb[0:n1, 0:64]),
                                B_kcb, r=[B_kcb], w=[B_VC])
                            if n1 < nn:
                                dma("sp", lambda e, g=g, tn=tn, n1=n1: e.dma_start(out=VC[0:nn - n1, tn + 1, g, 0:64], in_=kcb[n1:nn, 0:64]),
                                    B_kcb, r=[B_kcb], w=[B_VC])
                        else:
                            op("act", lambda e: e.activation(out=kcf[0:nn, :], in_=ms[0:nn, 0:64], func=AF.Square, accum_out=kss[0:nn, 0:1]),
                               r=[B_ms], w=[B_kcf, B_kss])
                            yield
                            rstd_from_ss(kss[0:nn, 0:1], [B_kss], 1.0 / 64)
                            yield
                            op("dve", lambda e, g=g: e.scalar_tensor_tensor(out=kcb[0:nn, g * 64:(g + 1) * 64], in0=ms[0:nn, 0:64], scalar=kss[0:nn, 0:1],
                                                                            in1=kwr[0:nn, 0:64], op0=ALU.mult, op1=ALU.mult),
                               r=[B_ms, B_kss, B_kwr], w=[B_kcb])
                            if g == 1:
                                yield
                                op("pe", lambda e: e.transpose(out=tpc[:, 0:nn], in_=kcb[0:nn, :], identity=identb[0:nn, 0:nn]), r=[B_kcb, B_cst], w=[B_tpc])
                                yield
                                op("act", lambda e: e.activation(out=KC[:, nlo:nlo + nn], in_=tpc[:, 0:nn], func=AF.Copy), r=[B_tpc], w=[B_KC])
                        yield

            pending = []
            for J in range(NOWN):
                if J > 0:
                    for k in range(2):
                        for g in range(2):
                            op("dve", lambda e, k=k, g=g: e.tensor_copy(out=KSd[J % 2][k][g][:, 0:16], in_=KSd[(J - 1) % 2][k][g][:, 512:528]),
                               r=[B_KSd[(J - 1) % 2]], w=[B_KSd[J % 2]])
                run_interleaved(pending + [proj_block(J, jj) for jj in range(4)], 3 if pending else 2)
                pending = [compress_gen(J)]
            run_interleaved(pending, 1)

            sc.barrier()
            e1a.close()
            eA.close()
            e1b_ = ExitStack()
            e1 = e1b_
            relb = sb(e1, "relb", [33, 8])
            B_relb = Buf()
            op("dve", lambda e: e.memset(relb[32:33, :], 1.0), w=[B_relb])
            dma("sp", lambda e: e.dma_start(out=relb[0:32, :], in_=rel_bias), B_relb, w=[B_relb])
            hi9 = sb(e1, "hi9", [128, 9])
            lo9 = sb(e1, "lo9", [128, 9])
            anti48 = sb(e1, "anti48", [48, 48])
            B_c1 = Buf()
            for t_, s_ in ((hi9, c_hi), (lo9, c_lo), (anti48, c_anti48)):
                dma("sp", lambda e, t_=t_, s_=s_: e.dma_start(out=t_[:], in_=s_), B_c1, w=[B_c1])
            with ExitStack() as e1t:
                oht = sb(e1t, "oht", [33, 4224])
                ftab = sb(e1t, "ftab", [8, 4224])
                ftabb = sb(e1t, "ftabb", [8, 4224], BF16)
                B_oht, B_ftab, B_ftabb = Buf(), Buf(), Buf()
                for (src, L, dst, Dd, isb) in ((c_ohs, 768, fs_d, D_fs, True), (c_ohw, 1152, fw_d, D_fw, True),
                                               (c_ohq, 880, fq_d, D_fq, False), (c_ohk, 4224, fk_d, D_fk, True)):
                    dma("sp", lambda e, src=src, L=L: e.dma_start(out=oht[:, 0:L], in_=src), B_oht, w=[B_oht])
                    for c0 in range(0, L, 512):
                        n = min(512, L - c0)
                        op("pe", lambda e, c0=c0, n=n: e.matmul(ms[0:8, 0:n], lhsT=relb[:], rhs=oht[:, c0:c0 + n], start=True, stop=True),
                           r=[B_relb, B_oht], w=[B_ms])
                        op("act", lambda e, c0=c0, n=n: e.activation(out=ftab[:, c0:c0 + n], in_=ms[0:8, 0:n], func=AF.Copy),
                           r=[B_ms], w=[B_ftab])
                    if isb:
                        op("dve", lambda e, L=L: e.tensor_copy(out=ftabb[:, 0:L], in_=ftab[:, 0:L]), r=[B_ftab], w=[B_ftabb])
                        dma("sp", lambda e, L=L, dst=dst: e.dma_start(out=dst, in_=ftabb[:, 0:L]), B_ftabb, r=[B_ftabb], w=[Dd])
                    else:
                        dma("sp", lambda e, L=L, dst=dst: e.dma_start(out=dst, in_=ftab[:, 0:L]), B_ftab, r=[B_ftab], w=[Dd])
                sc.barrier()

            def toep(dram_ap, off, pstride, rowlen):
                return bass.AP(dram_ap.tensor, dram_ap.offset + off, [[pstride, 128], [rowlen, 8], [1, 128]])

            BTs = sb(e1, "BTs", [128, 5, 8, 128], BF16)
            BTw = sb(e1, "BTw", [128, 8, 8, 128], BF16)
            B_BT = Buf()
            for tr in range(-1, 4):
                dma("sp", lambda e, tr=tr: e.dma_start(out=BTs[:, tr + 1], in_=toep(fs_d, 128 * (3 - tr), 1, 768)), B_BT, r=[D_fs], w=[B_BT])
            for tr in range(-4, 4):
                dma("sp", lambda e, tr=tr: e.dma_start(out=BTw[:, tr + 4], in_=toep(fw_d, 128 * (3 - tr), 1, 1152)), B_BT, r=[D_fw], w=[B_BT])
            bk48 = sb(e1, "bk48", [48, 8, 128])
            Bq = sb(e1, "Bq", [128, 8, 48])
            B_bk, B_Bq = Buf(), Buf()
            dma("sp", lambda e: e.dma_start(out=bk48[:], in_=bass.AP(fq_d.tensor, fq_d.offset, [[16, 48], [880, 8], [1, 128]])),
                B_bk, r=[D_fq], w=[B_bk])
            for h in range(8):
                op("pe", lambda e, h=h: e.matmul(ms[:, h * 48:(h + 1) * 48], lhsT=bk48[:, h, :], rhs=anti48[:], start=True, stop=True),
                   r=[B_bk, B_c1], w=[B_ms])
            op("act", lambda e: e.activation(out=Bq[:].rearrange("p h c -> p (h c)"), in_=ms[:, 0:384], func=AF.Copy), r=[B_ms], w=[B_Bq])

            KW = sb(e1, "KW", [128, 8, 128], BF16)
            VW = sb(e1, "VW", [128, 8, 2, 65], BF16)
            B_KW = [Buf() for _ in range(8)]
            B_VW = [Buf() for _ in range(8)]
            win_loaded = set()
            kss = sb(e1, "kss2", [128, 2])
            QAs = [[[sb(e1, "QA%d%d%d" % (q, g, c), [128, 512], BF16) for c in range(NCH)] for g in range(2)] for q in range(2)]
            B_QAs = [[[Buf() for _ in range(NCH)] for _ in range(2)] for q in range(2)]
            prs2 = sb(e1, "prs2", [128, 2])
            stepper = [None]

            def step():
                g_ = stepper[0]
                if g_ is not None:
                    try:
                        next(g_)
                    except StopIteration:
                        stepper[0] = None
            PT = [sb(e1, "PT%d" % k, [128, 512], BF16) for k in range(3)]
            B_PT = [Buf(), Buf(), Buf()]
            STR = [st[0][:], st[1][:], pj[:, 512:1024]]
            B_STR = [B_st[0], B_st[1], B_pjB]
            BTc = sb(e1, "BTc", [128, 2, 8, 128], BF16)
            B_BTc = Buf()
            Ph = sb(e1, "Ph", [128, 1024])
            prs = sb(e1, "prs", [128, 1])
            acc0_ = sb(e1, "acc0", [128, 1032])
            acc = [acc0_, acc0_]
            imp = sb(e1, "imp", [128, 256])
            scv = sb(e1, "scv", [128, NSEL])
            scw = sb(e1, "scw", [128, NSEL])
            m8 = sb(e1, "m8", [128, 8])
            thr = sb(e1, "thr", [128, 1])
            mvb = sb(e1, "mvb", [128, NCH, 2, 64], BF16)
            oT = sb(e1, "oT", [65, 512])
            obr = sb(e1, "obr", [128, 3, 2, 4, 65])
            gt = sb(e1, "gt", [128, 24])
            rinv = sb(e1, "rinv", [128, 24])
            comb = sb(e1, "comb", [128, 512])
            csq = sb(e1, "csq", [128, 512])
            css = sb(e1, "css", [128, 8])
            mixb = sb(e1, "mixb", [128, 512], BF16)
            mixT = sb(e1, "mixT", [128, 512], BF16)
            B_Ph, B_prs, B_imp, B_scv, B_scw, B_m8, B_thr, B_mvb = Buf(), Buf(), Buf(), Buf(), Buf(), Buf(), Buf(), Buf()
            B_acc0_ = Buf()
            B_acc = [B_acc0_, B_acc0_]
            B_oT, B_obr, B_gt, B_rinv, B_comb, B_csq, B_css, B_mixb, B_mixT = (Buf() for _ in range(9))
            op("pool", lambda e: e.memset(acc0_[:], 0.0), w=[B_acc0_])
            op("pool", lambda e: e.memset(mvb[:], NEG), w=[B_mvb])

            st_i = [0]
            pt_i = [0]
            oa_i = [0]

            def attend(branch, g, tiles, qrows):
                oi = oa_i[0] % 2
                oa_i[0] += 1
                n = len(tiles)
                pend = []
                npv = [0]

                def pv(item, first, last):
                    first = (npv[0] == 0)
                    npv[0] += 1
                    pti, V_ap, vbufs = item
                    op("pe", lambda e: e.matmul(oa[oi][0:65, :], lhsT=V_ap, rhs=PT[pti][:], start=first, stop=last),
                       r=vbufs + [B_PT[pti]], w=[B_oa[oi]])

                for idx, (lhsT_ap, kbufs, rhs_ap, qbufs, bias_rhs, V_ap, vbufs) in enumerate(tiles):
                    si = st_i[0] % 3
                    st_i[0] += 1
                    pti = pt_i[0] % 3
                    pt_i[0] += 1
                    op("pe", lambda e: e.matmul(STR[si], lhsT=lhsT_ap, rhs=rhs_ap, start=True, stop=(bias_rhs is None)),
                       r=kbufs + qbufs, w=[B_STR[si]])
                    if bias_rhs is not None:
                        b_ap, b_bufs = bias_rhs
                        op("pe", lambda e: e.matmul(STR[si], lhsT=antib[:], rhs=b_ap, start=False, stop=True),
                           r=[B_cst] + b_bufs, w=[B_STR[si]])
                    op("act", lambda e: e.activation(out=PT[pti][:], in_=STR[si], func=AF.Exp), r=[B_STR[si]], w=[B_PT[pti]])
                    pend.append((pti, V_ap, vbufs))
                    if len(pend) > 2:
                        pv(pend.pop(0), False, False)
                    if idx % 3 == 2:
                        step()
                while len(pend) > 1:
                    pv(pend.pop(0), False, False)
                pv(pend.pop(0), False, True)
                op("act", lambda e: e.activation(out=oT[:], in_=oa[oi][0:65, :], func=AF.Copy), r=[B_oa[oi]], w=[B_oT])
                for h in range(4):
                    op("pe", lambda e, h=h: e.transpose(out=ms[:, h * 65:(h + 1) * 65], in_=oT[:, h * 128:(h + 1) * 128],
                                                        identity=identf[0:65, 0:65]), r=[B_oT, B_cst], w=[B_ms])
                op("act", lambda e: e.activation(out=obr[:, branch, g].rearrange("p h d -> p (h d)"), in_=ms[:, 0:260], func=AF.Copy),
                   r=[B_ms], w=[B_obr])

            def prep_gen(i):
                nch = (8 * i + 7) // 64 + 1
                QA, B_QA = QAs[i % 2], B_QAs[i % 2]
                for c in range(nch):
                    dma("sp", lambda e, c=c: e.dma_start(out=QA[0][c][0:64, :], in_=qT_d[i, 0:64, :]), B_QA[0][c], r=[D_qT], w=[B_QA[0][c]])
                    dma("sp", lambda e, c=c: e.dma_start(out=QA[1][c][64:128, :], in_=qT_d[i, 64:128, :]), B_QA[1][c], r=[D_qT], w=[B_QA[1][c]])
                ncol = min(32 * i + 32, NCMP)
                for g in range(2):
                    qrow = slice(0, 64) if g == 0 else slice(64, 128)
                    for a in range(4):
                        h = g * 4 + a
                        blo = max(32 * i - 16, 0)
                        bhi = min(32 * i + 32, ncol)
                        nchk = 0
                        for c0 in range(0, ncol, 512):
                            n = min(512, ncol - c0)
                            op("pe", lambda e, c0=c0, n=n, a=a: e.matmul(pj[:, 0:n], lhsT=QA[g][0][qrow, a * 128:(a + 1) * 128],
                                                                        rhs=KC[qrow, c0:c0 + n], start=True, stop=True),
                               r=[B_QA[g][0], B_KC], w=[B_pjA])
                            lo_, hi_ = max(blo, c0), min(bhi, c0 + n)
                            if lo_ < hi_:
                                op("dve", lambda e, h=h, lo_=lo_, hi_=hi_, c0=c0: e.tensor_tensor(out=pj[:, lo_ - c0:hi_ - c0], in0=pj[:, lo_ - c0:hi_ - c0],
                                                                                              in1=Bq[:, h, lo_ - (32 * i - 16):hi_ - (32 * i - 16)], op=ALU.add),
                                   r=[B_Bq, B_pjA], w=[B_pjA])
                            op("act", lambda e, c0=c0, n=n, nchk=nchk: e.activation(out=Ph[:, c0:c0 + n], in_=pj[:, 0:n], func=AF.Exp,
                                                                                  accum_out=prs2[:, nchk:nchk + 1]),
                               r=[B_pjA], w=[B_Ph, B_prs])
                            nchk += 1
                            yield
                        if nchk == 2:
                            op("dve", lambda e: e.tensor_tensor(out=prs[:], in0=prs2[:, 0:1], in1=prs2[:, 1:2], op=ALU.add), r=[B_prs], w=[B_prs])
                        else:
                            op("dve", lambda e: e.tensor_copy(out=prs[:], in_=prs2[:, 0:1]), r=[B_prs], w=[B_prs])
                        op("dve", lambda e: e.tensor_scalar_max(out=prs[:], in0=prs[:], scalar1=1e-30), r=[B_prs], w=[B_prs])
                        op("dve", lambda e: e.reciprocal(out=prs[:], in_=prs[:]), r=[B_prs], w=[B_prs])
                        if a == 0:
                            op("dve", lambda e: e.tensor_scalar(out=acc[g][:, 4:4 + ncol], in0=Ph[:, 0:ncol], scalar1=prs[:, 0:1], scalar2=None,
                                                                op0=ALU.mult), r=[B_Ph, B_prs], w=[B_acc[g]])
                        else:
                            op("dve", lambda e: e.scalar_tensor_tensor(out=acc[g][:, 4:4 + ncol], in0=Ph[:, 0:ncol], scalar=prs[:, 0:1],
                                                                       in1=acc[g][:, 4:4 + ncol], op0=ALU.mult, op1=ALU.add),
                               r=[B_Ph, B_prs, B_acc[g]], w=[B_acc[g]])
                        yield
                    nm = 8 * i + 8
                    op("dve", lambda e: e.tensor_reduce(out=imp[:, 0:nm], in_=acc[g][:, 4:4 + 4 * nm].rearrange("p (m f) -> p m f", f=4),
                                                        axis=AX.X, op=ALU.add), r=[B_acc[g]], w=[B_imp])
                    op("dve", lambda e: e.tensor_tensor(out=imp[:, 0:nm], in0=imp[:, 0:nm],
                                                        in1=acc[g][:, 0:4 * nm].rearrange("p (m f) -> p m f", f=4)[:, :, 3], op=ALU.add),
                       r=[B_imp, B_acc[g]], w=[B_imp])
                    op("pool", lambda e: e.memset(scv[:], -1.0), w=[B_scv])
                    mlo = max(8 * i - 1, 0)
                    if mlo > 0:
                        op("dve", lambda e: e.tensor_copy(out=scv[:, 0:mlo], in_=imp[:, 0:mlo]), r=[B_imp], w=[B_scv])
                    cl = mlo - (8 * i - 1)
                    op("dve", lambda e: e.tensor_tensor(out=scv[:, mlo:nm], in0=imp[:, mlo:nm], in1=hi9[:, cl:9], op=ALU.min), r=[B_imp, B_c1], w=[B_scv])
                    op("dve", lambda e: e.tensor_tensor(out=scv[:, mlo:nm], in0=scv[:, mlo:nm], in1=lo9[:, cl:9], op=ALU.max), r=[B_c1, B_scv], w=[B_scv])
                    op("dve", lambda e: e.memset(scv[:, 0:1], 1e4), w=[B_scv])
                    yield
                    op("dve", lambda e: e.max(out=m8[:], in_=scv[:]), r=[B_scv], w=[B_m8])
                    op("dve", lambda e: e.match_replace(out=scw[:], in_to_replace=m8[:], in_values=scv[:], imm_value=-2.0), r=[B_scv, B_m8], w=[B_scw])
                    op("dve", lambda e: e.max(out=m8[:], in_=scw[:]), r=[B_scw], w=[B_m8])
                    op("dve", lambda e: e.tensor_scalar_max(out=thr[:], in0=m8[:, 7:8], scalar1=0.0), r=[B_m8], w=[B_thr])
                    op("dve", lambda e: e.tensor_scalar(out=scw[:], in0=scv[:], scalar1=thr[:, 0:1], scalar2=-NEG, op0=ALU.is_ge, op1=ALU.mult),
                       r=[B_scv, B_thr], w=[B_scw])
                    ncb = nch * 64
                    nv = min(ncb, NSEL)
                    if nv < 64:
                        op("dve", lambda e, g=g, nv=nv: e.tensor_scalar_add(out=mvb[:, 0, 1 - g, 0:nv], in0=scw[:, 0:nv], scalar1=NEG), r=[B_scw], w=[B_mvb])
                    else:
                        op("dve", lambda e, g=g, nv=nv: e.tensor_scalar_add(out=mvb[:, 0:nv // 64, 1 - g, :], in0=scw[:, 0:nv].rearrange("p (c m) -> p c m", m=64),
                                                                            scalar1=NEG), r=[B_scw], w=[B_mvb])
                    yield
                for c in range(nch):
                    op("pe", lambda e, c=c: e.transpose(out=tpb[:, c * 128:(c + 1) * 128], in_=mvb[:, c].rearrange("p g m -> p (g m)"),
                                                        identity=identb[:]), r=[B_mvb, B_cst], w=[B_tpb])
                for c in range(nch):
                    op("act", lambda e, c=c: e.activation(out=QA[1][c][0:64, :].rearrange("p (h q) -> p h q", h=4),
                                                          in_=tpb[0:64, c * 128:(c + 1) * 128].unsqueeze(1).to_broadcast([64, 4, 128]), func=AF.Copy),
                       r=[B_tpb], w=[B_QA[1][c]])
                    op("act", lambda e, c=c: e.activation(out=QA[0][c][64:128, :].rearrange("p (h q) -> p h q", h=4),
                                                          in_=tpb[64:128, c * 128:(c + 1) * 128].unsqueeze(1).to_broadcast([64, 4, 128]), func=AF.Copy),
                       r=[B_tpb], w=[B_QA[0][c]])

            stepper[0] = prep_gen(0)
            while stepper[0] is not None:
                step()
            for J in range(NOWN):
                i = J
                for t in range(max(4 * i - 4, 0), 4 * i + 4):
                    if t in win_loaded:
                        continue
                    win_loaded.add(t)
                    dma("sp", lambda e, t=t: e.dma_start(out=KW[:, t % 8, :], in_=kw_d[t]), B_KW[t % 8], r=[D_kw[t]], w=[B_KW[t % 8]])
                    dma("sp", lambda e, t=t: e.dma_start(out=VW[:, t % 8].rearrange("p g d -> p (g d)"), in_=vw_d[t]), B_VW[t % 8],
                        r=[D_vw[t]], w=[B_VW[t % 8]])
                QA, B_QA = QAs[i % 2], B_QAs[i % 2]
                dma("sp", lambda e: e.dma_start(out=gt[:], in_=gates_d[i]), B_gt, r=[D_gates], w=[B_gt])
                stepper[0] = prep_gen(i + 1) if i + 1 < NOWN else None
                i4 = i % 4
                tnl = i // 4
                dma("sp", lambda e: e.dma_start(out=BTc[:, 0], in_=toep(fk_d, 512 * i4, 16, 4224)), B_BTc, r=[D_fk], w=[B_BTc])
                if i4 == 0 and tnl >= 1:
                    dma("sp", lambda e: e.dma_start(out=BTc[:, 1], in_=toep(fk_d, 2048, 16, 4224)), B_BTc, r=[D_fk], w=[B_BTc])
                for g in range(2):
                    qrow = slice(0, 64) if g == 0 else slice(64, 128)
                    hs = slice(g * 4, g * 4 + 4)
                    tiles = []
                    for tn in range(tnl + 1):
                        bias = None
                        if tn == tnl:
                            bias = (BTc[:, 0, hs].rearrange("p h q -> p (h q)"), [B_BTc])
                        elif tn == tnl - 1 and i4 == 0:
                            bias = (BTc[:, 1, hs].rearrange("p h q -> p (h q)"), [B_BTc])
                        tiles.append((KC[qrow, tn * 128:(tn + 1) * 128], [B_KC], QA[g][0][qrow, :], [B_QA[g][0]], bias, VC[:, tn, g, :], [B_VC]))
                    attend(0, g, tiles, qrow)
                    tiles = []
                    for t in range(4 * i + 4):
                        tr = t - 4 * i
                        bias = None
                        if tr >= -1:
                            bias = (BTs[:, tr + 1, hs].rearrange("p h q -> p (h q)"), [B_BT])
                        c = t // 32
                        tiles.append((KA[g][:, t * 128:(t + 1) * 128], [B_KA[g][t], B_init], QA[g][c][:], [B_QA[g][c]], bias, VS[:, t, g, :], [B_VS[t]]))
                    attend(1, g, tiles, None)
                    tiles = []
                    for tr in range(-4, 4):
                        t = 4 * i + tr
                        if t < 0:
                            continue
                        bias = (BTw[:, tr + 4, hs].rearrange("p h q -> p (h q)"), [B_BT])
                        tiles.append((KW[qrow, t % 8, :], [B_KW[t % 8]], QA[g][0][qrow, :], [B_QA[g][0]], bias, VW[:, t % 8, g, :], [B_VW[t % 8]]))
                    attend(2, g, tiles, qrow)
                while stepper[0] is not None:
                    step()
                op("dve", lambda e: e.tensor_scalar_max(out=rinv[:].rearrange("p (h b) -> p b h", b=3),
                                                        in0=obr[:, :, :, :, 64].rearrange("p b g a -> p b (g a)"), scalar1=1e-30), r=[B_obr], w=[B_rinv])
                op("dve", lambda e: e.reciprocal(out=rinv[:], in_=rinv[:]), r=[B_rinv], w=[B_rinv])
                op("dve", lambda e: e.tensor_tensor(out=rinv[:], in0=rinv[:], in1=gt[:], op=ALU.mult), r=[B_rinv, B_gt], w=[B_rinv])
                for br in range(3):
                    src = obr[:, br, :, :, 0:64].rearrange("p g a d -> p (g a) d")
                    wv = rinv[:].rearrange("p (h b) -> p h b", b=3)[:, :, br].unsqueeze(2).to_broadcast([128, 8, 64])
                    if br == 0:
                        op("dve", lambda e, src=src, wv=wv: e.tensor_tensor(out=comb[:].rearrange("p (h d) -> p h d", d=64), in0=src, in1=wv, op=ALU.mult),
                           r=[B_obr, B_rinv], w=[B_comb])
                    else:
                        op("dve", lambda e, src=src, wv=wv: e.tensor_tensor(out=csq[:].rearrange("p (h d) -> p h d", d=64), in0=src, in1=wv, op=ALU.mult),
                           r=[B_obr, B_rinv], w=[B_csq])
                        op("pool", lambda e: e.tensor_tensor(out=comb[:], in0=comb[:], in1=csq[:], op=ALU.add), r=[B_csq, B_comb], w=[B_comb])
                op("pool", lambda e: e.tensor_tensor(out=csq[:], in0=comb[:], in1=comb[:], op=ALU.mult), r=[B_comb], w=[B_csq])
                op("dve", lambda e: e.tensor_reduce(out=css[:], in_=csq[:].rearrange("p (h d) -> p h d", d=64), axis=AX.X, op=ALU.add), r=[B_csq], w=[B_css])
                rstd_from_ss(css[:], [B_css], 1.0 / 64)
                op("dve", lambda e: e.tensor_tensor(out=csq[:].rearrange("p (h d) -> p h d", d=64), in0=comb[:].rearrange("p (h d) -> p h d", d=64),
                                                    in1=css[:].unsqueeze(2).to_broadcast([128, 8, 64]), op=ALU.mult), r=[B_comb, B_css], w=[B_csq])
                op("dve", lambda e: e.tensor_tensor(out=mixb[:], in0=csq[:], in1=onar[:], op=ALU.mult), r=[B_csq, B_kwr], w=[B_mixb])
                for a in range(4):
                    op("pe", lambda e, a=a: e.transpose(out=tpb[:, a * 128:(a + 1) * 128], in_=mixb[:, a * 128:(a + 1) * 128], identity=identb[:]),
                       r=[B_mixb, B_cst], w=[B_tpb])
                op("act", lambda e: e.activation(out=mixT[:], in_=tpb[:, 0:512], func=AF.Copy), r=[B_tpb], w=[B_mixT])
                dma("sp", lambda e: e.dma_start(out=mixa_d[i], in_=mixT[:]), B_mixT, r=[B_mixT], w=[D_mixa])
            sc.barrier()
            e1b_.close()
        eKV.close()

        widx = sb(es, "widx", [128, NBLK], I32)
        B_widx = Buf()
        dest = sb(es, "dest", [128, NOWN, 2], I32)
        wts = sb(es, "wts", [128, NOWN, 2])
        B_dest, B_wts = Buf(), Buf()
        with ExitStack() as e2:
            G1 = load_mod(e2, "G1", 2)
            SH2 = load_mod(e2, "SH2", 3)
            A2 = load_mod(e2, "A2", 4)
            wo = sb(e2, "wo", [128, 8, 1024], BF16)
            wr = sb(e2, "wr", [128, 8, 72], BF16)
            B_wo = Buf()
            w_out_v = w_out.rearrange("(kt p) n -> p kt n", p=128)
            w_rt_v = w_rt.rearrange("(kt p) n -> p kt n", p=128)
            for kt in range(8):
                dma("pool", lambda e, kt=kt: e.dma_start(out=wo[:, kt, :], in_=w_out_v[:, kt, :]), B_wo, w=[B_wo])
                dma("pool", lambda e, kt=kt: e.dma_start(out=wr[:, kt, :], in_=w_rt_v[:, kt, :]), B_wo, w=[B_wo])
            triu = sb(e2, "triu", [128, 128], BF16)
            onesb = sb(e2, "onesb", [128, 128], BF16)
            OH = sb(e2, "OH", [128, NOWN * 2, 64])
            rk = sb(e2, "rk", [128, NOWN * 2])
            h2_d = dscr("h2_d", [NOWN, 128, 1024], BF16)
            D_h2 = Buf()
            B_OH = Buf()
            dma("pool", lambda e: e.dma_start(out=triu[:], in_=c_triu), B_c2, w=[B_c2])
            op("dve", lambda e: e.memset(onesb[:], 1.0), w=[B_c2])
            run = sb(e2, "run", [128, 64])
            B_run = Buf()
            op("dve", lambda e: e.memset(run[:], 0.0), w=[B_run])
            OHS = sb(e2, "OHS", [128, NOWN, 64], BF16)
            def make_a2(sl):
                mT = sb(e2, "mT_%d" % sl, [128, 8, 128], BF16)
                xt = sb(e2, "xt2_%d" % sl, [128, 1024])
                x1 = sb(e2, "x1_%d" % sl, [128, 1024])
                scr = sb(e2, "scr2_%d" % sl, [128, 1024])
                ss = sb(e2, "ss2_%d" % sl, [128, 1])
                h2b = sb(e2, "h2b_%d" % sl, [128, 1024], BF16)
                h2T = sb(e2, "h2T_%d" % sl, [128, 1024], BF16)
                lg = sb(e2, "lg_%d" % sl, [128, 72])
                gm = sb(e2, "gm_%d" % sl, [128, 8])
                ohg = sb(e2, "ohg_%d" % sl, [128, 8])
                eg = sb(e2, "eg_%d" % sl, [128, 8])
                gs = sb(e2, "gs_%d" % sl, [128, 1])
                esel = sb(e2, "esel_%d" % sl, [128, 64])
                ein = sb(e2, "ein_%d" % sl, [128, 8])
                em8 = sb(e2, "em8_%d" % sl, [128, 8])
                ohe = sb(e2, "ohe_%d" % sl, [128, 2, 8])
                oh64 = sb(e2, "oh64_%d" % sl, [128, 2, 64])
                ohs = sb(e2, "ohs_%d" % sl, [128, 64], BF16)
                slot = sb(e2, "slot_%d" % sl, [128, 64])
                dsf = sb(e2, "dsf_%d" % sl, [128, 2])
                wk = sb(e2, "wk_%d" % sl, [128, 2])
                tmp64 = sb(e2, "tmp64_%d" % sl, [128, 64])
                B_mT, B_xt2, B_x1, B_scr2, B_h2b, B_h2T, B_lg = (Buf() for _ in range(7))
                B_rt = Buf()
                TP, B_TP = (tpb[:], B_tpb) if sl == 0 else (st[0][:].bitcast(BF16), B_st[0])
                PJ = (pj[:, 0:512], pj[:, 512:1024]) if sl == 0 else (oa[0][:], oa[1][:])
                B_PJ = (B_pjA, B_pjB) if sl == 0 else (B_oa[0], B_oa[1])
                MS, B_MS = (ms[:], B_ms) if sl == 0 else (st[1][:], B_st[1])

                def body(i):
                    dma("sp", lambda e: e.dma_start(out=mT[:, 0:4, :].rearrange("p c t -> p (c t)"), in_=mixa_d[i]), B_mT, r=[D_mixa], w=[B_mT])
                    dma("sp", lambda e: e.dma_start(out=mT[:, 4:8, :].rearrange("p c t -> p (c t)"), in_=mixc_d[i]), B_mT, r=[D_mixc], w=[B_mT])
                    dma("sp", lambda e: e.dma_start(out=xt[:], in_=x_own[i * 128:(i + 1) * 128, :]), B_xt2, w=[B_xt2])
                    for hf, Bp in ((0, B_PJ[0]), (1, B_PJ[1])):
                        for kt in range(8):
                            op("pe", lambda e, kt=kt, hf=hf: e.matmul(PJ[hf], lhsT=mT[:, kt, :], rhs=wo[:, kt, hf * 512:(hf + 1) * 512],
                                                                      start=(kt == 0), stop=(kt == 7)), r=[B_mT, B_wo], w=[Bp])
                        op("dve", lambda e, hf=hf: e.tensor_tensor(out=x1[:, hf * 512:(hf + 1) * 512], in0=PJ[hf],
                                                                   in1=G1[:, hf * 512:(hf + 1) * 512], op=ALU.mult), r=[Bp, B_mods], w=[B_x1])
                    op("pool", lambda e: e.tensor_tensor(out=x1[:], in0=x1[:], in1=xt[:], op=ALU.add), r=[B_x1, B_xt2], w=[B_x1])
                    dma("sp", lambda e: e.dma_start(out=x1_d[i * 128:(i + 1) * 128, :], in_=x1[:]), B_x1, r=[B_x1], w=[D_x1])
                    rms_mod(x1[:], h2b[:], A2, SH2, 128, [B_x1], [B_h2b], scr, ss, B_scr2)
                    for kt in range(8):
                        op("pe", lambda e, kt=kt: e.transpose(out=TP[:, kt * 128:(kt + 1) * 128], in_=h2b[:, kt * 128:(kt + 1) * 128], identity=identb[:]),
                           r=[B_h2b, B_cst], w=[B_TP])
                    op("act", lambda e: e.activation(out=h2T[:], in_=TP, func=AF.Copy), r=[B_TP], w=[B_h2T])
                    for kt in range(8):
                        op("pe", lambda e, kt=kt: e.matmul(MS[:, 0:72], lhsT=h2T[:, kt * 128:(kt + 1) * 128], rhs=wr[:, kt, :], start=(kt == 0), stop=(kt == 7)),
                           r=[B_h2T, B_wo], w=[B_MS])
                    R = [B_rt]
                    op("dve", lambda e: e.tensor_tensor(out=lg[:], in0=MS[:, 0:72], in1=brt[:], op=ALU.add), r=[B_MS, B_c2], w=R)
                    op("dve", lambda e: e.max(out=gm[:], in_=lg[:, 0:8]), r=R, w=R)
                    op("dve", lambda e: e.tensor_scalar(out=ohg[:], in0=lg[:, 0:8], scalar1=gm[:, 0:1], scalar2=None, op0=ALU.is_ge), r=R, w=R)
                    op("dve", lambda e: e.tensor_scalar(out=eg[:], in0=lg[:, 0:8], scalar1=gm[:, 0:1], scalar2=None, op0=ALU.subtract), r=R, w=R)
                    op("act", lambda e: e.activation(out=eg[:], in_=eg[:], func=AF.Exp, accum_out=gs[:]), r=R, w=R)
                    op("dve", lambda e: e.reciprocal(out=gs[:], in_=gs[:]), r=R, w=R)
                    op("dve", lambda e: e.tensor_tensor(out=esel[:].rearrange("p (g x) -> p g x", x=8), in0=lg[:, 8:72].rearrange("p (g x) -> p g x", x=8),
                                                        in1=ohg[:].unsqueeze(2).to_broadcast([128, 8, 8]), op=ALU.mult), r=R, w=R)
                    op("dve", lambda e: e.tensor_reduce(out=ein[:], in_=esel[:].rearrange("p (g x) -> p x g", x=8), axis=AX.X, op=ALU.add), r=R, w=R)
                    op("dve", lambda e: e.max(out=em8[:], in_=ein[:]), r=R, w=R)
                    for k in range(2):
                        op("dve", lambda e, k=k: e.tensor_scalar(out=ohe[:, k, :], in0=ein[:], scalar1=em8[:, k:k + 1], scalar2=None, op0=ALU.is_equal), r=R, w=R)
                    op("dve", lambda e: e.tensor_tensor(out=wk[:, 0:1], in0=em8[:, 1:2], in1=em8[:, 0:1], op=ALU.subtract), r=R, w=R)
                    op("act", lambda e: e.activation(out=wk[:, 0:1], in_=wk[:, 0:1], func=AF.Exp), r=R, w=R)
                    op("dve", lambda e: e.tensor_scalar_add(out=wk[:, 0:1], in0=wk[:, 0:1], scalar1=1.0), r=R, w=R)
                    op("dve", lambda e: e.reciprocal(out=wk[:, 0:1], in_=wk[:, 0:1]), r=R, w=R)
                    op("dve", lambda e: e.tensor_scalar(out=wk[:, 1:2], in0=wk[:, 0:1], scalar1=-1.0, scalar2=1.0, op0=ALU.mult, op1=ALU.add), r=R, w=R)
                    op("dve", lambda e: e.tensor_scalar(out=wts[:, i, :], in0=wk[:], scalar1=gs[:, 0:1], scalar2=None, op0=ALU.mult), r=R, w=R + [B_wts])
                    for k in range(2):
                        op("dve", lambda e, k=k: e.tensor_tensor(out=oh64[:, k, :].rearrange("p (g x) -> p g x", x=8),
                                                                 in0=ohg[:].unsqueeze(2).to_broadcast([128, 8, 8]),
                                                                 in1=ohe[:, k, :].unsqueeze(1).to_broadcast([128, 8, 8]), op=ALU.mult), r=R, w=R)
                    op("dve", lambda e: e.tensor_tensor(out=OHS[:, i, :], in0=oh64[:, 0, :], in1=oh64[:, 1, :], op=ALU.add), r=R, w=R + [B_OH])
                    for k in range(2):
                        op("pool", lambda e, k=k: e.tensor_copy(out=OH[:, 2 * i + k, :], in_=oh64[:, k, :]), r=R, w=[B_OH])
                    dma("sp", lambda e: e.dma_start(out=h2_d[i], in_=h2b[:]), B_h2b, r=[B_h2b], w=[D_h2])
                return body

            weave_slots(make_a2, 2, NOWN)
            slot = sb(e2, "slot_p", [128, 64])
            tmp64 = sb(e2, "tmp64_p", [128, 64])
            R = [Buf()]
            for i in range(NOWN):
                op("pe", lambda e: e.matmul(ms[:, 128:192], lhsT=triu[:], rhs=OHS[:, i, :], start=True, stop=True), r=[B_OH, B_c2], w=[B_ms])
                op("dve", lambda e: e.tensor_tensor(out=slot[:], in0=ms[:, 128:192], in1=run[:], op=ALU.add), r=[B_ms, B_run], w=R)
                op("pe", lambda e: e.matmul(ms[:, 192:256], lhsT=onesb[:], rhs=OHS[:, i, :], start=True, stop=True), r=[B_OH, B_c2], w=[B_ms])
                op("dve", lambda e: e.tensor_tensor(out=run[:], in0=run[:], in1=ms[:, 192:256], op=ALU.add), r=[B_ms, B_run], w=[B_run])
                for k in range(2):
                    op("dve", lambda e, k=k: e.tensor_tensor(out=tmp64[:], in0=slot[:], in1=OH[:, 2 * i + k, :], op=ALU.mult), r=R + [B_OH], w=R)
                    op("dve", lambda e, k=k: e.tensor_reduce(out=rk[:, 2 * i + k:2 * i + k + 1], in_=tmp64[:], axis=AX.X, op=ALU.add), r=R, w=R + [B_OH])
            cnt = run
            pe_a = sb(e2, "pe_a", [128, 64])
            pe_b = sb(e2, "pe_b", [128, 64])
            padd = sb(e2, "padd", [128, 64])
            pst = sb(e2, "pst", [128, 64])
            R2 = [Buf()]
            blkc = sb(e2, "blkc", [128, NBLK])
            pidx = sb(e2, "pidx", [128, 1])
            dma("sp", lambda e: e.dma_start(out=blkc[:], in_=c_blk), B_c2, w=[B_c2])
            dma("sp", lambda e: e.dma_start(out=pidx[:], in_=c_pidx), B_c2, w=[B_c2])
            cmp3 = sb(e2, "cmp3", [128, NBLK, 64])
            cmpk = cmp3[:].rearrange("p b e -> p (b e)")[:, 0:64 * NOWN].rearrange("p (e k) -> p e k", k=NOWN)
            op("dve", lambda e: e.tensor_tensor(out=cmpk, in0=cnt[:].unsqueeze(2).to_broadcast([128, 64, NOWN]),
                                                in1=blkc[:, 0:NOWN].unsqueeze(1).to_broadcast([128, 64, NOWN]), op=ALU.is_gt), r=[B_run, B_c2], w=R2)
            op("dve", lambda e: e.tensor_reduce(out=padd[:], in_=cmpk, axis=AX.X, op=ALU.add), r=R2, w=R2)
            op("dve", lambda e: e.tensor_scalar_mul(out=padd[:], in0=padd[:], scalar1=128.0), r=R2, w=R2)
            op("dve", lambda e: e.tensor_copy(out=pe_a[:], in_=padd[:]), r=R2, w=R2)
            cur, oth = pe_a, pe_b
            for sft in (1, 2, 4, 8, 16, 32):
                op("dve", lambda e, cur=cur, oth=oth, sft=sft: e.tensor_copy(out=oth[:, 0:sft], in_=cur[:, 0:sft]), r=R2, w=R2)
                op("dve", lambda e, cur=cur, oth=oth, sft=sft: e.tensor_tensor(out=oth[:, sft:64], in0=cur[:, sft:64], in1=cur[:, 0:64 - sft], op=ALU.add),
                   r=R2, w=R2)
                cur, oth = oth, cur
            pend_ = cur
            op("dve", lambda e: e.tensor_tensor(out=pst[:], in0=pend_[:], in1=padd[:], op=ALU.subtract), r=R2, w=R2)
            ber = sb(e2, "ber", [128, NBLK])
            bpv = sb(e2, "bpv", [128, NBLK])
            idxf = sb(e2, "idxf", [128, NBLK])
            op("dve", lambda e: e.tensor_tensor(out=cmp3[:], in0=pend_[:].unsqueeze(1).to_broadcast([128, NBLK, 64]),
                                                in1=blkc[:].unsqueeze(2).to_broadcast([128, NBLK, 64]), op=ALU.is_le), r=R2 + [B_c2], w=R2)
            op("dve", lambda e: e.tensor_reduce(out=ber[:], in_=cmp3[:], axis=AX.X, op=ALU.add), r=R2, w=R2)
            op("dve", lambda e: e.tensor_scalar_min(out=ber[:], in0=ber[:], scalar1=63.0), r=R2, w=R2)
            op("dve", lambda e: e.memset(bpv[:, 0:2], -1.0), r=R2, w=R2)
            op("dve", lambda e: e.tensor_copy(out=bpv[:, 2:NBLK], in_=ber[:, 0:NBLK - 2]), r=R2, w=R2)
            op("dve", lambda e: e.tensor_tensor(out=bpv[:], in0=bpv[:], in1=ber[:], op=ALU.is_equal), r=R2, w=R2)
            op("dve", lambda e: e.tensor_scalar(out=idxf[:], in0=ber[:], scalar1=128.0, scalar2=pidx[:, 0:1], op0=ALU.mult, op1=ALU.add), r=R2 + [B_c2], w=R2)
            op("dve", lambda e: e.scalar_tensor_tensor(out=idxf[:], in0=bpv[:], scalar=1.0e6, in1=idxf[:], op0=ALU.mult, op1=ALU.add), r=R2, w=R2)
            op("dve", lambda e: e.tensor_copy(out=widx[:], in_=idxf[:]), r=R2, w=[B_widx])
            ohp = cmp3[:, 0:NOWN * 2, :] if NBLK >= NOWN * 2 else None
            op("dve", lambda e: e.tensor_tensor(out=ohp, in0=OH[:], in1=pst[:].unsqueeze(1).to_broadcast([128, NOWN * 2, 64]), op=ALU.mult),
               r=R2 + [B_OH], w=R2)
            op("dve", lambda e: e.tensor_reduce(out=idxf[:, 0:NOWN * 2], in_=ohp, axis=AX.X, op=ALU.add), r=R2, w=R2)
            op("dve", lambda e: e.tensor_tensor(out=idxf[:, 0:NOWN * 2], in0=idxf[:, 0:NOWN * 2], in1=rk[:], op=ALU.add), r=R2 + [B_OH], w=R2)
            op("dve", lambda e: e.tensor_copy(out=dest[:].rearrange("p i k -> p (i k)"), in_=idxf[:, 0:NOWN * 2]), r=R2, w=[B_dest])
            h2sc = [sb(e2, "h2sc%d" % q, [128, 1024], BF16) for q in range(2)]
            B_h2sc = [Buf(), Buf()]
            for i in range(NOWN):
                q = i % 2
                dma("sp", lambda e: e.dma_start(out=h2sc[q][:], in_=h2_d[i]), B_h2sc[q], r=[D_h2], w=[B_h2sc[q]])
                for k in range(2):
                    dma("pool", lambda e, k=k: e.indirect_dma_start(out=xdisp_d[:, :], out_offset=bass.IndirectOffsetOnAxis(ap=dest[:, i, k:k + 1], axis=0),
                                                                    in_=h2sc[q][:], in_offset=None), B_h2sc[q], r=[B_h2sc[q], B_dest], w=[D_xd])
            sc.barrier()

        with ExitStack() as e3:
            Wf = [[sb(e3, "Wf%d_%d" % (m, q), [128, 4096]) for m in range(3)] for q in range(2)]
            B_Wf = [[Buf(), Buf(), Buf()] for q in range(2)]
            Wb = [[sb(e3, "Wb%d_%d" % (m, k), [128, 4096], BF16) for m in range(3)] for k in range(2)]
            B_Wb = [[Buf(), Buf(), Buf()] for _ in range(2)]
            wsrc = (w1, w3, w2)
            bnd_reg = nc.gpsimd.alloc_register('bnd')
            nc.gpsimd.reg_mov(bnd_reg, 64 * 128 - 1)
            cast_eng = ("act", "act", "dve")
            TPm = [tpb[:], ms[:].bitcast(BF16)]
            B_TPm = [B_tpb, B_ms]
            H1p = [st[0][:], oa[0][:]]
            B_H1p = [B_st[0], B_oa[0]]
            H3p = [st[1][:], oa[1][:]]
            B_H3p = [B_st[1], B_oa[1]]

            def make_moe(sl):
                xe = sb(e3, "xe%d" % sl, [128, 1024], BF16)
                xeT = sb(e3, "xeT%d" % sl, [128, 8, 128], BF16)
                hs = sb(e3, "hs%d" % sl, [128, 512])
                h1e = sb(e3, "h1e%d" % sl, [128, 512])
                actb = sb(e3, "actb%d" % sl, [128, 512], BF16)
                aT = sb(e3, "aT%d" % sl, [128, 4, 128], BF16)
                yo = sb(e3, "yo%d" % sl, [128, 1024], BF16)
                B_xe, B_xeT, B_hs, B_h1e, B_actb, B_aT, B_yo = (Buf() for _ in range(7))
                tp, B_tp = TPm[sl], B_TPm[sl]
                h1p, B_h1p, h3p, B_h3p = H1p[sl], B_H1p[sl], H3p[sl], B_H3p[sl]

                def body(b):
                    p = b % 2
                    while b >= 2 and not body_done[b - 2]:
                        weave_yield()
                    dma("sp", lambda e: e.dma_start(out=xe[:], in_=xdisp_d[b * 128:(b + 1) * 128, :]), B_xe, r=[D_xd], w=[B_xe])
                    for kt in range(8):
                        op("pe", lambda e, kt=kt: e.transpose(out=tp[:, kt * 128:(kt + 1) * 128], in_=xe[:, kt * 128:(kt + 1) * 128], identity=identb[:]),
                           r=[B_xe, B_cst], w=[B_tp])
                    op("dve", lambda e: e.tensor_copy(out=xeT[:].rearrange("p k t -> p (k t)"), in_=tp), r=[B_tp], w=[B_xeT])
                    while not cast_issued[b]:
                        weave_yield()
                    W1b = Wb[p][0][:].rearrange("p (k f) -> p k f", k=8)
                    W3b = Wb[p][1][:].rearrange("p (k f) -> p k f", k=8)
                    W2b = Wb[p][2][:].rearrange("p (k f) -> p k f", k=4)
                    for (Wm, mi, dst, Bd) in ((W1b, 0, h1p, B_h1p), (W3b, 1, h3p, B_h3p)):
                        for kt in range(8):
                            op("pe", lambda e, kt=kt, Wm=Wm, dst=dst: e.matmul(dst, lhsT=xeT[:, kt, :], rhs=Wm[:, kt, :], start=(kt == 0), stop=(kt == 7)),
                               r=[B_Wb[p][mi], B_xeT], w=[Bd])
                    op("act", lambda e: e.activation(out=h1e[:], in_=h1p, func=AF.Exp, scale=-1.0), r=[B_h1p], w=[B_h1e])
                    op("act", lambda e: e.activation(out=hs[:], in_=h1p, func=AF.Copy), r=[B_h1p], w=[B_hs])
                    op("dve", lambda e: e.tensor_scalar_add(out=h1e[:], in0=h1e[:], scalar1=1.0), r=[B_h1e], w=[B_h1e])
                    op("dve", lambda e: e.reciprocal(out=h1e[:], in_=h1e[:]), r=[B_h1e], w=[B_h1e])
                    op("dve", lambda e: e.tensor_tensor(out=hs[:], in0=hs[:], in1=h3p, op=ALU.mult), r=[B_hs, B_h3p], w=[B_hs])
                    op("dve", lambda e: e.tensor_tensor(out=actb[:], in0=hs[:], in1=h1e[:], op=ALU.mult), r=[B_hs, B_h1e], w=[B_actb])
                    for ft in range(4):
                        op("pe", lambda e, ft=ft: e.transpose(out=tp[:, ft * 128:(ft + 1) * 128], in_=actb[:, ft * 128:(ft + 1) * 128], identity=identb[:]),
                           r=[B_actb, B_cst], w=[B_tp])
                    op("dve", lambda e: e.tensor_copy(out=aT[:].rearrange("p k t -> p (k t)"), in_=tp[:, 0:512]), r=[B_tp], w=[B_aT])
                    while pj_lock[0]:
                        weave_yield()
                    pj_lock[0] = True
                    for hf, Bp in ((0, B_pjA), (1, B_pjB)):
                        for ft in range(4):
                            op("pe", lambda e, ft=ft, hf=hf: e.matmul(pj[:, hf * 512:(hf + 1) * 512], lhsT=aT[:, ft, :],
                                                                      rhs=W2b[:, ft, hf * 512:(hf + 1) * 512], start=(ft == 0), stop=(ft == 3)),
                               r=[B_aT, B_Wb[p][2]], w=[Bp])
                        op("act", lambda e, hf=hf: e.activation(out=yo[:, hf * 512:(hf + 1) * 512], in_=pj[:, hf * 512:(hf + 1) * 512], func=AF.Copy),
                           r=[Bp], w=[B_yo])
                    pj_lock[0] = False
                    comp_done[b] = True
                    dma("sp", lambda e: e.dma_start(out=ydisp_d[b * 128:(b + 1) * 128, :], in_=yo[:]), B_yo, r=[B_yo], w=[D_yd])
                    body_done[b] = True
                return body

            body_done = [False] * NBLK
            cast_issued = [False] * NBLK
            comp_done = [False] * NBLK
            pj_lock = [False]

            def weights_task():
                for b in range(NBLK):
                    p = b % 2
                    for m in range(3):
                        dma("pool", lambda e, m=m: e.indirect_dma_start(out=Wf[p][m][:], out_offset=None, in_=wsrc[m][:, :],
                                                                        in_offset=bass.IndirectOffsetOnAxis(ap=widx[:, b:b + 1], axis=0),
                                                                        bounds_check=bnd_reg, oob_is_err=False),
                            B_Wf[p][m], r=[B_widx], w=[B_Wf[p][m]])
                    while b >= 2 and not comp_done[b - 2]:
                        weave_yield()
                    for m in range(3):
                        if cast_eng[m] == "act":
                            op("act", lambda e, m=m: e.activation(out=Wb[p][m][:], in_=Wf[p][m][:], func=AF.Copy), r=[B_Wf[p][m]], w=[B_Wb[p][m]])
                        else:
                            op(cast_eng[m], lambda e, m=m: e.tensor_copy(out=Wb[p][m][:], in_=Wf[p][m][:]), r=[B_Wf[p][m]], w=[B_Wb[p][m]])
                    cast_issued[b] = True

            moe_bodies = [make_moe(0), make_moe(1)]
            assert nc.sbuf_bytes_remaining >= 20000, nc.sbuf_bytes_remaining
            weave([weights_task] + [(lambda b=b: moe_bodies[b % 2](b)) for b in range(NBLK)], 3)
            sc.barrier()

        with ExitStack() as e4:
            G2 = load_mod(e4, "G2", 5)
            def make_c(sl):
                y0 = sb(e4, "y0_%d" % sl, [128, 1024], BF16)
                y1 = sb(e4, "y1_%d" % sl, [128, 1024], BF16)
                xr = sb(e4, "xr_%d" % sl, [128, 1024])
                ob = sb(e4, "ob_%d" % sl, [128, 1024])
                B_y0, B_y1, B_xr, B_ob = Buf(), Buf(), Buf(), Buf()

                def body(i):
                    dma("pool", lambda e: e.indirect_dma_start(out=y0[:], out_offset=None, in_=ydisp_d[:, :],
                                                               in_offset=bass.IndirectOffsetOnAxis(ap=dest[:, i, 0:1], axis=0)), B_y0, r=[D_yd, B_dest], w=[B_y0])
                    dma("pool", lambda e: e.indirect_dma_start(out=y1[:], out_offset=None, in_=ydisp_d[:, :],
                                                               in_offset=bass.IndirectOffsetOnAxis(ap=dest[:, i, 1:2], axis=0)), B_y1, r=[D_yd, B_dest], w=[B_y1])
                    dma("sp", lambda e: e.dma_start(out=xr[:], in_=x1_d[i * 128:(i + 1) * 128, :]), B_xr, r=[D_x1], w=[B_xr])
                    op("dve", lambda e: e.tensor_scalar(out=ob[:], in0=y0[:], scalar1=wts[:, i, 0:1], scalar2=None, op0=ALU.mult), r=[B_y0, B_wts], w=[B_ob])
                    op("dve", lambda e: e.scalar_tensor_tensor(out=ob[:], in0=y1[:], scalar=wts[:, i, 1:2], in1=ob[:], op0=ALU.mult, op1=ALU.add),
                       r=[B_y1, B_wts, B_ob], w=[B_ob])
                    op("pool", lambda e: e.tensor_tensor(out=ob[:], in0=ob[:], in1=G2, op=ALU.mult), r=[B_ob, B_mods], w=[B_ob])
                    op("dve", lambda e: e.tensor_tensor(out=ob[:], in0=ob[:], in1=xr[:], op=ALU.add), r=[B_ob, B_xr], w=[B_ob])
                    dma("sp", lambda e: e.dma_start(out=out[i * 128:(i + 1) * 128, :], in_=ob[:]), B_ob, r=[B_ob], w=[])
                return body

            weave_slots(make_c, 2, NOWN)
            sc.barrier()
    return nc


def host_consts(S, C, r):
    c = {}
    c["c_ident"] = np.eye(128, dtype=np.float32)
    c["c_anti"] = np.eye(128, dtype=np.float32)[::-1].copy()
    c["c_anti48"] = np.eye(48, dtype=np.float32)[::-1].copy()
    bd = np.zeros((128, 128), np.float32)
    bd[:64, :64] = 1
    bd[64:, 64:] = 1
    c["c_bd"] = bd
    c["c_triu"] = np.triu(np.ones((128, 128), np.float32), 1)
    er = np.zeros((128, 4096), np.float32)
    k = np.arange(4096)
    er[(k // 64) % 64, k] = 1.0
    er[64 + (k // 64) % 64, k] = 1.0
    c["c_erow"] = er
    v = np.arange(768)
    c["c_ohs"] = oh_table(v - 511 + 128 * r)
    v = np.arange(1152)
    c["c_ohw"] = oh_table(v - 511 + 128 * r, 0, 512)
    w = np.arange(880)
    c["c_ohq"] = oh_table(w + 128 * r - 527)
    w = np.arange(4224)
    c["c_ohk"] = oh_table(w + 128 * r - 2063)
    hi = np.full((128, 9), 3.0e38, np.float32)
    lo = np.full((128, 9), -1.0, np.float32)
    ql = np.arange(128)
    for cc in range(9):
        m_rel = cc - 1
        cur = 2 * r + (ql >= 64)
        forced = (m_rel == cur) | (m_rel == cur - 1)
        invalid = m_rel > cur
        lo[forced, cc] = 1e4
        hi[invalid, cc] = -1.0
    c["c_hi"] = hi
    c["c_lo"] = lo
    c["c_pad"] = np.full((128, 1), 0.0 if r == 0 else 1.0, np.float32)
    NBLK = (S // 512 * 256 + 64 * 127) // 128
    c["c_blk"] = np.tile((np.arange(NBLK, dtype=np.float32) * 128)[None, :], (128, 1))
    c["c_pidx"] = np.arange(128, dtype=np.float32).reshape(128, 1)
    return c


def kernel(x, c, w_ada, b_ada, norm1_w, w_in, q_norm_w, k_norm_w, cmp_pos_k, cmp_pos_v,
           cmp_k_w1, cmp_k_w2, cmp_v_w1, cmp_v_w2, conv_w, out_norm_w, w_out, rel_bias,
           norm2_w, w_group, b_group, w_expert, b_expert, w1, w3, w2, _C=None):
    f = lambda a: np.ascontiguousarray(np.asarray(a, dtype=np.float32))
    x = f(x)
    B, S, D = x.shape
    NB = S // 128
    NOWN = NB // 4
    C = _C if _C is not None else (256 if S >= 8192 else 128)
    nc = build(S, C)

    def pos_lay(p):
        p = f(p)[0]
        return np.ascontiguousarray(p.reshape(16, 2, 64).transpose(1, 2, 0).reshape(128, 16, 1))

    shared = {
        "w_ada": f(w_ada)[0], "b_ada": f(b_ada), "norm1_w": f(norm1_w), "w_in": f(w_in)[0],
        "q_norm_w": f(q_norm_w), "k_norm_w": f(k_norm_w).reshape(1, 192),
        "cmp_pos_k": pos_lay(cmp_pos_k), "cmp_pos_v": pos_lay(cmp_pos_v),
        "cmp_k_w1": f(cmp_k_w1)[0], "cmp_k_w2": f(cmp_k_w2)[0], "cmp_v_w1": f(cmp_v_w1)[0], "cmp_v_w2": f(cmp_v_w2)[0],
        "conv_wl": np.ascontiguousarray(f(conv_w)[0].reshape(3, 4, 128).transpose(2, 1, 0).reshape(128, 12)),
        "onw_c": np.ascontiguousarray(f(out_norm_w)[0, 512:].reshape(4, 128).T),
        "onw_a": np.ascontiguousarray(f(out_norm_w)[:, :512]),
        "w_out": f(w_out)[0], "rel_bias": f(rel_bias), "norm2_w": f(norm2_w),
        "w_rt": np.ascontiguousarray(np.concatenate([f(w_group)[0], f(w_expert)[0]], axis=1)),
        "b_rt": np.ascontiguousarray(np.concatenate([f(b_group), f(b_expert)], axis=1)),
        "w1": np.ascontiguousarray(f(w1)[0].reshape(64, 8, 128, 512).transpose(0, 2, 1, 3).reshape(64 * 128, 4096)),
        "w3": np.ascontiguousarray(f(w3)[0].reshape(64, 8, 128, 512).transpose(0, 2, 1, 3).reshape(64 * 128, 4096)),
        "w2": np.ascontiguousarray(f(w2)[0].reshape(64, 4, 128, 1024).transpose(0, 2, 1, 3).reshape(64 * 128, 4096)),
    }
    cf = f(c)
    in_maps = []
    for core in range(8):
        b, r = core // 4, core % 4
        xb = x[b]
        blocks = xb.reshape(NB, 128, D)
        own = np.ascontiguousarray(blocks[r::4].reshape(NOWN * 128, D))
        prev = np.zeros((NOWN, 2, D), np.float32)
        for i in range(NOWN):
            j = 4 * i + r
            if j > 0:
                prev[i] = xb[128 * j - 2:128 * j]
        m = dict(shared)
        m.update(host_consts(S, C, r))
        m["x_all"] = xb
        m["x_own"] = own
        m["x_prev"] = prev.reshape(NOWN * 2, D)
        m["c_lay"] = np.ascontiguousarray(cf[b].reshape(8, 128).T)
        in_maps.append(m)
    res = run_bass_kernel_spmd(nc, in_maps, core_ids=list(range(8)))
    outp = np.zeros((B, NB, 128, D), np.float32)
    for core in range(8):
        b, r = core // 4, core % 4
        outp[b, r::4] = np.asarray(res.results[core]["out"]).reshape(NOWN, 128, D)
    return outp.reshape(B, S, D)
```

```python
import math
import threading
from contextlib import ExitStack

import numpy as np
import concourse.bass as bass
import concourse.mybir as mybir
from concourse.bass_utils import run_bass_kernel_spmd

F32 = mybir.dt.float32
BF16 = mybir.dt.bfloat16
I32 = mybir.dt.int32
AF = mybir.ActivationFunctionType
ALU = mybir.AluOpType
AX = mybir.AxisListType
NEG = -30000.0
EPS = 1e-6
SAME_ENGINE_SYNC = True


class Buf:
    __slots__ = ("w", "r", "ds", "name")

    def __init__(self, name=""):
        self.w = []
        self.r = {}
        self.ds = {}
        self.name = name


class _Task:
    def __init__(self, fn):
        self.fn = fn
        self.go = threading.Event()
        self.back = threading.Event()
        self.done = False
        self.exc = None
        self.th = threading.Thread(target=self._run, daemon=True)
        self.th.start()

    def _run(self):
        self.go.wait()
        self.go.clear()
        _TL.task = self
        try:
            self.fn()
        except BaseException as e:
            self.exc = e
        self.done = True
        self.back.set()

    def step(self):
        self.go.set()
        self.back.wait()
        self.back.clear()
        if self.exc is not None:
            raise self.exc


_TL = threading.local()


def weave_yield():
    t = getattr(_TL, "task", None)
    if t is not None:
        t.back.set()
        t.go.wait()
        t.go.clear()


def weave(fns, width):
    it = iter(fns)
    active = []
    while True:
        while len(active) < width:
            f_ = next(it, None)
            if f_ is None:
                break
            active.append(_Task(f_))
        if not active:
            break
        for t in list(active):
            t.step()
            if t.done:
                active.remove(t)


def weave_slots(make_body, nslots, nitems):
    bodies = [make_body(k) for k in range(nslots)]
    done = [False] * nitems

    def task(i):
        while i >= nslots and not done[i - nslots]:
            weave_yield()
        bodies[i % nslots](i)
        done[i] = True

    weave([(lambda i=i: task(i)) for i in range(nitems)], nslots)


class Sched:
    def __init__(self, nc, es):
        self.nc = nc
        self.es = es
        self.E = {"pe": nc.tensor, "act": nc.scalar, "dve": nc.vector, "pool": nc.gpsimd, "sp": nc.sync}
        self.sem = {k: es.enter_context(nc.semaphore("cs_" + k)) for k in ("pe", "act", "dve", "pool")}
        self.cnt = {k: 0 for k in self.sem}
        self.seen = {k: {} for k in self.E}
        self.dbufs = []
        self.free_dsems = {"sw": [], "hw": []}
        self.bar = es.enter_context(nc.semaphore("bar"))
        self.barc = 0
        self.nd = 0

    def _wait(self, e, ev):
        if ev is None:
            return
        sem, val, owner = ev
        if owner == e and (e == "pe" or not SAME_ENGINE_SYNC):
            return
        if self.seen[e].get(sem, 0) >= val:
            return
        self.E[e].wait_ge(sem, val)
        self.seen[e][sem] = val

    def _deps(self, e, r, w):
        for b in r:
            for ev in b.w:
                self._wait(e, ev)
        for b in w:
            for ev in b.w:
                self._wait(e, ev)
            for ev in b.r.values():
                self._wait(e, ev)

    def _rec(self, ev, r, w):
        for b in r:
            b.r[ev[0]] = ev
        for b in w:
            if ev[2] == "dma":
                b.w = [o for o in b.w if o[2] == "dma" and o[0] != ev[0]] + [ev]
            else:
                b.w = [ev]
            b.r = {}

    def op(self, e, fn, r=(), w=()):
        self._deps(e, r, w)
        ins = fn(self.E[e])
        self.cnt[e] += 1
        ins.then_inc(self.sem[e], 1)
        self._rec((self.sem[e], self.cnt[e], e), r, w)
        weave_yield()

    def dma(self, q, fn, owner, r=(), w=()):
        self._deps(q, r, w)
        kind = "sw" if q == "pool" else "hw"
        if kind not in owner.ds:
            if self.free_dsems[kind]:
                owner.ds[kind] = list(self.free_dsems[kind].pop())
            else:
                self.nd += 1
                owner.ds[kind] = [self.es.enter_context(self.nc.semaphore("ds%d" % self.nd)), 0]
            self.dbufs.append((owner, kind))
        ins = fn(self.E[q])
        d = owner.ds[kind]
        d[1] += 16
        ins.then_inc(d[0], 16)
        self._rec((d[0], d[1], "dma"), r, w)
        weave_yield()

    def barrier(self):
        for k in self.sem:
            self._wait("sp", (self.sem[k], self.cnt[k], k))
        for b, kind in self.dbufs:
            self._wait("sp", (b.ds[kind][0], b.ds[kind][1], "dma"))
        for b, kind in self.dbufs:
            self.free_dsems[kind].append(tuple(b.ds.pop(kind)))
        self.dbufs = []
        self.barc += 1
        self.E["sp"].sem_inc(self.bar, 1)
        for k in self.sem:
            self.E[k].wait_ge(self.bar, self.barc)


def t5_bucket_np(dist):
    n = np.maximum(dist, 0)
    nf = np.maximum(n, 1).astype(np.float32)
    large = 16 + (np.log(nf / np.float32(16)) / np.float32(math.log(8.0)) * np.float32(16)).astype(np.int32)
    large = np.minimum(large, 31)
    return np.where(n < 16, n, large)


def oh_table(dists, lo_valid=0, hi_valid=None):
    L = len(dists)
    t = np.zeros((33, L), np.float32)
    valid = dists >= lo_valid
    if hi_valid is not None:
        valid &= dists < hi_valid
    bk = t5_bucket_np(dists)
    for i in range(L):
        if valid[i]:
            t[bk[i], i] += 1.0
            t[31, i] -= 1.0
        else:
            t[32, i] = NEG
    return t


def build(S, C):
    NB = S // 128
    NOWN = NB // 4
    NCMP = S // 16 - 1
    NSEL = S // 64
    NCH = (NSEL + 63) // 64
    NCT = (NCMP + 127) // 128
    NBLK = (NOWN * 256 + 64 * 127) // 128
    NROWS = NBLK * 128

    nc = bass.Bass("TRN2", target_bir_lowering=False)

    def din(name, shape, dt=F32):
        return nc.dram_tensor(name, list(shape), dt, kind="ExternalInput").ap()

    def dscr(name, shape, dt):
        return nc.dram_tensor(name, list(shape), dt).ap()

    x_all = din("x_all", [S, 1024])
    x_own = din("x_own", [NOWN * 128, 1024])
    x_prev = din("x_prev", [NOWN * 2, 1024])
    c_lay = din("c_lay", [128, 8])
    w_ada = din("w_ada", [1024, 6144])
    b_ada = din("b_ada", [1, 6144])
    norm1_w = din("norm1_w", [1, 1024])
    w_in = din("w_in", [1024, 2840])
    q_norm_w = din("q_norm_w", [1, 64])
    k_norm_w = din("k_norm_w", [1, 192])
    cmp_pos_k = din("cmp_pos_k", [128, 16, 1])
    cmp_pos_v = din("cmp_pos_v", [128, 16, 1])
    cmp_k_w1 = din("cmp_k_w1", [2048, 256])
    cmp_k_w2 = din("cmp_k_w2", [256, 64])
    cmp_v_w1 = din("cmp_v_w1", [2048, 256])
    cmp_v_w2 = din("cmp_v_w2", [256, 64])
    conv_wl = din("conv_wl", [128, 12])
    onw_c = din("onw_c", [128, 4])
    onw_a = din("onw_a", [1, 512])
    w_out = din("w_out", [1024, 1024])
    rel_bias = din("rel_bias", [32, 8])
    norm2_w = din("norm2_w", [1, 1024])
    w_rt = din("w_rt", [1024, 72])
    b_rt = din("b_rt", [1, 72])
    w1 = din("w1", [64 * 128, 4096])
    w3 = din("w3", [64 * 128, 4096])
    w2 = din("w2", [64 * 128, 4096])
    c_blk = din("c_blk", [128, NBLK])
    c_pidx = din("c_pidx", [128, 1])
    c_ident = din("c_ident", [128, 128])
    c_anti = din("c_anti", [128, 128])
    c_anti48 = din("c_anti48", [48, 48])
    c_bd = din("c_bd", [128, 128])
    c_triu = din("c_triu", [128, 128])
    c_erow = din("c_erow", [128, 4096])
    c_ohs = din("c_ohs", [33, 768])
    c_ohw = din("c_ohw", [33, 1152])
    c_ohq = din("c_ohq", [33, 880])
    c_ohk = din("c_ohk", [33, 4224])
    c_hi = din("c_hi", [128, 9])
    c_lo = din("c_lo", [128, 9])
    c_pad = din("c_pad", [128, 1])
    out = nc.dram_tensor("out", [NOWN * 128, 1024], F32, kind="ExternalOutput").ap()

    qT_d = dscr("qT_d", [NOWN, 128, 512], BF16)
    gates_d = dscr("gates_d", [NOWN, 128, 24], F32)
    mixc_d = dscr("mixc_d", [NOWN, 128, 512], BF16)
    mixa_d = dscr("mixa_d", [NOWN, 128, 512], BF16)
    x1_d = dscr("x1_d", [NOWN * 128, 1024], F32)
    fs_d = dscr("fs_d", [8, 768], BF16)
    fw_d = dscr("fw_d", [8, 1152], BF16)
    fq_d = dscr("fq_d", [8, 880], F32)
    fk_d = dscr("fk_d", [8, 4224], BF16)
    xdisp_d = dscr("xdisp_d", [NROWS, 1024], BF16)
    ydisp_d = dscr("ydisp_d", [NROWS, 1024], BF16)
    D_qT, D_gates, D_mixc, D_mixa, D_x1 = Buf(), Buf(), Buf(), Buf(), Buf()
    D_fs, D_fw, D_fq, D_fk, D_xd, D_yd = Buf(), Buf(), Buf(), Buf(), Buf(), Buf()

    with ExitStack() as es:
        sc = Sched(nc, es)
        op, dma = sc.op, sc.dma

        def sb(es_, name, shape, dt=F32):
            return es_.enter_context(nc.sbuf_tensor(name, list(shape), dt))

        tpb = es.enter_context(nc.psum_tensor("tpb", [128, 1024], BF16))
        pj = es.enter_context(nc.psum_tensor("pj", [128, 1024], F32))
        st = [es.enter_context(nc.psum_tensor("st%d" % i, [128, 512], F32)) for i in range(2)]
        oa = [es.enter_context(nc.psum_tensor("oa%d" % i, [128, 512], F32)) for i in range(2)]
        ms = es.enter_context(nc.psum_tensor("ms", [128, 512], F32))
        B_tpb, B_pjA, B_pjB, B_ms = Buf(), Buf(), Buf(), Buf()
        B_st = [Buf(), Buf()]
        B_oa = [Buf(), Buf()]

        identb = sb(es, "identb", [128, 128], BF16)
        antib = sb(es, "antib", [128, 128], BF16)
        identf = sb(es, "identf", [128, 128])
        ones1 = sb(es, "ones1", [1, 128])
        epsc = sb(es, "epsc", [128, 1])
        B_cst = Buf()
        dma("pool", lambda e: e.dma_start(out=identb[:], in_=c_ident), B_cst, w=[B_cst])
        dma("pool", lambda e: e.dma_start(out=antib[:], in_=c_anti), B_cst, w=[B_cst])
        dma("sp", lambda e: e.dma_start(out=identf[:], in_=c_ident), B_cst, w=[B_cst])
        B_ones = Buf()
        op("dve", lambda e: e.memset(ones1[:], 1.0), w=[B_ones])
        op("dve", lambda e: e.memset(epsc[:], EPS), w=[B_ones])
        qwr = sb(es, "qwr", [128, 64])
        kwr = sb(es, "kwr", [128, 192])
        onar = sb(es, "onar", [128, 512])
        brt = sb(es, "brt", [128, 72])
        B_qwr, B_kwr, B_c2 = Buf(), Buf(), Buf()
        esu = ExitStack()
        zt = sb(esu, "zt", [128, 1024], BF16)
        B_zt = Buf()
        op("pool", lambda e: e.memset(zt[:], 0.0), w=[B_zt])
        for k in range(NROWS // 128):
            dma("sp", lambda e, k=k: e.dma_start(out=xdisp_d[k * 128:(k + 1) * 128, :], in_=zt[:]), B_zt, r=[B_zt], w=[])
        D_xd.w = [(B_zt.ds["hw"][0], B_zt.ds["hw"][1], "dma")]

        def bcast_row(dst_ps, dbuf, row_ap, n):
            op("pe", lambda e: e.matmul(dst_ps, lhsT=ones1[0:1, :], rhs=row_ap, start=True, stop=True),
               r=[B_ones, B_row], w=[dbuf])

        def rstd_from_ss(ss_ap, bufs, inv_n):
            op("act", lambda e: e.activation(out=ss_ap, in_=ss_ap, func=AF.Ln, scale=inv_n, bias=epsc[0:ss_ap.shape[0], :]),
               r=bufs + [B_ones], w=bufs)
            op("act", lambda e: e.activation(out=ss_ap, in_=ss_ap, func=AF.Exp, scale=-0.5), r=bufs, w=bufs)

        rows = sb(esu, "rows", [1, 1024 + 1024 + 64 + 192 + 512 + 72])
        B_row = Buf()
        R_N1, R_N2, R_QW, R_KW, R_ONA, R_BRT = 0, 1024, 2048, 2112, 2304, 2816
        for src, off, n in ((norm1_w, R_N1, 1024), (norm2_w, R_N2, 1024), (q_norm_w, R_QW, 64),
                            (k_norm_w, R_KW, 192), (onw_a, R_ONA, 512), (b_rt, R_BRT, 72)):
            dma("sp", lambda e, src=src, off=off, n=n: e.dma_start(out=rows[0:1, off:off + n], in_=src), B_row, w=[B_row])

        cond = sb(esu, "cond", [128, 8])
        condr = sb(esu, "condr", [128, 8, 128])
        B_cond = Buf()
        dma("sp", lambda e: e.dma_start(out=cond[:], in_=c_lay), B_cond, w=[B_cond])
        ctmp = sb(esu, "ctmp", [128, 8])
        B_ctmp = Buf()
        op("act", lambda e: e.activation(out=ctmp[:], in_=cond[:], func=AF.Exp, scale=-1.0), r=[B_cond], w=[B_ctmp])
        op("dve", lambda e: e.tensor_scalar_add(out=ctmp[:], in0=ctmp[:], scalar1=1.0), r=[B_ctmp], w=[B_ctmp])
        op("dve", lambda e: e.reciprocal(out=ctmp[:], in_=ctmp[:]), r=[B_ctmp], w=[B_ctmp])
        op("dve", lambda e: e.tensor_mul(out=cond[:], in0=cond[:], in1=ctmp[:]), r=[B_ctmp, B_cond], w=[B_cond])
        B_condr = Buf()
        op("dve", lambda e: e.tensor_copy(out=condr[:], in_=cond[:].unsqueeze(2).to_broadcast([128, 8, 128])),
           r=[B_cond], w=[B_condr])
        w_ada_v = w_ada.rearrange("(kt p) n -> p kt n", p=128)
        MODS = sb(esu, "MODS", [128, 6, 1024])
        B_mods = Buf()

        def compute_mods():
            with ExitStack() as es2:
                was = [sb(es2, "wa%d" % q, [128, 8, 512]) for q in range(2)]
                brows = [sb(es2, "brow%d" % q, [1, 512]) for q in range(2)]
                B_was, B_brows = [Buf(), Buf()], [Buf(), Buf()]
                for ch in range(12):
                    c0 = ch * 512
                    wa, brow, B_wa, B_brow = was[ch % 2], brows[ch % 2], B_was[ch % 2], B_brows[ch % 2]
                    dma("sp", lambda e: e.dma_start(out=wa[:], in_=w_ada_v[:, :, c0:c0 + 512]), B_wa, w=[B_wa])
                    dma("sp", lambda e: e.dma_start(out=brow[:], in_=b_ada[0:1, c0:c0 + 512]), B_brow, w=[B_brow])
                    for kt in range(8):
                        op("pe", lambda e, kt=kt: e.matmul(pj[:, 0:512], lhsT=condr[:, kt, :], rhs=wa[:, kt, :],
                                                           start=(kt == 0), stop=False),
                           r=[B_condr, B_wa], w=[B_pjA])
                    op("pe", lambda e: e.matmul(pj[:, 0:512], lhsT=ones1[0:1, :], rhs=brow[:], start=False, stop=True),
                       r=[B_ones, B_brow], w=[B_pjA])
                    op("act", lambda e: e.activation(out=MODS[:, ch // 2, (ch % 2) * 512:(ch % 2) * 512 + 512],
                                                     in_=pj[:, 0:512], func=AF.Copy), r=[B_pjA], w=[B_mods])
                for (slot, roff) in ((1, R_N1), (4, R_N2)):
                    for hh in range(2):
                        bcast_row(pj[:, 0:512], B_pjA, rows[0:1, roff + hh * 512: roff + hh * 512 + 512], 512)
                        seg = MODS[:, slot, hh * 512:(hh + 1) * 512]
                        op("dve", lambda e, seg=seg: e.scalar_tensor_tensor(out=seg, in0=seg, scalar=1.0, in1=pj[:, 0:512],
                                                                            op0=ALU.add, op1=ALU.mult),
                           r=[B_pjA, B_mods], w=[B_mods])

        compute_mods()
        mods_d = dscr("mods_d", [128, 6144], F32)
        D_mods = Buf()
        dma("sp", lambda e: e.dma_start(out=mods_d, in_=MODS[:].rearrange("p k n -> p (k n)")), B_mods, r=[B_mods], w=[D_mods])
        bcast_row(ms[:, 0:64], B_ms, rows[0:1, R_QW:R_QW + 64], 64)
        op("act", lambda e: e.activation(out=qwr[:], in_=ms[:, 0:64], func=AF.Copy, scale=0.125), r=[B_ms], w=[B_qwr])
        bcast_row(ms[:, 0:192], B_ms, rows[0:1, R_KW:R_KW + 192], 192)
        op("act", lambda e: e.activation(out=kwr[:], in_=ms[:, 0:192], func=AF.Copy), r=[B_ms], w=[B_kwr])
        bcast_row(ms[:, 0:512], B_ms, rows[0:1, R_ONA:R_ONA + 512], 512)
        op("act", lambda e: e.activation(out=onar[:], in_=ms[:, 0:512], func=AF.Copy), r=[B_ms], w=[B_kwr])
        bcast_row(ms[:, 0:72], B_ms, rows[0:1, R_BRT:R_BRT + 72], 72)
        op("act", lambda e: e.activation(out=brt[:], in_=ms[:, 0:72], func=AF.Copy), r=[B_ms], w=[B_c2])
        sc.barrier()
        esu.close()

        def load_mod(es_, name, k):
            t = sb(es_, name, [128, 1024])
            dma("sp", lambda e: e.dma_start(out=t[:], in_=mods_d[:, k * 1024:(k + 1) * 1024]), B_mods, r=[D_mods], w=[B_mods])
            return t[:]

        eKV = ExitStack()
        KA = [sb(eKV, "KA%d" % g, [128, S], BF16) for g in range(2)]
        VS = sb(eKV, "VS", [128, NB, 2, 65], BF16)
        KC = sb(eKV, "KC", [128, NCT * 128], BF16)
        VC = sb(eKV, "VC", [128, NCT, 2, 65], BF16)
        kw_d = dscr("kw_d", [NB, 128, 128], BF16)
        vw_d = dscr("vw_d", [NB, 128, 130], BF16)
        D_kw = [Buf() for _ in range(NB)]
        D_vw = [Buf() for _ in range(NB)]
        B_KA = [[Buf() for _ in range(NB)] for _ in range(2)]
        B_VS = [Buf() for _ in range(NB)]
        B_KC, B_VC = Buf(), Buf()
        B_init = Buf()
        op("pool", lambda e: e.memset(VS[:, :, :, 64:65], 1.0), w=B_VS)
        op("pool", lambda e: e.memset(VC[:], 0.0), w=[B_VC])
        op("pool", lambda e: e.memset(VC[:, :, :, 64:65], 1.0), w=[B_VC])
        op("pool", lambda e: e.memset(KC[:], 0.0), w=[B_KC])
        for per in range(S // 4096 if S >= 4096 else 1):
            n = min(4096, S)
            dma("pool", lambda e, per=per, n=n: e.dma_start(out=KA[0][64:128, per * 4096:per * 4096 + n], in_=c_erow[64:128, 0:n]),
                B_init, w=[B_init] + B_KA[0])
            dma("pool", lambda e, per=per, n=n: e.dma_start(out=KA[1][0:64, per * 4096:per * 4096 + n], in_=c_erow[0:64, 0:n]),
                B_init, w=[B_init] + B_KA[1])
        eA = ExitStack()
        SH1 = load_mod(eA, "SH1", 0)
        A1 = load_mod(eA, "A1", 1)

        def rms_mod(xt_ap, hb_ap, A, Bm, npart, bufs_x, bufs_h, scr, ss, B_scr):
            op("act", lambda e: e.activation(out=scr[0:npart, :], in_=xt_ap, func=AF.Square, accum_out=ss[0:npart, :]),
               r=bufs_x, w=[B_scr])
            rstd_from_ss(ss[0:npart, :], [B_scr], 1.0 / 1024)
            op("dve", lambda e: e.scalar_tensor_tensor(out=scr[0:npart, :], in0=xt_ap, scalar=ss[0:npart, 0:1],
                                                       in1=A[0:npart, :], op0=ALU.mult, op1=ALU.mult),
               r=bufs_x + [B_scr, B_mods], w=[B_scr])
            op("pool", lambda e: e.tensor_tensor(out=hb_ap, in0=scr[0:npart, :], in1=Bm[0:npart, :], op=ALU.add),
               r=[B_scr, B_mods], w=bufs_h)

        w_in_v = w_in.rearrange("(kt p) n -> p kt n", p=128)

        with ExitStack() as e0:
            wq = sb(e0, "wq", [128, 8, 512], BF16)
            wg = sb(e0, "wg", [128, 8, 24], BF16)
            wc = sb(e0, "wc", [128, 8, 1536], BF16)
            B_w0 = Buf()
            for kt in range(8):
                dma("pool", lambda e, kt=kt: e.dma_start(out=wq[:, kt, :], in_=w_in_v[:, kt, 0:512]), B_w0, w=[B_w0])
                dma("pool", lambda e, kt=kt: e.dma_start(out=wg[:, kt, :], in_=w_in_v[:, kt, 1280:1304]), B_w0, w=[B_w0])
                dma("pool", lambda e, kt=kt: e.dma_start(out=wc[:, kt, :], in_=w_in_v[:, kt, 1304:2840]), B_w0, w=[B_w0])
            convw = sb(e0, "convw", [128, 12])
            onwc = sb(e0, "onwc", [128, 4])
            bdm = sb(e0, "bdm", [128, 128])
            padf = sb(e0, "padf", [128, 1])
            B_c0 = Buf()
            for t_, s_ in ((convw, conv_wl), (onwc, onw_c), (bdm, c_bd), (padf, c_pad)):
                dma("sp", lambda e, t_=t_, s_=s_: e.dma_start(out=t_[:], in_=s_), B_c0, w=[B_c0])

            def make_a0(sl):
                xt = sb(e0, "xt0_%d" % sl, [128, 1024])
                xp = sb(e0, "xp0_%d" % sl, [2, 1024])
                scr = sb(e0, "scr0_%d" % sl, [128, 1024])
                ss = sb(e0, "ss0_%d" % sl, [128, 1])
                hb = sb(e0, "hb0_%d" % sl, [128, 1024], BF16)
                hbp = sb(e0, "hbp0_%d" % sl, [2, 1024], BF16)
                hT = sb(e0, "hT0_%d" % sl, [128, 8, 130], BF16)
                qf = sb(e0, "qf_%d" % sl, [128, 512])
                qsq = sb(e0, "qsq_%d" % sl, [128, 512])
                qss = sb(e0, "qss_%d" % sl, [128, 8])
                qp = sb(e0, "qp_%d" % sl, [128, 512], BF16)
                qTs = sb(e0, "qTs_%d" % sl, [128, 512], BF16)
                gts = sb(e0, "gts_%d" % sl, [128, 24])
                uT = sb(e0, "uT_%d" % sl, [128, 130])
                cu = sb(e0, "cu_%d" % sl, [128, 130])
                yc = sb(e0, "yc_%d" % sl, [128, 128])
                ysq = sb(e0, "ysq_%d" % sl, [128, 128])
                yrs = sb(e0, "yrs_%d" % sl, [128, 128])
                mixc = sb(e0, "mixc_%d" % sl, [128, 4, 128], BF16)
                B_xt, B_xp, B_scr, B_hb, B_hbp, B_hT = Buf(), Buf(), Buf(), Buf(), Buf(), Buf()
                B_qf, B_qsq, B_qss, B_qp, B_qTs, B_gts = Buf(), Buf(), Buf(), Buf(), Buf(), Buf()
                B_uT, B_cu, B_yc, B_ysq, B_yrs, B_mixc = Buf(), Buf(), Buf(), Buf(), Buf(), Buf()
                TP, B_TP = (tpb[:], B_tpb) if sl == 0 else (oa[0][:].bitcast(BF16), B_oa[0])
                PQ, B_PQ = (pj[:, 0:512], B_pjA) if sl == 0 else (pj[:, 512:1024], B_pjB)
                MS, B_MS = (ms[:], B_ms) if sl == 0 else (oa[1][:], B_oa[1])
                CV, B_CV = st[sl], B_st[sl]

                def body(i):
                    dma("sp", lambda e: e.dma_start(out=xt[:], in_=x_own[i * 128:(i + 1) * 128, :]), B_xt, w=[B_xt])
                    dma("sp", lambda e: e.dma_start(out=xp[:], in_=x_prev[i * 2:(i + 1) * 2, :]), B_xp, w=[B_xp])
                    rms_mod(xt[:], hb[:], A1, SH1, 128, [B_xt], [B_hb], scr, ss, B_scr)
                    rms_mod(xp[:], hbp[:], A1, SH1, 2, [B_xp], [B_hbp], scr, ss, B_scr)
                    for kt in range(8):
                        op("pe", lambda e, kt=kt: e.transpose(out=TP[:, kt * 128:(kt + 1) * 128], in_=hb[:, kt * 128:(kt + 1) * 128],
                                                              identity=identb[:]), r=[B_hb, B_cst], w=[B_TP])
                    op("act", lambda e: e.activation(out=hT[:, :, 2:130], in_=TP.rearrange("p (k t) -> p k t", k=8), func=AF.Copy),
                       r=[B_TP], w=[B_hT])
                    for kt in range(8):
                        op("pe", lambda e, kt=kt: e.transpose(out=TP[:, kt * 2:(kt + 1) * 2], in_=hbp[:, kt * 128:(kt + 1) * 128],
                                                              identity=identb[0:2, 0:2]), r=[B_hbp, B_cst], w=[B_TP])
                    op("act", lambda e: e.activation(out=hT[:, :, 0:2], in_=TP[:, 0:16].rearrange("p (k t) -> p k t", k=8), func=AF.Copy),
                       r=[B_TP], w=[B_hT])
                    for kt in range(8):
                        op("pe", lambda e, kt=kt: e.matmul(PQ, lhsT=hT[:, kt, 2:130], rhs=wq[:, kt, :],
                                                           start=(kt == 0), stop=(kt == 7)), r=[B_hT, B_w0], w=[B_PQ])
                    for kt in range(8):
                        op("pe", lambda e, kt=kt: e.matmul(MS[:, 0:24], lhsT=hT[:, kt, 2:130], rhs=wg[:, kt, :],
                                                           start=(kt == 0), stop=(kt == 7)), r=[B_hT, B_w0], w=[B_MS])
                    op("act", lambda e: e.activation(out=gts[:], in_=MS[:, 0:24], func=AF.Exp, scale=-1.0), r=[B_MS], w=[B_gts])
                    op("dve", lambda e: e.tensor_scalar_add(out=gts[:], in0=gts[:], scalar1=1.0), r=[B_gts], w=[B_gts])
                    op("dve", lambda e: e.reciprocal(out=gts[:], in_=gts[:]), r=[B_gts], w=[B_gts])
                    dma("sp", lambda e: e.dma_start(out=gates_d[i], in_=gts[:]), B_gts, r=[B_gts], w=[D_gates])
                    op("act", lambda e: e.activation(out=qf[:], in_=PQ, func=AF.Copy), r=[B_PQ], w=[B_qf])
                    op("pool", lambda e: e.tensor_tensor(out=qsq[:], in0=qf[:], in1=qf[:], op=ALU.mult), r=[B_qf], w=[B_qsq])
                    op("dve", lambda e: e.tensor_reduce(out=qss[:], in_=qsq[:].rearrange("p (h d) -> p h d", d=64), axis=AX.X, op=ALU.add),
                       r=[B_qsq], w=[B_qss])
                    rstd_from_ss(qss[:], [B_qss], 1.0 / 64)
                    op("dve", lambda e: e.tensor_tensor(out=qsq[:].rearrange("p (h d) -> p h d", d=64),
                                                        in0=qf[:].rearrange("p (h d) -> p h d", d=64),
                                                        in1=qss[:].unsqueeze(2).to_broadcast([128, 8, 64]), op=ALU.mult),
                       r=[B_qf, B_qss], w=[B_qsq])
                    op("dve", lambda e: e.tensor_tensor(out=qp[:].rearrange("p (a g d) -> p g a d", a=4, g=2, d=64),
                                                        in0=qsq[:].rearrange("p (g a d) -> p g a d", g=2, a=4, d=64),
                                                        in1=qwr[:].unsqueeze(1).unsqueeze(1).to_broadcast([128, 2, 4, 64]), op=ALU.mult),
                       r=[B_qsq, B_qwr], w=[B_qp])
                    for a in range(4):
                        op("pe", lambda e, a=a: e.transpose(out=TP[:, a * 128:(a + 1) * 128], in_=qp[:, a * 128:(a + 1) * 128],
                                                            identity=identb[:]), r=[B_qp, B_cst], w=[B_TP])
                    op("act", lambda e: e.activation(out=qTs[:], in_=TP[:, 0:512], func=AF.Copy), r=[B_TP], w=[B_qTs])
                    dma("sp", lambda e: e.dma_start(out=qT_d[i], in_=qTs[:]), B_qTs, r=[B_qTs], w=[D_qT])
                    for ct in range(4):
                        ps = CV
                        Bp = B_CV
                        for (k, col0, t0) in ((0, 0, 2), (1, 512, 0), (2, 1024, 0)):
                            nt = 130 - t0
                            for kt in range(8):
                                op("pe", lambda e, kt=kt, k=k, col0=col0, t0=t0, nt=nt: e.matmul(
                                    ps[:, k * 130:k * 130 + nt], lhsT=wc[:, kt, col0 + ct * 128: col0 + ct * 128 + 128],
                                    rhs=hT[:, kt, t0:130], start=(kt == 0), stop=(kt == 7)), r=[B_hT, B_w0], w=[Bp])
                        op("act", lambda e: e.activation(out=uT[:], in_=ps[:, 260:390], func=AF.Copy), r=[Bp], w=[B_uT])
                        op("dve", lambda e: e.tensor_tensor(out=cu[:], in0=ps[:, 130:260], in1=uT[:], op=ALU.mult), r=[Bp, B_uT], w=[B_cu])
                        if i == 0:
                            op("dve", lambda e: e.tensor_scalar(out=cu[:, 0:2], in0=cu[:, 0:2], scalar1=padf[:, 0:1], scalar2=None,
                                                                op0=ALU.mult), r=[B_cu, B_c0], w=[B_cu])
                        op("dve", lambda e: e.tensor_scalar(out=yc[:], in0=cu[:, 0:128], scalar1=convw[:, ct * 3:ct * 3 + 1], scalar2=None,
                                                            op0=ALU.mult), r=[B_cu, B_c0], w=[B_yc])
                        for k in (1, 2):
                            op("dve", lambda e, k=k: e.scalar_tensor_tensor(out=yc[:], in0=cu[:, k:k + 128],
                                                                            scalar=convw[:, ct * 3 + k:ct * 3 + k + 1], in1=yc[:],
                                                                            op0=ALU.mult, op1=ALU.add), r=[B_cu, B_c0, B_yc], w=[B_yc])
                        op("dve", lambda e: e.tensor_tensor(out=yc[:], in0=yc[:], in1=ps[:, 0:128], op=ALU.mult), r=[Bp, B_yc], w=[B_yc])
                        op("pool", lambda e: e.tensor_tensor(out=ysq[:], in0=yc[:], in1=yc[:], op=ALU.mult), r=[B_yc], w=[B_ysq])
                        op("pe", lambda e: e.matmul(MS[:, 0:128], lhsT=bdm[:], rhs=ysq[:], start=True, stop=True), r=[B_c0, B_ysq], w=[B_MS])
                        op("act", lambda e: e.activation(out=yrs[:], in_=MS[:, 0:128], func=AF.Ln, scale=1.0 / 64, bias=epsc[:]),
                           r=[B_MS, B_ones], w=[B_yrs])
                        op("act", lambda e: e.activation(out=yrs[:], in_=yrs[:], func=AF.Exp, scale=-0.5), r=[B_yrs], w=[B_yrs])
                        op("dve", lambda e: e.scalar_tensor_tensor(out=mixc[:, ct, :], in0=yc[:], scalar=onwc[:, ct:ct + 1], in1=yrs[:],
                                                                   op0=ALU.mult, op1=ALU.mult), r=[B_yc, B_yrs, B_c0], w=[B_mixc])
                    dma("sp", lambda e: e.dma_start(out=mixc_d[i], in_=mixc[:].rearrange("p c t -> p (c t)")), B_mixc, r=[B_mixc], w=[D_mixc])
                return body

            weave_slots(make_a0, 2, NOWN)
            sc.barrier()

        with ExitStack() as e1:
            e1a = ExitStack()
            e1_real = e1
            e1 = e1a
            wkv = sb(e1, "wkv", [128, 8, 768], BF16)
            B_wkv = Buf()
            for kt in range(8):
                dma("pool", lambda e, kt=kt: e.dma_start(out=wkv[:, kt, :], in_=w_in_v[:, kt, 512:1280]), B_wkv, w=[B_wkv])
            cw1 = [sb(e1, "cw1_%d" % k, [128, 16, 256], BF16) for k in range(2)]
            cw2 = [sb(e1, "cw2_%d" % k, [128, 2, 64], BF16) for k in range(2)]
            cpos = [sb(e1, "cpos_%d" % k, [128, 16, 1], BF16) for k in range(2)]
            B_cw = Buf()
            for k, (a1_, a2_, ap_) in enumerate(((cmp_k_w1, cmp_k_w2, cmp_pos_k), (cmp_v_w1, cmp_v_w2, cmp_pos_v))):
                dma("pool", lambda e, k=k, a1_=a1_: e.dma_start(out=cw1[k][:], in_=a1_.rearrange("(kt p) n -> p kt n", p=128)), B_cw, w=[B_cw])
                dma("pool", lambda e, k=k, a2_=a2_: e.dma_start(out=cw2[k][:], in_=a2_.rearrange("(kt p) n -> p kt n", p=128)), B_cw, w=[B_cw])
                dma("pool", lambda e, k=k, ap_=ap_: e.dma_start(out=cpos[k][:], in_=ap_), B_cw, w=[B_cw])
            cb1 = [sb(e1, "cb1_%d" % k, [128, 2]) for k in range(2)]
            B_cb1 = Buf()
            for k in range(2):
                for hf in range(2):
                    for kt in range(16):
                        op("pe", lambda e, k=k, hf=hf, kt=kt: e.matmul(ms[:, hf:hf + 1], lhsT=cw1[k][:, kt, hf * 128:(hf + 1) * 128],
                                                                      rhs=cpos[k][:, kt, :], start=(kt == 0), stop=(kt == 15)),
                           r=[B_cw], w=[B_ms])
                op("act", lambda e, k=k: e.activation(out=cb1[k][:], in_=ms[:, 0:2], func=AF.Copy), r=[B_ms], w=[B_cb1])
            KSd = [[[sb(e1, "KS%d%d%d" % (q, k, g), [128, 544], BF16) for g in range(2)] for k in range(2)] for q in range(2)]
            B_KSd = [Buf(), Buf()]
            for q in range(2):
                for k in range(2):
                    for g in range(2):
                        op("pool", lambda e, q=q, k=k, g=g: e.memset(KSd[q][k][g][:], 0.0), w=[B_KSd[q]])
            NSL = 2
            xt = [sb(e1, "xt1_%d" % k, [128, 1024]) for k in range(NSL)]
            scr_ = [sb(e1, "scr1_%d" % k, [128, 1024]) for k in range(NSL)]
            ss_ = [sb(e1, "ss1_%d" % k, [128, 1]) for k in range(NSL)]
            hb_ = [sb(e1, "hb1_%d" % k, [128, 1024], BF16) for k in range(NSL)]
            hT_ = [sb(e1, "hT1_%d" % k, [128, 8, 128], BF16) for k in range(NSL)]
            kvf_ = [sb(e1, "kvf_%d" % k, [128, 768]) for k in range(NSL)]
            ksq_ = [sb(e1, "ksq_%d" % k, [128, 128]) for k in range(NSL)]
            kss_ = [sb(e1, "kss_%d" % k, [128, 2]) for k in range(NSL)]
            knb_ = [sb(e1, "knb_%d" % k, [128, 128], BF16) for k in range(NSL)]
            craw_ = [sb(e1, "craw_%d" % k, [128, 2, 2, 128], BF16) for k in range(NSL)]
            kwst = [sb(e1, "kwst%d" % k, [128, 128], BF16) for k in range(2)]
            vwst = [sb(e1, "vwst%d" % k, [128, 2, 65], BF16) for k in range(2)]
            B_kwst = [Buf(), Buf()]
            B_vwst = [Buf(), Buf()]
            for k in range(2):
                op("pool", lambda e, k=k: e.memset(vwst[k][:, :, 64:65], 1.0), w=[B_vwst[k]])
            B_xt1 = [Buf() for _ in range(NSL)]
            B_scr_ = [Buf() for _ in range(NSL)]
            B_hb_ = [Buf() for _ in range(NSL)]
            B_hT_ = [Buf() for _ in range(NSL)]
            B_kvf_ = [Buf() for _ in range(NSL)]
            B_ksq_ = [Buf() for _ in range(NSL)]
            B_kss_ = [Buf() for _ in range(NSL)]
            B_knb_ = [Buf() for _ in range(NSL)]
            B_craw_ = [Buf() for _ in range(NSL)]
            TPs = [tpb[:], st[0][:].bitcast(BF16)]
            B_TPs = [B_tpb, B_st[0]]
            PAs = [pj[:, 0:512], oa[0][:, 0:512]]
            B_PAs = [B_pjA, B_oa[0]]
            PBs = [pj[:, 512:768], oa[1][:, 0:256]]
            B_PBs = [B_pjB, B_oa[1]]
            tpc = st[1][:].bitcast(BF16)
            B_tpc = B_st[1]
            kssc = sb(e1, "kssc", [128, 2])
            B_kssc = Buf()
            hid = sb(e1, "hid", [128, 2, 32], BF16)
            hidf = sb(e1, "hidf", [128, 32])
            hide = sb(e1, "hide", [128, 32])
            kcf = sb(e1, "kcf", [32, 64])
            kcb = sb(e1, "kcb", [32, 128], BF16)
            B_hid, B_hidf, B_hide, B_kcf, B_kcb = Buf(), Buf(), Buf(), Buf(), Buf()

            def run_interleaved(gens, width):
                active = []
                it = iter(gens)
                while True:
                    while len(active) < width:
                        g_ = next(it, None)
                        if g_ is None:
                            break
                        active.append(g_)
                    if not active:
                        break
                    for g_ in list(active):
                        try:
                            next(g_)
                        except StopIteration:
                            active.remove(g_)

            def proj_block(J, jj):
                j = 4 * J + jj
                sl = j % NSL
                xb_, Bx = xt[sl], B_xt1[sl]
                scr, ss, hb, hT, kvf, ksq, kss, knb, craw = scr_[sl], ss_[sl], hb_[sl], hT_[sl], kvf_[sl], ksq_[sl], kss_[sl], knb_[sl], craw_[sl]
                B_scr1, B_hb1, B_hT1, B_kvf, B_ksq, B_kss, B_knb, B_craw = (B_scr_[sl], B_hb_[sl], B_hT_[sl], B_kvf_[sl], B_ksq_[sl], B_kss_[sl],
                                                                             B_knb_[sl], B_craw_[sl])
                tp, B_tp = TPs[sl], B_TPs[sl]
                KSr = KSd[J % 2]
                B_KS = B_KSd[J % 2]
                dma("sp", lambda e: e.dma_start(out=xb_[:], in_=x_all[j * 128:(j + 1) * 128, :]), Bx, w=[Bx])
                yield
                op("act", lambda e: e.activation(out=scr[:], in_=xb_[:], func=AF.Square, accum_out=ss[:]), r=[Bx], w=[B_scr1])
                yield
                op("act", lambda e: e.activation(out=ss[:], in_=ss[:], func=AF.Ln, scale=1.0 / 1024, bias=epsc[:]), r=[B_scr1, B_ones], w=[B_scr1])
                op("act", lambda e: e.activation(out=ss[:], in_=ss[:], func=AF.Exp, scale=-0.5), r=[B_scr1], w=[B_scr1])
                yield
                op("dve", lambda e: e.scalar_tensor_tensor(out=scr[:], in0=xb_[:], scalar=ss[:, 0:1], in1=A1, op0=ALU.mult, op1=ALU.mult),
                   r=[Bx, B_scr1, B_mods], w=[B_scr1])
                yield
                op("pool", lambda e: e.tensor_tensor(out=hb[:], in0=scr[:], in1=SH1, op=ALU.add), r=[B_scr1, B_mods], w=[B_hb1])
                yield
                for kt in range(8):
                    op("pe", lambda e, kt=kt: e.transpose(out=tp[:, kt * 128:(kt + 1) * 128], in_=hb[:, kt * 128:(kt + 1) * 128],
                                                          identity=identb[:]), r=[B_hb1, B_cst], w=[B_tp])
                yield
                op("act", lambda e: e.activation(out=hT[:].rearrange("p k t -> p (k t)"), in_=tp, func=AF.Copy), r=[B_tp], w=[B_hT1])
                yield
                for (c0, n, pdst, Bp) in ((0, 512, PAs[sl], B_PAs[sl]), (512, 256, PBs[sl], B_PBs[sl])):
                    for kt in range(8):
                        op("pe", lambda e, kt=kt, c0=c0, n=n, pdst=pdst: e.matmul(pdst, lhsT=hT[:, kt, :], rhs=wkv[:, kt, c0:c0 + n],
                                                                                start=(kt == 0), stop=(kt == 7)),
                           r=[B_hT1, B_wkv], w=[Bp])
                yield
                op("act", lambda e: e.activation(out=kvf[:, 0:512], in_=PAs[sl], func=AF.Copy), r=[B_PAs[sl]], w=[B_kvf])
                op("act", lambda e: e.activation(out=kvf[:, 512:768], in_=PBs[sl], func=AF.Copy), r=[B_PBs[sl]], w=[B_kvf])
                yield
                ws2 = j % 2
                op("dve", lambda e: e.tensor_copy(out=VS[:, j, :, 0:64], in_=kvf[:, 384:512].rearrange("p (g d) -> p g d", g=2)),
                   r=[B_kvf], w=[B_VS[j]])
                op("dve", lambda e: e.tensor_copy(out=vwst[ws2][:, :, 0:64], in_=kvf[:, 640:768].rearrange("p (g d) -> p g d", g=2)),
                   r=[B_kvf], w=[B_vwst[ws2]])
                dma("sp", lambda e: e.dma_start(out=vw_d[j], in_=vwst[ws2][:].rearrange("p g d -> p (g d)")), B_vwst[ws2],
                    r=[B_vwst[ws2]], w=[D_vw[j]])
                yield
                for (col0, kw_i, which) in ((256, 1, "slc"), (512, 2, "win")):
                    src = kvf[:, col0:col0 + 128]
                    op("pool", lambda e, src=src: e.tensor_tensor(out=ksq[:], in0=src, in1=src, op=ALU.mult), r=[B_kvf], w=[B_ksq])
                    yield
                    op("dve", lambda e: e.tensor_reduce(out=kss[:], in_=ksq[:].rearrange("p (g d) -> p g d", g=2), axis=AX.X, op=ALU.add),
                       r=[B_ksq], w=[B_kss])
                    yield
                    op("act", lambda e: e.activation(out=kss[:], in_=kss[:], func=AF.Ln, scale=1.0 / 64, bias=epsc[:]), r=[B_kss, B_ones], w=[B_kss])
                    op("act", lambda e: e.activation(out=kss[:], in_=kss[:], func=AF.Exp, scale=-0.5), r=[B_kss], w=[B_kss])
                    yield
                    op("dve", lambda e, src=src: e.tensor_tensor(out=ksq[:].rearrange("p (g d) -> p g d", g=2),
                                                                 in0=src.rearrange("p (g d) -> p g d", g=2),
                                                                 in1=kss[:].unsqueeze(2).to_broadcast([128, 2, 64]), op=ALU.mult),
                       r=[B_kvf, B_kss], w=[B_ksq])
                    op("dve", lambda e, kw_i=kw_i: e.tensor_tensor(out=knb[:].rearrange("p (g d) -> p g d", g=2),
                                                                   in0=ksq[:].rearrange("p (g d) -> p g d", g=2),
                                                                   in1=kwr[:, kw_i * 64:(kw_i + 1) * 64].unsqueeze(1).to_broadcast([128, 2, 64]),
                                                                   op=ALU.mult), r=[B_ksq, B_kwr], w=[B_knb])
                    yield
                    op("pe", lambda e: e.transpose(out=tp[:, 0:128], in_=knb[:], identity=identb[:]), r=[B_knb, B_cst], w=[B_tp])
                    yield
                    if which == "slc":
                        op("act", lambda e: e.activation(out=KA[0][0:64, j * 128:(j + 1) * 128], in_=tp[0:64, 0:128], func=AF.Copy),
                           r=[B_tp], w=[B_KA[0][j]])
                        op("act", lambda e: e.activation(out=KA[1][64:128, j * 128:(j + 1) * 128], in_=tp[64:128, 0:128], func=AF.Copy),
                           r=[B_tp], w=[B_KA[1][j]])
                    else:
                        op("act", lambda e: e.activation(out=kwst[ws2][:], in_=tp[:, 0:128], func=AF.Copy), r=[B_tp], w=[B_kwst[ws2]])
                        dma("sp", lambda e: e.dma_start(out=kw_d[j], in_=kwst[ws2][:]), B_kwst[ws2], r=[B_kwst[ws2]], w=[D_kw[j]])
                    yield
                for k in range(2):
                    base = k * 128
                    op("dve", lambda e, k=k, base=base: e.tensor_copy(out=craw[:, k, 0, :], in_=kvf[:, base:base + 128]), r=[B_kvf], w=[B_craw])
                    op("dve", lambda e, k=k, base=base: e.tensor_copy(out=craw[:, k, 1, 0:64], in_=kvf[:, base + 64:base + 128]), r=[B_kvf], w=[B_craw])
                    op("dve", lambda e, k=k, base=base: e.tensor_copy(out=craw[:, k, 1, 64:128], in_=kvf[:, base:base + 64]), r=[B_kvf], w=[B_craw])
                yield
                for k in range(2):
                    for o in range(2):
                        op("pe", lambda e, k=k, o=o: e.transpose(out=tp[:, (k * 2 + o) * 128:(k * 2 + o + 1) * 128], in_=craw[:, k, o, :],
                                                                identity=identb[:]), r=[B_craw, B_cst], w=[B_tp])
                yield
                c0 = 16 + jj * 128
                for k in range(2):
                    Ta = tp[:, (k * 2) * 128:(k * 2 + 1) * 128]
                    Tb = tp[:, (k * 2 + 1) * 128:(k * 2 + 2) * 128]
                    op("act", lambda e, k=k, Ta=Ta: e.activation(out=KSr[k][0][0:64, c0:c0 + 128], in_=Ta[0:64, :], func=AF.Copy), r=[B_tp], w=[B_KS])
                    op("act", lambda e, k=k, Tb=Tb: e.activation(out=KSr[k][0][64:128, c0 - 1:c0 + 127], in_=Tb[64:128, :], func=AF.Copy), r=[B_tp], w=[B_KS])
                    op("act", lambda e, k=k, Tb=Tb: e.activation(out=KSr[k][1][0:64, c0:c0 + 128], in_=Tb[0:64, :], func=AF.Copy), r=[B_tp], w=[B_KS])
                    op("act", lambda e, k=k, Ta=Ta: e.activation(out=KSr[k][1][64:128, c0 - 1:c0 + 127], in_=Ta[64:128, :], func=AF.Copy), r=[B_tp], w=[B_KS])
                    yield

            def compress_gen(J):
                KSr = KSd[J % 2]
                B_KS = B_KSd[J % 2]
                kss, B_kss = kssc, B_kssc
                n0 = 32 * J - 1
                nlo = max(n0, 0)
                nn = 32 * J + 31 - nlo
                col_lo = 16 * (nlo - n0)
                for k in range(2):
                    for g in range(2):
                        for hf in range(2):
                            for kt in range(16):
                                rhs_ap = bass.AP(KSr[k][g][:].tensor, KSr[k][g][:, col_lo + 2 * kt:col_lo + 2 * kt + 1].offset,
                                                 [[KSr[k][g][:].ap[0][0], 128], [16, nn]])
                                op("pe", lambda e, rhs_ap=rhs_ap, k=k, hf=hf, kt=kt: e.matmul(ms[:, 0:nn], lhsT=cw1[k][:, kt, hf * 128:(hf + 1) * 128],
                                                                                            rhs=rhs_ap, start=(kt == 0), stop=(kt == 15)),
                                   r=[B_KS, B_cw], w=[B_ms])
                            yield
                            op("dve", lambda e, k=k, hf=hf: e.tensor_scalar(out=hidf[:, 0:nn], in0=ms[:, 0:nn], scalar1=cb1[k][:, hf:hf + 1], scalar2=None,
                                                                            op0=ALU.add), r=[B_ms, B_cb1], w=[B_hidf])
                            yield
                            op("act", lambda e: e.activation(out=hide[:, 0:nn], in_=hidf[:, 0:nn], func=AF.Exp, scale=-1.0), r=[B_hidf], w=[B_hide])
                            yield
                            op("dve", lambda e: e.tensor_scalar_add(out=hide[:, 0:nn], in0=hide[:, 0:nn], scalar1=1.0), r=[B_hide], w=[B_hide])
                            op("dve", lambda e: e.reciprocal(out=hide[:, 0:nn], in_=hide[:, 0:nn]), r=[B_hide], w=[B_hide])
                            op("dve", lambda e, hf=hf: e.tensor_tensor(out=hid[:, hf, 0:nn], in0=hidf[:, 0:nn], in1=hide[:, 0:nn], op=ALU.mult),
                               r=[B_hidf, B_hide], w=[B_hid])
                            yield
                        for hf in range(2):
                            op("pe", lambda e, k=k, hf=hf: e.matmul(ms[0:nn, 0:64], lhsT=hid[:, hf, 0:nn], rhs=cw2[k][:, hf, :],
                                                                    start=(hf == 0), stop=(hf == 1)), r=[B_hid, B_cw], w=[B_ms])
                        yield
                        tn, r0 = nlo // 128, nlo % 128
                        if k == 1:
                            op("act", lambda e: e.activation(out=kcb[0:nn, 0:64], in_=ms[0:nn, 0:64], func=AF.Copy), r=[B_ms], w=[B_kcb])
                            n1 = min(nn, 128 - r0)
                            dma("sp", lambda e, g=g, tn=tn, r0=r0, n1=n1: e.dma_start(out=VC[r0:r0 + n1, tn, g, 0:64], in_=kcb[0:n1, 0:64]),
                                B_kcb, r=[B_kcb], w=[B_VC])
                            if n1 < nn:
                                dma("sp", lambda e, g=g, tn=tn, n1=n1: e.dma_start(out=VC[0:nn - n1, tn + 1, g, 0:64], in_=kcb[n1:nn, 0:64]),
                                    B_kcb, r=[B_kcb], w=[B_VC])
                        else:
                            op("act", lambda e: e.activation(out=kcf[0:nn, :], in_=ms[0:nn, 0:64], func=AF.Square, accum_out=kss[0:nn, 0:1]),
                               r=[B_ms], w=[B_kcf, B_kss])
                            yield
                            rstd_from_ss(kss[0:nn, 0:1], [B_kss], 1.0 / 64)
                            yield
                            op("dve", lambda e, g=g: e.scalar_tensor_tensor(out=kcb[0:nn, g * 64:(g + 1) * 64], in0=ms[0:nn, 0:64], scalar=kss[0:nn, 0:1],
                                                                            in1=kwr[0:nn, 0:64], op0=ALU.mult, op1=ALU.mult),
                               r=[B_ms, B_kss, B_kwr], w=[B_kcb])
                            if g == 1:
                                yield
                                op("pe", lambda e: e.transpose(out=tpc[:, 0:nn], in_=kcb[0:nn, :], identity=identb[0:nn, 0:nn]), r=[B_kcb, B_cst], w=[B_tpc])
                                yield
                                op("act", lambda e: e.activation(out=KC[:, nlo:nlo + nn], in_=tpc[:, 0:nn], func=AF.Copy), r=[B_tpc], w=[B_KC])
                        yield

            pending = []
            for J in range(NOWN):
                if J > 0:
                    for k in range(2):
                        for g in range(2):
                            op("dve", lambda e, k=k, g=g: e.tensor_copy(out=KSd[J % 2][k][g][:, 0:16], in_=KSd[(J - 1) % 2][k][g][:, 512:528]),
                               r=[B_KSd[(J - 1) % 2]], w=[B_KSd[J % 2]])
                run_interleaved(pending + [proj_block(J, jj) for jj in range(4)], 3 if pending else 2)
                pending = [compress_gen(J)]
            run_interleaved(pending, 1)

            sc.barrier()
            e1a.close()
            eA.close()
            e1b_ = ExitStack()
            e1 = e1b_
            relb = sb(e1, "relb", [33, 8])
            B_relb = Buf()
            op("dve", lambda e: e.memset(relb[32:33, :], 1.0), w=[B_relb])
            dma("sp", lambda e: e.dma_start(out=relb[0:32, :], in_=rel_bias), B_relb, w=[B_relb])
            hi9 = sb(e1, "hi9", [128, 9])
            lo9 = sb(e1, "lo9", [128, 9])
            anti48 = sb(e1, "anti48", [48, 48])
            B_c1 = Buf()
            for t_, s_ in ((hi9, c_hi), (lo9, c_lo), (anti48, c_anti48)):
                dma("sp", lambda e, t_=t_, s_=s_: e.dma_start(out=t_[:], in_=s_), B_c1, w=[B_c1])
            with ExitStack() as e1t:
                oht = sb(e1t, "oht", [33, 4224])
                ftab = sb(e1t, "ftab", [8, 4224])
                ftabb = sb(e1t, "ftabb", [8, 4224], BF16)
                B_oht, B_ftab, B_ftabb = Buf(), Buf(), Buf()
                for (src, L, dst, Dd, isb) in ((c_ohs, 768, fs_d, D_fs, True), (c_ohw, 1152, fw_d, D_fw, True),
                                               (c_ohq, 880, fq_d, D_fq, False), (c_ohk, 4224, fk_d, D_fk, True)):
                    dma("sp", lambda e, src=src, L=L: e.dma_start(out=oht[:, 0:L], in_=src), B_oht, w=[B_oht])
                    for c0 in range(0, L, 512):
                        n = min(512, L - c0)
                        op("pe", lambda e, c0=c0, n=n: e.matmul(ms[0:8, 0:n], lhsT=relb[:], rhs=oht[:, c0:c0 + n], start=True, stop=True),
                           r=[B_relb, B_oht], w=[B_ms])
                        op("act", lambda e, c0=c0, n=n: e.activation(out=ftab[:, c0:c0 + n], in_=ms[0:8, 0:n], func=AF.Copy),
                           r=[B_ms], w=[B_ftab])
                    if isb:
                        op("dve", lambda e, L=L: e.tensor_copy(out=ftabb[:, 0:L], in_=ftab[:, 0:L]), r=[B_ftab], w=[B_ftabb])
                        dma("sp", lambda e, L=L, dst=dst: e.dma_start(out=dst, in_=ftabb[:, 0:L]), B_ftabb, r=[B_ftabb], w=[Dd])
                    else:
                        dma("sp", lambda e, L=L, dst=dst: e.dma_start(out=dst, in_=ftab[:, 0:L]), B_ftab, r=[B_ftab], w=[Dd])
                sc.barrier()

            def toep(dram_ap, off, pstride, rowlen):
                return bass.AP(dram_ap.tensor, dram_ap.offset + off, [[pstride, 128], [rowlen, 8], [1, 128]])

            BTs = sb(e1, "BTs", [128, 5, 8, 128], BF16)
            BTw = sb(e1, "BTw", [128, 8, 8, 128], BF16)
            B_BT = Buf()
            for tr in range(-1, 4):
                dma("sp", lambda e, tr=tr: e.dma_start(out=BTs[:, tr + 1], in_=toep(fs_d, 128 * (3 - tr), 1, 768)), B_BT, r=[D_fs], w=[B_BT])
            for tr in range(-4, 4):
                dma("sp", lambda e, tr=tr: e.dma_start(out=BTw[:, tr + 4], in_=toep(fw_d, 128 * (3 - tr), 1, 1152)), B_BT, r=[D_fw], w=[B_BT])
            bk48 = sb(e1, "bk48", [48, 8, 128])
            Bq = sb(e1, "Bq", [128, 8, 48])
            B_bk, B_Bq = Buf(), Buf()
            dma("sp", lambda e: e.dma_start(out=bk48[:], in_=bass.AP(fq_d.tensor, fq_d.offset, [[16, 48], [880, 8], [1, 128]])),
                B_bk, r=[D_fq], w=[B_bk])
            for h in range(8):
                op("pe", lambda e, h=h: e.matmul(ms[:, h * 48:(h + 1) * 48], lhsT=bk48[:, h, :], rhs=anti48[:], start=True, stop=True),
                   r=[B_bk, B_c1], w=[B_ms])
            op("act", lambda e: e.activation(out=Bq[:].rearrange("p h c -> p (h c)"), in_=ms[:, 0:384], func=AF.Copy), r=[B_ms], w=[B_Bq])

            KW = sb(e1, "KW", [128, 8, 128], BF16)
            VW = sb(e1, "VW", [128, 8, 2, 65], BF16)
            B_KW = [Buf() for _ in range(8)]
            B_VW = [Buf() for _ in range(8)]
            win_loaded = set()
            kss = sb(e1, "kss2", [128, 2])
            QAs = [[[sb(e1, "QA%d%d%d" % (q, g, c), [128, 512], BF16) for c in range(NCH)] for g in range(2)] for q in range(2)]
            B_QAs = [[[Buf() for _ in range(NCH)] for _ in range(2)] for q in range(2)]
            prs2 = sb(e1, "prs2", [128, 2])
            stepper = [None]

            def step():
                g_ = stepper[0]
                if g_ is not None:
                    try:
                        next(g_)
                    except StopIteration:
                        stepper[0] = None
            PT = [sb(e1, "PT%d" % k, [128, 512], BF16) for k in range(3)]
            B_PT = [Buf(), Buf(), Buf()]
            STR = [st[0][:], st[1][:], pj[:, 512:1024]]
            B_STR = [B_st[0], B_st[1], B_pjB]
            BTc = sb(e1, "BTc", [128, 2, 8, 128], BF16)
            B_BTc = Buf()
            Ph = sb(e1, "Ph", [128, 1024])
            prs = sb(e1, "prs", [128, 1])
            acc0_ = sb(e1, "acc0", [128, 1032])
            acc = [acc0_, acc0_]
            imp = sb(e1, "imp", [128, 256])
            scv = sb(e1, "scv", [128, NSEL])
            scw = sb(e1, "scw", [128, NSEL])
            m8 = sb(e1, "m8", [128, 8])
            thr = sb(e1, "thr", [128, 1])
            mvb = sb(e1, "mvb", [128, NCH, 2, 64], BF16)
            oT = sb(e1, "oT", [65, 512])
            obr = sb(e1, "obr", [128, 3, 2, 4, 65])
            gt = sb(e1, "gt", [128, 24])
            rinv = sb(e1, "rinv", [128, 24])
            comb = sb(e1, "comb", [128, 512])
            csq = sb(e1, "csq", [128, 512])
            css = sb(e1, "css", [128, 8])
            mixb = sb(e1, "mixb", [128, 512], BF16)
            mixT = sb(e1, "mixT", [128, 512], BF16)
            B_Ph, B_prs, B_imp, B_scv, B_scw, B_m8, B_thr, B_mvb = Buf(), Buf(), Buf(), Buf(), Buf(), Buf(), Buf(), Buf()
            B_acc0_ = Buf()
            B_acc = [B_acc0_, B_acc0_]
            B_oT, B_obr, B_gt, B_rinv, B_comb, B_csq, B_css, B_mixb, B_mixT = (Buf() for _ in range(9))
            op("pool", lambda e: e.memset(acc0_[:], 0.0), w=[B_acc0_])
            op("pool", lambda e: e.memset(mvb[:], NEG), w=[B_mvb])

            st_i = [0]
            pt_i = [0]
            oa_i = [0]

            def attend(branch, g, tiles, qrows):
                oi = oa_i[0] % 2
                oa_i[0] += 1
                n = len(tiles)
                pend = []
                npv = [0]

                def pv(item, first, last):
                    first = (npv[0] == 0)
                    npv[0] += 1
                    pti, V_ap, vbufs = item
                    op("pe", lambda e: e.matmul(oa[oi][0:65, :], lhsT=V_ap, rhs=PT[pti][:], start=first, stop=last),
                       r=vbufs + [B_PT[pti]], w=[B_oa[oi]])

                for idx, (lhsT_ap, kbufs, rhs_ap, qbufs, bias_rhs, V_ap, vbufs) in enumerate(tiles):
                    si = st_i[0] % 3
                    st_i[0] += 1
                    pti = pt_i[0] % 3
                    pt_i[0] += 1
                    op("pe", lambda e: e.matmul(STR[si], lhsT=lhsT_ap, rhs=rhs_ap, start=True, stop=(bias_rhs is None)),
                       r=kbufs + qbufs, w=[B_STR[si]])
                    if bias_rhs is not None:
                        b_ap, b_bufs = bias_rhs
                        op("pe", lambda e: e.matmul(STR[si], lhsT=antib[:], rhs=b_ap, start=False, stop=True),
                           r=[B_cst] + b_bufs, w=[B_STR[si]])
                    op("act", lambda e: e.activation(out=PT[pti][:], in_=STR[si], func=AF.Exp), r=[B_STR[si]], w=[B_PT[pti]])
                    pend.append((pti, V_ap, vbufs))
                    if len(pend) > 2:
                        pv(pend.pop(0), False, False)
                    if idx % 3 == 2:
                        step()
                while len(pend) > 1:
                    pv(pend.pop(0), False, False)
                pv(pend.pop(0), False, True)
                op("act", lambda e: e.activation(out=oT[:], in_=oa[oi][0:65, :], func=AF.Copy), r=[B_oa[oi]], w=[B_oT])
                for h in range(4):
                    op("pe", lambda e, h=h: e.transpose(out=ms[:, h * 65:(h + 1) * 65], in_=oT[:, h * 128:(h + 1) * 128],
                                                        identity=identf[0:65, 0:65]), r=[B_oT, B_cst], w=[B_ms])
                op("act", lambda e: e.activation(out=obr[:, branch, g].rearrange("p h d -> p (h d)"), in_=ms[:, 0:260], func=AF.Copy),
                   r=[B_ms], w=[B_obr])

            def prep_gen(i):
                nch = (8 * i + 7) // 64 + 1
                QA, B_QA = QAs[i % 2], B_QAs[i % 2]
                for c in range(nch):
                    dma("sp", lambda e, c=c: e.dma_start(out=QA[0][c][0:64, :], in_=qT_d[i, 0:64, :]), B_QA[0][c], r=[D_qT], w=[B_QA[0][c]])
                    dma("sp", lambda e, c=c: e.dma_start(out=QA[1][c][64:128, :], in_=qT_d[i, 64:128, :]), B_QA[1][c], r=[D_qT], w=[B_QA[1][c]])
                ncol = min(32 * i + 32, NCMP)
                for g in range(2):
                    qrow = slice(0, 64) if g == 0 else slice(64, 128)
                    for a in range(4):
                        h = g * 4 + a
                        blo = max(32 * i - 16, 0)
                        bhi = min(32 * i + 32, ncol)
                        nchk = 0
                        for c0 in range(0, ncol, 512):
                            n = min(512, ncol - c0)
                            op("pe", lambda e, c0=c0, n=n, a=a: e.matmul(pj[:, 0:n], lhsT=QA[g][0][qrow, a * 128:(a + 1) * 128],
                                                                        rhs=KC[qrow, c0:c0 + n], start=True, stop=True),
                               r=[B_QA[g][0], B_KC], w=[B_pjA])
                            lo_, hi_ = max(blo, c0), min(bhi, c0 + n)
                            if lo_ < hi_:
                                op("dve", lambda e, h=h, lo_=lo_, hi_=hi_, c0=c0: e.tensor_tensor(out=pj[:, lo_ - c0:hi_ - c0], in0=pj[:, lo_ - c0:hi_ - c0],
                                                                                              in1=Bq[:, h, lo_ - (32 * i - 16):hi_ - (32 * i - 16)], op=ALU.add),
                                   r=[B_Bq, B_pjA], w=[B_pjA])
                            op("act", lambda e, c0=c0, n=n, nchk=nchk: e.activation(out=Ph[:, c0:c0 + n], in_=pj[:, 0:n], func=AF.Exp,
                                                                                  accum_out=prs2[:, nchk:nchk + 1]),
                               r=[B_pjA], w=[B_Ph, B_prs])
                            nchk += 1
                            yield
                        if nchk == 2:
                            op("dve", lambda e: e.tensor_tensor(out=prs[:], in0=prs2[:, 0:1], in1=prs2[:, 1:2], op=ALU.add), r=[B_prs], w=[B_prs])
                        else:
                            op("dve", lambda e: e.tensor_copy(out=prs[:], in_=prs2[:, 0:1]), r=[B_prs], w=[B_prs])
                        op("dve", lambda e: e.tensor_scalar_max(out=prs[:], in0=prs[:], scalar1=1e-30), r=[B_prs], w=[B_prs])
                        op("dve", lambda e: e.reciprocal(out=prs[:], in_=prs[:]), r=[B_prs], w=[B_prs])
                        if a == 0:
                            op("dve", lambda e: e.tensor_scalar(out=acc[g][:, 4:4 + ncol], in0=Ph[:, 0:ncol], scalar1=prs[:, 0:1], scalar2=None,
                                                                op0=ALU.mult), r=[B_Ph, B_prs], w=[B_acc[g]])
                        else:
                            op("dve", lambda e: e.scalar_tensor_tensor(out=acc[g][:, 4:4 + ncol], in0=Ph[:, 0:ncol], scalar=prs[:, 0:1],
                                                                       in1=acc[g][:, 4:4 + ncol], op0=ALU.mult, op1=ALU.add),
                               r=[B_Ph, B_prs, B_acc[g]], w=[B_acc[g]])
                        yield
                    nm = 8 * i + 8
                    op("dve", lambda e: e.tensor_reduce(out=imp[:, 0:nm], in_=acc[g][:, 4:4 + 4 * nm].rearrange("p (m f) -> p m f", f=4),
                                                        axis=AX.X, op=ALU.add), r=[B_acc[g]], w=[B_imp])
                    op("dve", lambda e: e.tensor_tensor(out=imp[:, 0:nm], in0=imp[:, 0:nm],
                                                        in1=acc[g][:, 0:4 * nm].rearrange("p (m f) -> p m f", f=4)[:, :, 3], op=ALU.add),
                       r=[B_imp, B_acc[g]], w=[B_imp])
                    op("pool", lambda e: e.memset(scv[:], -1.0), w=[B_scv])
                    mlo = max(8 * i - 1, 0)
                    if mlo > 0:
                        op("dve", lambda e: e.tensor_copy(out=scv[:, 0:mlo], in_=imp[:, 0:mlo]), r=[B_imp], w=[B_scv])
                    cl = mlo - (8 * i - 1)
                    op("dve", lambda e: e.tensor_tensor(out=scv[:, mlo:nm], in0=imp[:, mlo:nm], in1=hi9[:, cl:9], op=ALU.min), r=[B_imp, B_c1], w=[B_scv])
                    op("dve", lambda e: e.tensor_tensor(out=scv[:, mlo:nm], in0=scv[:, mlo:nm], in1=lo9[:, cl:9], op=ALU.max), r=[B_c1, B_scv], w=[B_scv])
                    op("dve", lambda e: e.memset(scv[:, 0:1], 1e4), w=[B_scv])
                    yield
                    op("dve", lambda e: e.max(out=m8[:], in_=scv[:]), r=[B_scv], w=[B_m8])
                    op("dve", lambda e: e.match_replace(out=scw[:], in_to_replace=m8[:], in_values=scv[:], imm_value=-2.0), r=[B_scv, B_m8], w=[B_scw])
                    op("dve", lambda e: e.max(out=m8[:], in_=scw[:]), r=[B_scw], w=[B_m8])
                    op("dve", lambda e: e.tensor_scalar_max(out=thr[:], in0=m8[:, 7:8], scalar1=0.0), r=[B_m8], w=[B_thr])
                    op("dve", lambda e: e.tensor_scalar(out=scw[:], in0=scv[:], scalar1=thr[:, 0:1], scalar2=-NEG, op0=ALU.is_ge, op1=ALU.mult),
                       r=[B_scv, B_thr], w=[B_scw])
                    ncb = nch * 64
                    nv = min(ncb, NSEL)
                    if nv < 64:
                        op("dve", lambda e, g=g, nv=nv: e.tensor_scalar_add(out=mvb[:, 0, 1 - g, 0:nv], in0=scw[:, 0:nv], scalar1=NEG), r=[B_scw], w=[B_mvb])
                    else:
                        op("dve", lambda e, g=g, nv=nv: e.tensor_scalar_add(out=mvb[:, 0:nv // 64, 1 - g, :], in0=scw[:, 0:nv].rearrange("p (c m) -> p c m", m=64),
                                                                            scalar1=NEG), r=[B_scw], w=[B_mvb])
                    yield
                for c in range(nch):
                    op("pe", lambda e, c=c: e.transpose(out=tpb[:, c * 128:(c + 1) * 128], in_=mvb[:, c].rearrange("p g m -> p (g m)"),
                                                        identity=identb[:]), r=[B_mvb, B_cst], w=[B_tpb])
                for c in range(nch):
                    op("act", lambda e, c=c: e.activation(out=QA[1][c][0:64, :].rearrange("p (h q) -> p h q", h=4),
                                                          in_=tpb[0:64, c * 128:(c + 1) * 128].unsqueeze(1).to_broadcast([64, 4, 128]), func=AF.Copy),
                       r=[B_tpb], w=[B_QA[1][c]])
                    op("act", lambda e, c=c: e.activation(out=QA[0][c][64:128, :].rearrange("p (h q) -> p h q", h=4),
                                                          in_=tpb[64:128, c * 128:(c + 1) * 128].unsqueeze(1).to_broadcast([64, 4, 128]), func=AF.Copy),
                       r=[B_tpb], w=[B_QA[0][c]])

            stepper[0] = prep_gen(0)
            while stepper[0] is not None:
                step()
            for J in range(NOWN):
                i = J
                for t in range(max(4 * i - 4, 0), 4 * i + 4):
                    if t in win_loaded:
                        continue
                    win_loaded.add(t)
                    dma("sp", lambda e, t=t: e.dma_start(out=KW[:, t % 8, :], in_=kw_d[t]), B_KW[t % 8], r=[D_kw[t]], w=[B_KW[t % 8]])
                    dma("sp", lambda e, t=t: e.dma_start(out=VW[:, t % 8].rearrange("p g d -> p (g d)"), in_=vw_d[t]), B_VW[t % 8],
                        r=[D_vw[t]], w=[B_VW[t % 8]])
                QA, B_QA = QAs[i % 2], B_QAs[i % 2]
                dma("sp", lambda e: e.dma_start(out=gt[:], in_=gates_d[i]), B_gt, r=[D_gates], w=[B_gt])
                stepper[0] = prep_gen(i + 1) if i + 1 < NOWN else None
                i4 = i % 4
                tnl = i // 4
                dma("sp", lambda e: e.dma_start(out=BTc[:, 0], in_=toep(fk_d, 512 * i4, 16, 4224)), B_BTc, r=[D_fk], w=[B_BTc])
                if i4 == 0 and tnl >= 1:
                    dma("sp", lambda e: e.dma_start(out=BTc[:, 1], in_=toep(fk_d, 2048, 16, 4224)), B_BTc, r=[D_fk], w=[B_BTc])
                for g in range(2):
                    qrow = slice(0, 64) if g == 0 else slice(64, 128)
                    hs = slice(g * 4, g * 4 + 4)
                    tiles = []
                    for tn in range(tnl + 1):
                        bias = None
                        if tn == tnl:
                            bias = (BTc[:, 0, hs].rearrange("p h q -> p (h q)"), [B_BTc])
                        elif tn == tnl - 1 and i4 == 0:
                            bias = (BTc[:, 1, hs].rearrange("p h q -> p (h q)"), [B_BTc])
                        tiles.append((KC[qrow, tn * 128:(tn + 1) * 128], [B_KC], QA[g][0][qrow, :], [B_QA[g][0]], bias, VC[:, tn, g, :], [B_VC]))
                    attend(0, g, tiles, qrow)
                    tiles = []
                    for t in range(4 * i + 4):
                        tr = t - 4 * i
                        bias = None
                        if tr >= -1:
                            bias = (BTs[:, tr + 1, hs].rearrange("p h q -> p (h q)"), [B_BT])
                        c = t // 32
                        tiles.append((KA[g][:, t * 128:(t + 1) * 128], [B_KA[g][t], B_init], QA[g][c][:], [B_QA[g][c]], bias, VS[:, t, g, :], [B_VS[t]]))
                    attend(1, g, tiles, None)
                    tiles = []
                    for tr in range(-4, 4):
                        t = 4 * i + tr
                        if t < 0:
                            continue
                        bias = (BTw[:, tr + 4, hs].rearrange("p h q -> p (h q)"), [B_BT])
                        tiles.append((KW[qrow, t % 8, :], [B_KW[t % 8]], QA[g][0][qrow, :], [B_QA[g][0]], bias, VW[:, t % 8, g, :], [B_VW[t % 8]]))
                    attend(2, g, tiles, qrow)
                while stepper[0] is not None:
                    step()
                op("dve", lambda e: e.tensor_scalar_max(out=rinv[:].rearrange("p (h b) -> p b h", b=3),
                                                        in0=obr[:, :, :, :, 64].rearrange("p b g a -> p b (g a)"), scalar1=1e-30), r=[B_obr], w=[B_rinv])
                op("dve", lambda e: e.reciprocal(out=rinv[:], in_=rinv[:]), r=[B_rinv], w=[B_rinv])
                op("dve", lambda e: e.tensor_tensor(out=rinv[:], in0=rinv[:], in1=gt[:], op=ALU.mult), r=[B_rinv, B_gt], w=[B_rinv])
                for br in range(3):
                    src = obr[:, br, :, :, 0:64].rearrange("p g a d -> p (g a) d")
                    wv = rinv[:].rearrange("p (h b) -> p h b", b=3)[:, :, br].unsqueeze(2).to_broadcast([128, 8, 64])
                    if br == 0:
                        op("dve", lambda e, src=src, wv=wv: e.tensor_tensor(out=comb[:].rearrange("p (h d) -> p h d", d=64), in0=src, in1=wv, op=ALU.mult),
                           r=[B_obr, B_rinv], w=[B_comb])
                    else:
                        op("dve", lambda e, src=src, wv=wv: e.tensor_tensor(out=csq[:].rearrange("p (h d) -> p h d", d=64), in0=src, in1=wv, op=ALU.mult),
                           r=[B_obr, B_rinv], w=[B_csq])
                        op("pool", lambda e: e.tensor_tensor(out=comb[:], in0=comb[:], in1=csq[:], op=ALU.add), r=[B_csq, B_comb], w=[B_comb])
                op("pool", lambda e: e.tensor_tensor(out=csq[:], in0=comb[:], in1=comb[:], op=ALU.mult), r=[B_comb], w=[B_csq])
                op("dve", lambda e: e.tensor_reduce(out=css[:], in_=csq[:].rearrange("p (h d) -> p h d", d=64), axis=AX.X, op=ALU.add), r=[B_csq], w=[B_css])
                rstd_from_ss(css[:], [B_css], 1.0 / 64)
                op("dve", lambda e: e.tensor_tensor(out=csq[:].rearrange("p (h d) -> p h d", d=64), in0=comb[:].rearrange("p (h d) -> p h d", d=64),
                                                    in1=css[:].unsqueeze(2).to_broadcast([128, 8, 64]), op=ALU.mult), r=[B_comb, B_css], w=[B_csq])
                op("dve", lambda e: e.tensor_tensor(out=mixb[:], in0=csq[:], in1=onar[:], op=ALU.mult), r=[B_csq, B_kwr], w=[B_mixb])
                for a in range(4):
                    op("pe", lambda e, a=a: e.transpose(out=tpb[:, a * 128:(a + 1) * 128], in_=mixb[:, a * 128:(a + 1) * 128], identity=identb[:]),
                       r=[B_mixb, B_cst], w=[B_tpb])
                op("act", lambda e: e.activation(out=mixT[:], in_=tpb[:, 0:512], func=AF.Copy), r=[B_tpb], w=[B_mixT])
                dma("sp", lambda e: e.dma_start(out=mixa_d[i], in_=mixT[:]), B_mixT, r=[B_mixT], w=[D_mixa])
            sc.barrier()
            e1b_.close()
        eKV.close()

        widx = sb(es, "widx", [128, NBLK], I32)
        B_widx = Buf()
        dest = sb(es, "dest", [128, NOWN, 2], I32)
        wts = sb(es, "wts", [128, NOWN, 2])
        B_dest, B_wts = Buf(), Buf()
        with ExitStack() as e2:
            G1 = load_mod(e2, "G1", 2)
            SH2 = load_mod(e2, "SH2", 3)
            A2 = load_mod(e2, "A2", 4)
            wo = sb(e2, "wo", [128, 8, 1024], BF16)
            wr = sb(e2, "wr", [128, 8, 72], BF16)
            B_wo = Buf()
            w_out_v = w_out.rearrange("(kt p) n -> p kt n", p=128)
            w_rt_v = w_rt.rearrange("(kt p) n -> p kt n", p=128)
            for kt in range(8):
                dma("pool", lambda e, kt=kt: e.dma_start(out=wo[:, kt, :], in_=w_out_v[:, kt, :]), B_wo, w=[B_wo])
                dma("pool", lambda e, kt=kt: e.dma_start(out=wr[:, kt, :], in_=w_rt_v[:, kt, :]), B_wo, w=[B_wo])
            triu = sb(e2, "triu", [128, 128], BF16)
            onesb = sb(e2, "onesb", [128, 128], BF16)
            OH = sb(e2, "OH", [128, NOWN * 2, 64])
            rk = sb(e2, "rk", [128, NOWN * 2])
            h2_d = dscr("h2_d", [NOWN, 128, 1024], BF16)
            D_h2 = Buf()
            B_OH = Buf()
            dma("pool", lambda e: e.dma_start(out=triu[:], in_=c_triu), B_c2, w=[B_c2])
            op("dve", lambda e: e.memset(onesb[:], 1.0), w=[B_c2])
            run = sb(e2, "run", [128, 64])
            B_run = Buf()
            op("dve", lambda e: e.memset(run[:], 0.0), w=[B_run])
            OHS = sb(e2, "OHS", [128, NOWN, 64], BF16)
            def make_a2(sl):
                mT = sb(e2, "mT_%d" % sl, [128, 8, 128], BF16)
                xt = sb(e2, "xt2_%d" % sl, [128, 1024])
                x1 = sb(e2, "x1_%d" % sl, [128, 1024])
                scr = sb(e2, "scr2_%d" % sl, [128, 1024])
                ss = sb(e2, "ss2_%d" % sl, [128, 1])
                h2b = sb(e2, "h2b_%d" % sl, [128, 1024], BF16)
                h2T = sb(e2, "h2T_%d" % sl, [128, 1024], BF16)
                lg = sb(e2, "lg_%d" % sl, [128, 72])
                gm = sb(e2, "gm_%d" % sl, [128, 8])
                ohg = sb(e2, "ohg_%d" % sl, [128, 8])
                eg = sb(e2, "eg_%d" % sl, [128, 8])
                gs = sb(e2, "gs_%d" % sl, [128, 1])
                esel = sb(e2, "esel_%d" % sl, [128, 64])
                ein = sb(e2, "ein_%d" % sl, [128, 8])
                em8 = sb(e2, "em8_%d" % sl, [128, 8])
                ohe = sb(e2, "ohe_%d" % sl, [128, 2, 8])
                oh64 = sb(e2, "oh64_%d" % sl, [128, 2, 64])
                ohs = sb(e2, "ohs_%d" % sl, [128, 64], BF16)
                slot = sb(e2, "slot_%d" % sl, [128, 64])
                dsf = sb(e2, "dsf_%d" % sl, [128, 2])
                wk = sb(e2, "wk_%d" % sl, [128, 2])
                tmp64 = sb(e2, "tmp64_%d" % sl, [128, 64])
                B_mT, B_xt2, B_x1, B_scr2, B_h2b, B_h2T, B_lg = (Buf() for _ in range(7))
                B_rt = Buf()
                TP, B_TP = (tpb[:], B_tpb) if sl == 0 else (st[0][:].bitcast(BF16), B_st[0])
                PJ = (pj[:, 0:512], pj[:, 512:1024]) if sl == 0 else (oa[0][:], oa[1][:])
                B_PJ = (B_pjA, B_pjB) if sl == 0 else (B_oa[0], B_oa[1])
                MS, B_MS = (ms[:], B_ms) if sl == 0 else (st[1][:], B_st[1])

                def body(i):
                    dma("sp", lambda e: e.dma_start(out=mT[:, 0:4, :].rearrange("p c t -> p (c t)"), in_=mixa_d[i]), B_mT, r=[D_mixa], w=[B_mT])
                    dma("sp", lambda e: e.dma_start(out=mT[:, 4:8, :].rearrange("p c t -> p (c t)"), in_=mixc_d[i]), B_mT, r=[D_mixc], w=[B_mT])
                    dma("sp", lambda e: e.dma_start(out=xt[:], in_=x_own[i * 128:(i + 1) * 128, :]), B_xt2, w=[B_xt2])
                    for hf, Bp in ((0, B_PJ[0]), (1, B_PJ[1])):
                        for kt in range(8):
                            op("pe", lambda e, kt=kt, hf=hf: e.matmul(PJ[hf], lhsT=mT[:, kt, :], rhs=wo[:, kt, hf * 512:(hf + 1) * 512],
                                                                      start=(kt == 0), stop=(kt == 7)), r=[B_mT, B_wo], w=[Bp])
                        op("dve", lambda e, hf=hf: e.tensor_tensor(out=x1[:, hf * 512:(hf + 1) * 512], in0=PJ[hf],
                                                                   in1=G1[:, hf * 512:(hf + 1) * 512], op=ALU.mult), r=[Bp, B_mods], w=[B_x1])
                    op("dve", lambda e: e.tensor_tensor(out=x1[:], in0=x1[:], in1=xt[:], op=ALU.add), r=[B_x1, B_xt2], w=[B_x1])
                    dma("sp", lambda e: e.dma_start(out=x1_d[i * 128:(i + 1) * 128, :], in_=x1[:]), B_x1, r=[B_x1], w=[D_x1])
                    rms_mod(x1[:], h2b[:], A2, SH2, 128, [B_x1], [B_h2b], scr, ss, B_scr2)
                    for kt in range(8):
                        op("pe", lambda e, kt=kt: e.transpose(out=TP[:, kt * 128:(kt + 1) * 128], in_=h2b[:, kt * 128:(kt + 1) * 128], identity=identb[:]),
                           r=[B_h2b, B_cst], w=[B_TP])
                    op("act", lambda e: e.activation(out=h2T[:], in_=TP, func=AF.Copy), r=[B_TP], w=[B_h2T])
                    for kt in range(8):
                        op("pe", lambda e, kt=kt: e.matmul(MS[:, 0:72], lhsT=h2T[:, kt * 128:(kt + 1) * 128], rhs=wr[:, kt, :], start=(kt == 0), stop=(kt == 7)),
                           r=[B_h2T, B_wo], w=[B_MS])
                    R = [B_rt]
                    op("dve", lambda e: e.tensor_tensor(out=lg[:], in0=MS[:, 0:72], in1=brt[:], op=ALU.add), r=[B_MS, B_c2], w=R)
                    op("dve", lambda e: e.max(out=gm[:], in_=lg[:, 0:8]), r=R, w=R)
                    op("dve", lambda e: e.tensor_scalar(out=ohg[:], in0=lg[:, 0:8], scalar1=gm[:, 0:1], scalar2=None, op0=ALU.is_ge), r=R, w=R)
                    op("dve", lambda e: e.tensor_scalar(out=eg[:], in0=lg[:, 0:8], scalar1=gm[:, 0:1], scalar2=None, op0=ALU.subtract), r=R, w=R)
                    op("act", lambda e: e.activation(out=eg[:], in_=eg[:], func=AF.Exp, accum_out=gs[:]), r=R, w=R)
                    op("dve", lambda e: e.reciprocal(out=gs[:], in_=gs[:]), r=R, w=R)
                    op("dve", lambda e: e.tensor_tensor(out=esel[:].rearrange("p (g x) -> p g x", x=8), in0=lg[:, 8:72].rearrange("p (g x) -> p g x", x=8),
                                                        in1=ohg[:].unsqueeze(2).to_broadcast([128, 8, 8]), op=ALU.mult), r=R, w=R)
                    op("dve", lambda e: e.tensor_reduce(out=ein[:], in_=esel[:].rearrange("p (g x) -> p x g", x=8), axis=AX.X, op=ALU.add), r=R, w=R)
                    op("dve", lambda e: e.max(out=em8[:], in_=ein[:]), r=R, w=R)
                    for k in range(2):
                        op("dve", lambda e, k=k: e.tensor_scalar(out=ohe[:, k, :], in0=ein[:], scalar1=em8[:, k:k + 1], scalar2=None, op0=ALU.is_equal), r=R, w=R)
                    op("dve", lambda e: e.tensor_tensor(out=wk[:, 0:1], in0=em8[:, 1:2], in1=em8[:, 0:1], op=ALU.subtract), r=R, w=R)
                    op("act", lambda e: e.activation(out=wk[:, 0:1], in_=wk[:, 0:1], func=AF.Exp), r=R, w=R)
                    op("dve", lambda e: e.tensor_scalar_add(out=wk[:, 0:1], in0=wk[:, 0:1], scalar1=1.0), r=R, w=R)
                    op("dve", lambda e: e.reciprocal(out=wk[:, 0:1], in_=wk[:, 0:1]), r=R, w=R)
                    op("dve", lambda e: e.tensor_scalar(out=wk[:, 1:2], in0=wk[:, 0:1], scalar1=-1.0, scalar2=1.0, op0=ALU.mult, op1=ALU.add), r=R, w=R)
                    op("dve", lambda e: e.tensor_scalar(out=wts[:, i, :], in0=wk[:], scalar1=gs[:, 0:1], scalar2=None, op0=ALU.mult), r=R, w=R + [B_wts])
                    for k in range(2):
                        op("dve", lambda e, k=k: e.tensor_tensor(out=oh64[:, k, :].rearrange("p (g x) -> p g x", x=8),
                                                                 in0=ohg[:].unsqueeze(2).to_broadcast([128, 8, 8]),
                                                                 in1=ohe[:, k, :].unsqueeze(1).to_broadcast([128, 8, 8]), op=ALU.mult), r=R, w=R)
                    op("dve", lambda e: e.tensor_tensor(out=OHS[:, i, :], in0=oh64[:, 0, :], in1=oh64[:, 1, :], op=ALU.add), r=R, w=R + [B_OH])
                    for k in range(2):
                        op("pool", lambda e, k=k: e.tensor_copy(out=OH[:, 2 * i + k, :], in_=oh64[:, k, :]), r=R, w=[B_OH])
                    dma("sp", lambda e: e.dma_start(out=h2_d[i], in_=h2b[:]), B_h2b, r=[B_h2b], w=[D_h2])
                return body

            weave_slots(make_a2, 2, NOWN)
            slot = sb(e2, "slot_p", [128, 64])
            tmp64 = sb(e2, "tmp64_p", [128, 64])
            R = [Buf()]
            for i in range(NOWN):
                op("pe", lambda e: e.matmul(ms[:, 128:192], lhsT=triu[:], rhs=OHS[:, i, :], start=True, stop=True), r=[B_OH, B_c2], w=[B_ms])
                op("dve", lambda e: e.tensor_tensor(out=slot[:], in0=ms[:, 128:192], in1=run[:], op=ALU.add), r=[B_ms, B_run], w=R)
                op("pe", lambda e: e.matmul(ms[:, 192:256], lhsT=onesb[:], rhs=OHS[:, i, :], start=True, stop=True), r=[B_OH, B_c2], w=[B_ms])
                op("dve", lambda e: e.tensor_tensor(out=run[:], in0=run[:], in1=ms[:, 192:256], op=ALU.add), r=[B_ms, B_run], w=[B_run])
                for k in range(2):
                    op("dve", lambda e, k=k: e.tensor_tensor(out=tmp64[:], in0=slot[:], in1=OH[:, 2 * i + k, :], op=ALU.mult), r=R + [B_OH], w=R)
                    op("dve", lambda e, k=k: e.tensor_reduce(out=rk[:, 2 * i + k:2 * i + k + 1], in_=tmp64[:], axis=AX.X, op=ALU.add), r=R, w=R + [B_OH])
            cnt = run
            pe_a = sb(e2, "pe_a", [128, 64])
            pe_b = sb(e2, "pe_b", [128, 64])
            padd = sb(e2, "padd", [128, 64])
            pst = sb(e2, "pst", [128, 64])
            R2 = [Buf()]
            blkc = sb(e2, "blkc", [128, NBLK])
            pidx = sb(e2, "pidx", [128, 1])
            dma("sp", lambda e: e.dma_start(out=blkc[:], in_=c_blk), B_c2, w=[B_c2])
            dma("sp", lambda e: e.dma_start(out=pidx[:], in_=c_pidx), B_c2, w=[B_c2])
            cmp3 = sb(e2, "cmp3", [128, NBLK, 64])
            cmpk = cmp3[:].rearrange("p b e -> p (b e)")[:, 0:64 * NOWN].rearrange("p (e k) -> p e k", k=NOWN)
            op("dve", lambda e: e.tensor_tensor(out=cmpk, in0=cnt[:].unsqueeze(2).to_broadcast([128, 64, NOWN]),
                                                in1=blkc[:, 0:NOWN].unsqueeze(1).to_broadcast([128, 64, NOWN]), op=ALU.is_gt), r=[B_run, B_c2], w=R2)
            op("dve", lambda e: e.tensor_reduce(out=padd[:], in_=cmpk, axis=AX.X, op=ALU.add), r=R2, w=R2)
            op("dve", lambda e: e.tensor_scalar_mul(out=padd[:], in0=padd[:], scalar1=128.0), r=R2, w=R2)
            op("dve", lambda e: e.tensor_copy(out=pe_a[:], in_=padd[:]), r=R2, w=R2)
            cur, oth = pe_a, pe_b
            for sft in (1, 2, 4, 8, 16, 32):
                op("dve", lambda e, cur=cur, oth=oth, sft=sft: e.tensor_copy(out=oth[:, 0:sft], in_=cur[:, 0:sft]), r=R2, w=R2)
                op("dve", lambda e, cur=cur, oth=oth, sft=sft: e.tensor_tensor(out=oth[:, sft:64], in0=cur[:, sft:64], in1=cur[:, 0:64 - sft], op=ALU.add),
                   r=R2, w=R2)
                cur, oth = oth, cur
            pend_ = cur
            op("dve", lambda e: e.tensor_tensor(out=pst[:], in0=pend_[:], in1=padd[:], op=ALU.subtract), r=R2, w=R2)
            ber = sb(e2, "ber", [128, NBLK])
            bpv = sb(e2, "bpv", [128, NBLK])
            idxf = sb(e2, "idxf", [128, NBLK])
            op("dve", lambda e: e.tensor_tensor(out=cmp3[:], in0=pend_[:].unsqueeze(1).to_broadcast([128, NBLK, 64]),
                                                in1=blkc[:].unsqueeze(2).to_broadcast([128, NBLK, 64]), op=ALU.is_le), r=R2 + [B_c2], w=R2)
            op("dve", lambda e: e.tensor_reduce(out=ber[:], in_=cmp3[:], axis=AX.X, op=ALU.add), r=R2, w=R2)
            op("dve", lambda e: e.tensor_scalar_min(out=ber[:], in0=ber[:], scalar1=63.0), r=R2, w=R2)
            op("dve", lambda e: e.memset(bpv[:, 0:2], -1.0), r=R2, w=R2)
            op("dve", lambda e: e.tensor_copy(out=bpv[:, 2:NBLK], in_=ber[:, 0:NBLK - 2]), r=R2, w=R2)
            op("dve", lambda e: e.tensor_tensor(out=bpv[:], in0=bpv[:], in1=ber[:], op=ALU.is_equal), r=R2, w=R2)
            op("dve", lambda e: e.tensor_scalar(out=idxf[:], in0=ber[:], scalar1=128.0, scalar2=pidx[:, 0:1], op0=ALU.mult, op1=ALU.add), r=R2 + [B_c2], w=R2)
            op("dve", lambda e: e.scalar_tensor_tensor(out=idxf[:], in0=bpv[:], scalar=1.0e6, in1=idxf[:], op0=ALU.mult, op1=ALU.add), r=R2, w=R2)
            op("dve", lambda e: e.tensor_copy(out=widx[:], in_=idxf[:]), r=R2, w=[B_widx])
            ohp = cmp3[:, 0:NOWN * 2, :] if NBLK >= NOWN * 2 else None
            op("dve", lambda e: e.tensor_tensor(out=ohp, in0=OH[:], in1=pst[:].unsqueeze(1).to_broadcast([128, NOWN * 2, 64]), op=ALU.mult),
               r=R2 + [B_OH], w=R2)
            op("dve", lambda e: e.tensor_reduce(out=idxf[:, 0:NOWN * 2], in_=ohp, axis=AX.X, op=ALU.add), r=R2, w=R2)
            op("dve", lambda e: e.tensor_tensor(out=idxf[:, 0:NOWN * 2], in0=idxf[:, 0:NOWN * 2], in1=rk[:], op=ALU.add), r=R2 + [B_OH], w=R2)
            op("dve", lambda e: e.tensor_copy(out=dest[:].rearrange("p i k -> p (i k)"), in_=idxf[:, 0:NOWN * 2]), r=R2, w=[B_dest])
            h2sc = [sb(e2, "h2sc%d" % q, [128, 1024], BF16) for q in range(2)]
            B_h2sc = [Buf(), Buf()]
            for i in range(NOWN):
                q = i % 2
                dma("sp", lambda e: e.dma_start(out=h2sc[q][:], in_=h2_d[i]), B_h2sc[q], r=[D_h2], w=[B_h2sc[q]])
                for k in range(2):
                    dma("pool", lambda e, k=k: e.indirect_dma_start(out=xdisp_d[:, :], out_offset=bass.IndirectOffsetOnAxis(ap=dest[:, i, k:k + 1], axis=0),
                                                                    in_=h2sc[q][:], in_offset=None), B_h2sc[q], r=[B_h2sc[q], B_dest], w=[D_xd])
            sc.barrier()

        with ExitStack() as e3:
            Wf = [[sb(e3, "Wf%d_%d" % (m, q), [128, 4096]) for m in range(3)] for q in range(2)]
            B_Wf = [[Buf(), Buf(), Buf()] for q in range(2)]
            Wb = [[sb(e3, "Wb%d_%d" % (m, k), [128, 4096], BF16) for m in range(3)] for k in range(2)]
            B_Wb = [[Buf(), Buf(), Buf()] for _ in range(2)]
            wsrc = (w1, w3, w2)
            bnd_reg = nc.gpsimd.alloc_register('bnd')
            nc.gpsimd.reg_mov(bnd_reg, 64 * 128 - 1)
            cast_eng = ("act", "act", "dve")
            TPm = [tpb[:], ms[:].bitcast(BF16)]
            B_TPm = [B_tpb, B_ms]
            H1p = [st[0][:], oa[0][:]]
            B_H1p = [B_st[0], B_oa[0]]
            H3p = [st[1][:], oa[1][:]]
            B_H3p = [B_st[1], B_oa[1]]

            def make_moe(sl):
                xe = sb(e3, "xe%d" % sl, [128, 1024], BF16)
                xeT = sb(e3, "xeT%d" % sl, [128, 8, 128], BF16)
                hs = sb(e3, "hs%d" % sl, [128, 512])
                h1e = sb(e3, "h1e%d" % sl, [128, 512])
                actb = sb(e3, "actb%d" % sl, [128, 512], BF16)
                aT = sb(e3, "aT%d" % sl, [128, 4, 128], BF16)
                yo = sb(e3, "yo%d" % sl, [128, 1024], BF16)
                B_xe, B_xeT, B_hs, B_h1e, B_actb, B_aT, B_yo = (Buf() for _ in range(7))
                tp, B_tp = TPm[sl], B_TPm[sl]
                h1p, B_h1p, h3p, B_h3p = H1p[sl], B_H1p[sl], H3p[sl], B_H3p[sl]

                def body(b):
                    p = b % 2
                    while b >= 2 and not body_done[b - 2]:
                        weave_yield()
                    dma("sp", lambda e: e.dma_start(out=xe[:], in_=xdisp_d[b * 128:(b + 1) * 128, :]), B_xe, r=[D_xd], w=[B_xe])
                    for kt in range(8):
                        op("pe", lambda e, kt=kt: e.transpose(out=tp[:, kt * 128:(kt + 1) * 128], in_=xe[:, kt * 128:(kt + 1) * 128], identity=identb[:]),
                           r=[B_xe, B_cst], w=[B_tp])
                    op("dve", lambda e: e.tensor_copy(out=xeT[:].rearrange("p k t -> p (k t)"), in_=tp), r=[B_tp], w=[B_xeT])
                    while not cast_issued[b]:
                        weave_yield()
                    W1b = Wb[p][0][:].rearrange("p (k f) -> p k f", k=8)
                    W3b = Wb[p][1][:].rearrange("p (k f) -> p k f", k=8)
                    W2b = Wb[p][2][:].rearrange("p (k f) -> p k f", k=4)
                    for (Wm, mi, dst, Bd) in ((W1b, 0, h1p, B_h1p), (W3b, 1, h3p, B_h3p)):
                        for kt in range(8):
                            op("pe", lambda e, kt=kt, Wm=Wm, dst=dst: e.matmul(dst, lhsT=xeT[:, kt, :], rhs=Wm[:, kt, :], start=(kt == 0), stop=(kt == 7)),
                               r=[B_Wb[p][mi], B_xeT], w=[Bd])
                    op("act", lambda e: e.activation(out=h1e[:], in_=h1p, func=AF.Exp, scale=-1.0), r=[B_h1p], w=[B_h1e])
                    op("act", lambda e: e.activation(out=hs[:], in_=h1p, func=AF.Copy), r=[B_h1p], w=[B_hs])
                    op("dve", lambda e: e.tensor_scalar_add(out=h1e[:], in0=h1e[:], scalar1=1.0), r=[B_h1e], w=[B_h1e])
                    op("dve", lambda e: e.reciprocal(out=h1e[:], in_=h1e[:]), r=[B_h1e], w=[B_h1e])
                    op("dve", lambda e: e.tensor_tensor(out=hs[:], in0=hs[:], in1=h3p, op=ALU.mult), r=[B_hs, B_h3p], w=[B_hs])
                    op("dve", lambda e: e.tensor_tensor(out=actb[:], in0=hs[:], in1=h1e[:], op=ALU.mult), r=[B_hs, B_h1e], w=[B_actb])
                    for ft in range(4):
                        op("pe", lambda e, ft=ft: e.transpose(out=tp[:, ft * 128:(ft + 1) * 128], in_=actb[:, ft * 128:(ft + 1) * 128], identity=identb[:]),
                           r=[B_actb, B_cst], w=[B_tp])
                    op("dve", lambda e: e.tensor_copy(out=aT[:].rearrange("p k t -> p (k t)"), in_=tp[:, 0:512]), r=[B_tp], w=[B_aT])
                    while pj_lock[0]:
                        weave_yield()
                    pj_lock[0] = True
                    for hf, Bp in ((0, B_pjA), (1, B_pjB)):
                        for ft in range(4):
                            op("pe", lambda e, ft=ft, hf=hf: e.matmul(pj[:, hf * 512:(hf + 1) * 512], lhsT=aT[:, ft, :],
                                                                      rhs=W2b[:, ft, hf * 512:(hf + 1) * 512], start=(ft == 0), stop=(ft == 3)),
                               r=[B_aT, B_Wb[p][2]], w=[Bp])
                        op("act", lambda e, hf=hf: e.activation(out=yo[:, hf * 512:(hf + 1) * 512], in_=pj[:, hf * 512:(hf + 1) * 512], func=AF.Copy),
                           r=[Bp], w=[B_yo])
                    pj_lock[0] = False
                    comp_done[b] = True
                    dma("sp", lambda e: e.dma_start(out=ydisp_d[b * 128:(b + 1) * 128, :], in_=yo[:]), B_yo, r=[B_yo], w=[D_yd])
                    body_done[b] = True
                return body

            body_done = [False] * NBLK
            cast_issued = [False] * NBLK
            comp_done = [False] * NBLK
            pj_lock = [False]

            def weights_task():
                for b in range(NBLK):
                    p = b % 2
                    for m in range(3):
                        dma("pool", lambda e, m=m: e.indirect_dma_start(out=Wf[p][m][:], out_offset=None, in_=wsrc[m][:, :],
                                                                        in_offset=bass.IndirectOffsetOnAxis(ap=widx[:, b:b + 1], axis=0),
                                                                        bounds_check=bnd_reg, oob_is_err=False),
                            B_Wf[p][m], r=[B_widx], w=[B_Wf[p][m]])
                    while b >= 2 and not comp_done[b - 2]:
                        weave_yield()
                    for m in range(3):
                        if cast_eng[m] == "act":
                            op("act", lambda e, m=m: e.activation(out=Wb[p][m][:], in_=Wf[p][m][:], func=AF.Copy), r=[B_Wf[p][m]], w=[B_Wb[p][m]])
                        else:
                            op(cast_eng[m], lambda e, m=m: e.tensor_copy(out=Wb[p][m][:], in_=Wf[p][m][:]), r=[B_Wf[p][m]], w=[B_Wb[p][m]])
                    cast_issued[b] = True

            moe_bodies = [make_moe(0), make_moe(1)]
            assert nc.sbuf_bytes_remaining >= 20000, nc.sbuf_bytes_remaining
            weave([weights_task] + [(lambda b=b: moe_bodies[b % 2](b)) for b in range(NBLK)], 3)
            sc.barrier()

        with ExitStack() as e4:
            G2 = load_mod(e4, "G2", 5)
            def make_c(sl):
                y0 = sb(e4, "y0_%d" % sl, [128, 1024], BF16)
                y1 = sb(e4, "y1_%d" % sl, [128, 1024], BF16)
                xr = sb(e4, "xr_%d" % sl, [128, 1024])
                ob = sb(e4, "ob_%d" % sl, [128, 1024])
                B_y0, B_y1, B_xr, B_ob = Buf(), Buf(), Buf(), Buf()

                def body(i):
                    dma("pool", lambda e: e.indirect_dma_start(out=y0[:], out_offset=None, in_=ydisp_d[:, :],
                                                               in_offset=bass.IndirectOffsetOnAxis(ap=dest[:, i, 0:1], axis=0)), B_y0, r=[D_yd, B_dest], w=[B_y0])
                    dma("pool", lambda e: e.indirect_dma_start(out=y1[:], out_offset=None, in_=ydisp_d[:, :],
                                                               in_offset=bass.IndirectOffsetOnAxis(ap=dest[:, i, 1:2], axis=0)), B_y1, r=[D_yd, B_dest], w=[B_y1])
                    dma("sp", lambda e: e.dma_start(out=xr[:], in_=x1_d[i * 128:(i + 1) * 128, :]), B_xr, r=[D_x1], w=[B_xr])
                    op("dve", lambda e: e.tensor_scalar(out=ob[:], in0=y0[:], scalar1=wts[:, i, 0:1], scalar2=None, op0=ALU.mult), r=[B_y0, B_wts], w=[B_ob])
                    op("dve", lambda e: e.scalar_tensor_tensor(out=ob[:], in0=y1[:], scalar=wts[:, i, 1:2], in1=ob[:], op0=ALU.mult, op1=ALU.add),
                       r=[B_y1, B_wts, B_ob], w=[B_ob])
                    op("dve", lambda e: e.tensor_tensor(out=ob[:], in0=ob[:], in1=G2, op=ALU.mult), r=[B_ob, B_mods], w=[B_ob])
                    op("dve", lambda e: e.tensor_tensor(out=ob[:], in0=ob[:], in1=xr[:], op=ALU.add), r=[B_ob, B_xr], w=[B_ob])
                    dma("sp", lambda e: e.dma_start(out=out[i * 128:(i + 1) * 128, :], in_=ob[:]), B_ob, r=[B_ob], w=[])
                return body

            weave_slots(make_c, 2, NOWN)
            sc.barrier()
    return nc


def host_consts(S, C, r):
    c = {}
    c["c_ident"] = np.eye(128, dtype=np.float32)
    c["c_anti"] = np.eye(128, dtype=np.float32)[::-1].copy()
    c["c_anti48"] = np.eye(48, dtype=np.float32)[::-1].copy()
    bd = np.zeros((128, 128), np.float32)
    bd[:64, :64] = 1
    bd[64:, 64:] = 1
    c["c_bd"] = bd
    c["c_triu"] = np.triu(np.ones((128, 128), np.float32), 1)
    er = np.zeros((128, 4096), np.float32)
    k = np.arange(4096)
    er[(k // 64) % 64, k] = 1.0
    er[64 + (k // 64) % 64, k] = 1.0
    c["c_erow"] = er
    v = np.arange(768)
    c["c_ohs"] = oh_table(v - 511 + 128 * r)
    v = np.arange(1152)
    c["c_ohw"] = oh_table(v - 511 + 128 * r, 0, 512)
    w = np.arange(880)
    c["c_ohq"] = oh_table(w + 128 * r - 527)
    w = np.arange(4224)
    c["c_ohk"] = oh_table(w + 128 * r - 2063)
    hi = np.full((128, 9), 3.0e38, np.float32)
    lo = np.full((128, 9), -1.0, np.float32)
    ql = np.arange(128)
    for cc in range(9):
        m_rel = cc - 1
        cur = 2 * r + (ql >= 64)
        forced = (m_rel == cur) | (m_rel == cur - 1)
        invalid = m_rel > cur
        lo[forced, cc] = 1e4
        hi[invalid, cc] = -1.0
    c["c_hi"] = hi
    c["c_lo"] = lo
    c["c_pad"] = np.full((128, 1), 0.0 if r == 0 else 1.0, np.float32)
    NBLK = (S // 512 * 256 + 64 * 127) // 128
    c["c_blk"] = np.tile((np.arange(NBLK, dtype=np.float32) * 128)[None, :], (128, 1))
    c["c_pidx"] = np.arange(128, dtype=np.float32).reshape(128, 1)
    return c


def kernel(x, c, w_ada, b_ada, norm1_w, w_in, q_norm_w, k_norm_w, cmp_pos_k, cmp_pos_v,
           cmp_k_w1, cmp_k_w2, cmp_v_w1, cmp_v_w2, conv_w, out_norm_w, w_out, rel_bias,
           norm2_w, w_group, b_group, w_expert, b_expert, w1, w3, w2, _C=None):
    f = lambda a: np.ascontiguousarray(np.asarray(a, dtype=np.float32))
    x = f(x)
    B, S, D = x.shape
    NB = S // 128
    NOWN = NB // 4
    C = _C if _C is not None else (256 if S >= 8192 else 128)
    nc = build(S, C)

    def pos_lay(p):
        p = f(p)[0]
        return np.ascontiguousarray(p.reshape(16, 2, 64).transpose(1, 2, 0).reshape(128, 16, 1))

    shared = {
        "w_ada": f(w_ada)[0], "b_ada": f(b_ada), "norm1_w": f(norm1_w), "w_in": f(w_in)[0],
        "q_norm_w": f(q_norm_w), "k_norm_w": f(k_norm_w).reshape(1, 192),
        "cmp_pos_k": pos_lay(cmp_pos_k), "cmp_pos_v": pos_lay(cmp_pos_v),
        "cmp_k_w1": f(cmp_k_w1)[0], "cmp_k_w2": f(cmp_k_w2)[0], "cmp_v_w1": f(cmp_v_w1)[0], "cmp_v_w2": f(cmp_v_w2)[0],
        "conv_wl": np.ascontiguousarray(f(conv_w)[0].reshape(3, 4, 128).transpose(2, 1, 0).reshape(128, 12)),
        "onw_c": np.ascontiguousarray(f(out_norm_w)[0, 512:].reshape(4, 128).T),
        "onw_a": np.ascontiguousarray(f(out_norm_w)[:, :512]),
        "w_out": f(w_out)[0], "rel_bias": f(rel_bias), "norm2_w": f(norm2_w),
        "w_rt": np.ascontiguousarray(np.concatenate([f(w_group)[0], f(w_expert)[0]], axis=1)),
        "b_rt": np.ascontiguousarray(np.concatenate([f(b_group), f(b_expert)], axis=1)),
        "w1": np.ascontiguousarray(f(w1)[0].reshape(64, 8, 128, 512).transpose(0, 2, 1, 3).reshape(64 * 128, 4096)),
        "w3": np.ascontiguousarray(f(w3)[0].reshape(64, 8, 128, 512).transpose(0, 2, 1, 3).reshape(64 * 128, 4096)),
        "w2": np.ascontiguousarray(f(w2)[0].reshape(64, 4, 128, 1024).transpose(0, 2, 1, 3).reshape(64 * 128, 4096)),
    }
    cf = f(c)
    in_maps = []
    for core in range(8):
        b, r = core // 4, core % 4
        xb = x[b]
        blocks = xb.reshape(NB, 128, D)
        own = np.ascontiguousarray(blocks[r::4].reshape(NOWN * 128, D))
        prev = np.zeros((NOWN, 2, D), np.float32)
        for i in range(NOWN):
            j = 4 * i + r
            if j > 0:
                prev[i] = xb[128 * j - 2:128 * j]
        m = dict(shared)
        m.update(host_consts(S, C, r))
        m["x_all"] = xb
        m["x_own"] = own
        m["x_prev"] = prev.reshape(NOWN * 2, D)
        m["c_lay"] = np.ascontiguousarray(cf[b].reshape(8, 128).T)
        in_maps.append(m)
    res = run_bass_kernel_spmd(nc, in_maps, core_ids=list(range(8)))
    outp = np.zeros((B, NB, 128, D), np.float32)
    for core in range(8):
        b, r = core // 4, core % 4
        outp[b, r::4] = np.asarray(res.results[core]["out"]).reshape(NOWN, 128, D)
    return outp.reshape(B, S, D)
```

```python
import math
import threading
from contextlib import ExitStack

import numpy as np
import concourse.bass as bass
import concourse.mybir as mybir
from concourse.bass_utils import run_bass_kernel_spmd

F32 = mybir.dt.float32
BF16 = mybir.dt.bfloat16
I32 = mybir.dt.int32
AF = mybir.ActivationFunctionType
ALU = mybir.AluOpType
AX = mybir.AxisListType
NEG = -30000.0
EPS = 1e-6
SAME_ENGINE_SYNC = True


class Buf:
    __slots__ = ("w", "r", "ds", "name")

    def __init__(self, name=""):
        self.w = []
        self.r = {}
        self.ds = {}
        self.name = name


class _Task:
    def __init__(self, fn):
        self.fn = fn
        self.go = threading.Event()
        self.back = threading.Event()
        self.done = False
        self.exc = None
        self.th = threading.Thread(target=self._run, daemon=True)
        self.th.start()

    def _run(self):
        self.go.wait()
        self.go.clear()
        _TL.task = self
        try:
            self.fn()
        except BaseException as e:
            self.exc = e
        self.done = True
        self.back.set()

    def step(self):
        self.go.set()
        self.back.wait()
        self.back.clear()
        if self.exc is not None:
            raise self.exc


_TL = threading.local()


def weave_yield():
    t = getattr(_TL, "task", None)
    if t is not None:
        t.back.set()
        t.go.wait()
        t.go.clear()


def weave(fns, width):
    it = iter(fns)
    active = []
    while True:
        while len(active) < width:
            f_ = next(it, None)
            if f_ is None:
                break
            active.append(_Task(f_))
        if not active:
            break
        for t in list(active):
            t.step()
            if t.done:
                active.remove(t)


def weave_slots(make_body, nslots, nitems):
    bodies = [make_body(k) for k in range(nslots)]
    done = [False] * nitems

    def task(i):
        while i >= nslots and not done[i - nslots]:
            weave_yield()
        bodies[i % nslots](i)
        done[i] = True

    weave([(lambda i=i: task(i)) for i in range(nitems)], nslots)


class Sched:
    def __init__(self, nc, es):
        self.nc = nc
        self.es = es
        self.E = {"pe": nc.tensor, "act": nc.scalar, "dve": nc.vector, "pool": nc.gpsimd, "sp": nc.sync}
        self.sem = {k: es.enter_context(nc.semaphore("cs_" + k)) for k in ("pe", "act", "dve", "pool")}
        self.cnt = {k: 0 for k in self.sem}
        self.seen = {k: {} for k in self.E}
        self.dbufs = []
        self.free_dsems = {"sw": [], "hw": []}
        self.bar = es.enter_context(nc.semaphore("bar"))
        self.barc = 0
        self.nd = 0

    def _wait(self, e, ev):
        if ev is None:
            return
        sem, val, owner = ev
        if owner == e and (e == "pe" or not SAME_ENGINE_SYNC):
            return
        if self.seen[e].get(sem, 0) >= val:
            return
        self.E[e].wait_ge(sem, val)
        self.seen[e][sem] = val

    def _deps(self, e, r, w):
        for b in r:
            for ev in b.w:
                self._wait(e, ev)
        for b in w:
            for ev in b.w:
                self._wait(e, ev)
            for ev in b.r.values():
                self._wait(e, ev)

    def _rec(self, ev, r, w):
        for b in r:
            b.r[ev[0]] = ev
        for b in w:
            if ev[2] == "dma":
                b.w = [o for o in b.w if o[2] == "dma" and o[0] != ev[0]] + [ev]
            else:
                b.w = [ev]
            b.r = {}

    def op(self, e, fn, r=(), w=()):
        self._deps(e, r, w)
        ins = fn(self.E[e])
        self.cnt[e] += 1
        ins.then_inc(self.sem[e], 1)
        self._rec((self.sem[e], self.cnt[e], e), r, w)
        weave_yield()

    def dma(self, q, fn, owner, r=(), w=()):
        self._deps(q, r, w)
        kind = "sw" if q == "pool" else "hw"
        if kind not in owner.ds:
            if self.free_dsems[kind]:
                owner.ds[kind] = list(self.free_dsems[kind].pop())
            else:
                self.nd += 1
                owner.ds[kind] = [self.es.enter_context(self.nc.semaphore("ds%d" % self.nd)), 0]
            self.dbufs.append((owner, kind))
        ins = fn(self.E[q])
        d = owner.ds[kind]
        d[1] += 16
        ins.then_inc(d[0], 16)
        self._rec((d[0], d[1], "dma"), r, w)
        weave_yield()

    def barrier(self):
        for k in self.sem:
            self._wait("sp", (self.sem[k], self.cnt[k], k))
        for b, kind in self.dbufs:
            self._wait("sp", (b.ds[kind][0], b.ds[kind][1], "dma"))
        for b, kind in self.dbufs:
            self.free_dsems[kind].append(tuple(b.ds.pop(kind)))
        self.dbufs = []
        self.barc += 1
        self.E["sp"].sem_inc(self.bar, 1)
        for k in self.sem:
            self.E[k].wait_ge(self.bar, self.barc)


def t5_bucket_np(dist):
    n = np.maximum(dist, 0)
    nf = np.maximum(n, 1).astype(np.float32)
    large = 16 + (np.log(nf / np.float32(16)) / np.float32(math.log(8.0)) * np.float32(16)).astype(np.int32)
    large = np.minimum(large, 31)
    return np.where(n < 16, n, large)


def oh_table(dists, lo_valid=0, hi_valid=None):
    L = len(dists)
    t = np.zeros((33, L), np.float32)
    valid = dists >= lo_valid
    if hi_valid is not None:
        valid &= dists < hi_valid
    bk = t5_bucket_np(dists)
    for i in range(L):
        if valid[i]:
            t[bk[i], i] += 1.0
            t[31, i] -= 1.0
        else:
            t[32, i] = NEG
    return t


def build(S, C):
    NB = S // 128
    NOWN = NB // 4
    NCMP = S // 16 - 1
    NSEL = S // 64
    NCH = (NSEL + 63) // 64
    NCT = (NCMP + 127) // 128
    NBLK = (NOWN * 256 + 64 * 127) // 128
    NROWS = NBLK * 128

    nc = bass.Bass("TRN2", target_bir_lowering=False)

    def din(name, shape, dt=F32):
        return nc.dram_tensor(name, list(shape), dt, kind="ExternalInput").ap()

    def dscr(name, shape, dt):
        return nc.dram_tensor(name, list(shape), dt).ap()

    x_all = din("x_all", [S, 1024])
    x_own = din("x_own", [NOWN * 128, 1024])
    x_prev = din("x_prev", [NOWN * 2, 1024])
    c_lay = din("c_lay", [128, 8])
    w_ada = din("w_ada", [1024, 6144])
    b_ada = din("b_ada", [1, 6144])
    norm1_w = din("norm1_w", [1, 1024])
    w_in = din("w_in", [1024, 2840])
    q_norm_w = din("q_norm_w", [1, 64])
    k_norm_w = din("k_norm_w", [1, 192])
    cmp_pos_k = din("cmp_pos_k", [128, 16, 1])
    cmp_pos_v = din("cmp_pos_v", [128, 16, 1])
    cmp_k_w1 = din("cmp_k_w1", [2048, 256])
    cmp_k_w2 = din("cmp_k_w2", [256, 64])
    cmp_v_w1 = din("cmp_v_w1", [2048, 256])
    cmp_v_w2 = din("cmp_v_w2", [256, 64])
    conv_wl = din("conv_wl", [128, 12])
    onw_c = din("onw_c", [128, 4])
    onw_a = din("onw_a", [1, 512])
    w_out = din("w_out", [1024, 1024])
    rel_bias = din("rel_bias", [32, 8])
    norm2_w = din("norm2_w", [1, 1024])
    w_rt = din("w_rt", [1024, 72])
    b_rt = din("b_rt", [1, 72])
    w1 = din("w1", [64 * 128, 4096])
    w3 = din("w3", [64 * 128, 4096])
    w2 = din("w2", [64 * 128, 4096])
    c_blk = din("c_blk", [128, NBLK])
    c_pidx = din("c_pidx", [128, 1])
    c_ident = din("c_ident", [128, 128])
    c_anti = din("c_anti", [128, 128])
    c_anti48 = din("c_anti48", [48, 48])
    c_bd = din("c_bd", [128, 128])
    c_triu = din("c_triu", [128, 128])
    c_erow = din("c_erow", [128, 4096])
    c_ohs = din("c_ohs", [33, 768])
    c_ohw = din("c_ohw", [33, 1152])
    c_ohq = din("c_ohq", [33, 880])
    c_ohk = din("c_ohk", [33, 4224])
    c_hi = din("c_hi", [128, 9])
    c_lo = din("c_lo", [128, 9])
    c_pad = din("c_pad", [128, 1])
    out = nc.dram_tensor("out", [NOWN * 128, 1024], F32, kind="ExternalOutput").ap()

    qT_d = dscr("qT_d", [NOWN, 128, 512], BF16)
    gates_d = dscr("gates_d", [NOWN, 128, 24], F32)
    mixc_d = dscr("mixc_d", [NOWN, 128, 512], BF16)
    mixa_d = dscr("mixa_d", [NOWN, 128, 512], BF16)
    x1_d = dscr("x1_d", [NOWN * 128, 1024], F32)
    fs_d = dscr("fs_d", [8, 768], BF16)
    fw_d = dscr("fw_d", [8, 1152], BF16)
    fq_d = dscr("fq_d", [8, 880], F32)
    fk_d = dscr("fk_d", [8, 4224], BF16)
    xdisp_d = dscr("xdisp_d", [NROWS, 1024], BF16)
    ydisp_d = dscr("ydisp_d", [NROWS, 1024], BF16)
    D_qT, D_gates, D_mixc, D_mixa, D_x1 = Buf(), Buf(), Buf(), Buf(), Buf()
    D_fs, D_fw, D_fq, D_fk, D_xd, D_yd = Buf(), Buf(), Buf(), Buf(), Buf(), Buf()

    with ExitStack() as es:
        sc = Sched(nc, es)
        op, dma = sc.op, sc.dma

        def sb(es_, name, shape, dt=F32):
            return es_.enter_context(nc.sbuf_tensor(name, list(shape), dt))

        tpb = es.enter_context(nc.psum_tensor("tpb", [128, 1024], BF16))
        pj = es.enter_context(nc.psum_tensor("pj", [128, 1024], F32))
        st = [es.enter_context(nc.psum_tensor("st%d" % i, [128, 512], F32)) for i in range(2)]
        oa = [es.enter_context(nc.psum_tensor("oa%d" % i, [128, 512], F32)) for i in range(2)]
        ms = es.enter_context(nc.psum_tensor("ms", [128, 512], F32))
        B_tpb, B_pjA, B_pjB, B_ms = Buf(), Buf(), Buf(), Buf()
        B_st = [Buf(), Buf()]
        B_oa = [Buf(), Buf()]

        identb = sb(es, "identb", [128, 128], BF16)
        antib = sb(es, "antib", [128, 128], BF16)
        identf = sb(es, "identf", [128, 128])
        ones1 = sb(es, "ones1", [1, 128])
        epsc = sb(es, "epsc", [128, 1])
        B_cst = Buf()
        dma("pool", lambda e: e.dma_start(out=identb[:], in_=c_ident), B_cst, w=[B_cst])
        dma("pool", lambda e: e.dma_start(out=antib[:], in_=c_anti), B_cst, w=[B_cst])
        dma("sp", lambda e: e.dma_start(out=identf[:], in_=c_ident), B_cst, w=[B_cst])
        B_ones = Buf()
        op("dve", lambda e: e.memset(ones1[:], 1.0), w=[B_ones])
        op("dve", lambda e: e.memset(epsc[:], EPS), w=[B_ones])
        qwr = sb(es, "qwr", [128, 64])
        kwr = sb(es, "kwr", [128, 192])
        onar = sb(es, "onar", [128, 512])
        brt = sb(es, "brt", [128, 72])
        B_qwr, B_kwr, B_c2 = Buf(), Buf(), Buf()
        esu = ExitStack()
        zt = sb(esu, "zt", [128, 1024], BF16)
        B_zt = Buf()
        op("pool", lambda e: e.memset(zt[:], 0.0), w=[B_zt])
        for k in range(NROWS // 128):
            dma("sp", lambda e, k=k: e.dma_start(out=xdisp_d[k * 128:(k + 1) * 128, :], in_=zt[:]), B_zt, r=[B_zt], w=[])
        D_xd.w = [(B_zt.ds["hw"][0], B_zt.ds["hw"][1], "dma")]

        def bcast_row(dst_ps, dbuf, row_ap, n):
            op("pe", lambda e: e.matmul(dst_ps, lhsT=ones1[0:1, :], rhs=row_ap, start=True, stop=True),
               r=[B_ones, B_row], w=[dbuf])

        def rstd_from_ss(ss_ap, bufs, inv_n):
            op("act", lambda e: e.activation(out=ss_ap, in_=ss_ap, func=AF.Ln, scale=inv_n, bias=epsc[0:ss_ap.shape[0], :]),
               r=bufs + [B_ones], w=bufs)
            op("act", lambda e: e.activation(out=ss_ap, in_=ss_ap, func=AF.Exp, scale=-0.5), r=bufs, w=bufs)

        rows = sb(esu, "rows", [1, 1024 + 1024 + 64 + 192 + 512 + 72])
        B_row = Buf()
        R_N1, R_N2, R_QW, R_KW, R_ONA, R_BRT = 0, 1024, 2048, 2112, 2304, 2816
        for src, off, n in ((norm1_w, R_N1, 1024), (norm2_w, R_N2, 1024), (q_norm_w, R_QW, 64),
                            (k_norm_w, R_KW, 192), (onw_a, R_ONA, 512), (b_rt, R_BRT, 72)):
            dma("sp", lambda e, src=src, off=off, n=n: e.dma_start(out=rows[0:1, off:off + n], in_=src), B_row, w=[B_row])

        cond = sb(esu, "cond", [128, 8])
        condr = sb(esu, "condr", [128, 8, 128])
        B_cond = Buf()
        dma("sp", lambda e: e.dma_start(out=cond[:], in_=c_lay), B_cond, w=[B_cond])
        ctmp = sb(esu, "ctmp", [128, 8])
        B_ctmp = Buf()
        op("act", lambda e: e.activation(out=ctmp[:], in_=cond[:], func=AF.Exp, scale=-1.0), r=[B_cond], w=[B_ctmp])
        op("dve", lambda e: e.tensor_scalar_add(out=ctmp[:], in0=ctmp[:], scalar1=1.0), r=[B_ctmp], w=[B_ctmp])
        op("dve", lambda e: e.reciprocal(out=ctmp[:], in_=ctmp[:]), r=[B_ctmp], w=[B_ctmp])
        op("dve", lambda e: e.tensor_mul(out=cond[:], in0=cond[:], in1=ctmp[:]), r=[B_ctmp, B_cond], w=[B_cond])
        B_condr = Buf()
        op("dve", lambda e: e.tensor_copy(out=condr[:], in_=cond[:].unsqueeze(2).to_broadcast([128, 8, 128])),
           r=[B_cond], w=[B_condr])
        w_ada_v = w_ada.rearrange("(kt p) n -> p kt n", p=128)
        MODS = sb(esu, "MODS", [128, 6, 1024])
        B_mods = Buf()

        def compute_mods():
            with ExitStack() as es2:
                was = [sb(es2, "wa%d" % q, [128, 8, 512]) for q in range(2)]
                brows = [sb(es2, "brow%d" % q, [1, 512]) for q in range(2)]
                B_was, B_brows = [Buf(), Buf()], [Buf(), Buf()]
                for ch in range(12):
                    c0 = ch * 512
                    wa, brow, B_wa, B_brow = was[ch % 2], brows[ch % 2], B_was[ch % 2], B_brows[ch % 2]
                    dma("sp", lambda e: e.dma_start(out=wa[:], in_=w_ada_v[:, :, c0:c0 + 512]), B_wa, w=[B_wa])
                    dma("sp", lambda e: e.dma_start(out=brow[:], in_=b_ada[0:1, c0:c0 + 512]), B_brow, w=[B_brow])
                    for kt in range(8):
                        op("pe", lambda e, kt=kt: e.matmul(pj[:, 0:512], lhsT=condr[:, kt, :], rhs=wa[:, kt, :],
                                                           start=(kt == 0), stop=False),
                           r=[B_condr, B_wa], w=[B_pjA])
                    op("pe", lambda e: e.matmul(pj[:, 0:512], lhsT=ones1[0:1, :], rhs=brow[:], start=False, stop=True),
                       r=[B_ones, B_brow], w=[B_pjA])
                    op("act", lambda e: e.activation(out=MODS[:, ch // 2, (ch % 2) * 512:(ch % 2) * 512 + 512],
                                                     in_=pj[:, 0:512], func=AF.Copy), r=[B_pjA], w=[B_mods])
                for (slot, roff) in ((1, R_N1), (4, R_N2)):
                    for hh in range(2):
                        bcast_row(pj[:, 0:512], B_pjA, rows[0:1, roff + hh * 512: roff + hh * 512 + 512], 512)
                        seg = MODS[:, slot, hh * 512:(hh + 1) * 512]
                        op("dve", lambda e, seg=seg: e.scalar_tensor_tensor(out=seg, in0=seg, scalar=1.0, in1=pj[:, 0:512],
                                                                            op0=ALU.add, op1=ALU.mult),
                           r=[B_pjA, B_mods], w=[B_mods])

        compute_mods()
        mods_d = dscr("mods_d", [128, 6144], F32)
        D_mods = Buf()
        dma("sp", lambda e: e.dma_start(out=mods_d, in_=MODS[:].rearrange("p k n -> p (k n)")), B_mods, r=[B_mods], w=[D_mods])
        bcast_row(ms[:, 0:64], B_ms, rows[0:1, R_QW:R_QW + 64], 64)
        op("act", lambda e: e.activation(out=qwr[:], in_=ms[:, 0:64], func=AF.Copy, scale=0.125), r=[B_ms], w=[B_qwr])
        bcast_row(ms[:, 0:192], B_ms, rows[0:1, R_KW:R_KW + 192], 192)
        op("act", lambda e: e.activation(out=kwr[:], in_=ms[:, 0:192], func=AF.Copy), r=[B_ms], w=[B_kwr])
        bcast_row(ms[:, 0:512], B_ms, rows[0:1, R_ONA:R_ONA + 512], 512)
        op("act", lambda e: e.activation(out=onar[:], in_=ms[:, 0:512], func=AF.Copy), r=[B_ms], w=[B_kwr])
        bcast_row(ms[:, 0:72], B_ms, rows[0:1, R_BRT:R_BRT + 72], 72)
        op("act", lambda e: e.activation(out=brt[:], in_=ms[:, 0:72], func=AF.Copy), r=[B_ms], w=[B_c2])
        sc.barrier()
        esu.close()

        def load_mod(es_, name, k):
            t = sb(es_, name, [128, 1024])
            dma("sp", lambda e: e.dma_start(out=t[:], in_=mods_d[:, k * 1024:(k + 1) * 1024]), B_mods, r=[D_mods], w=[B_mods])
            return t[:]

        eKV = ExitStack()
        KA = [sb(eKV, "KA%d" % g, [128, S], BF16) for g in range(2)]
        VS = sb(eKV, "VS", [128, NB, 2, 65], BF16)
        KC = sb(eKV, "KC", [128, NCT * 128], BF16)
        VC = sb(eKV, "VC", [128, NCT, 2, 65], BF16)
        kw_d = dscr("kw_d", [NB, 128, 128], BF16)
        vw_d = dscr("vw_d", [NB, 128, 130], BF16)
        D_kw = [Buf() for _ in range(NB)]
        D_vw = [Buf() for _ in range(NB)]
        B_KA = [[Buf() for _ in range(NB)] for _ in range(2)]
        B_VS = [Buf() for _ in range(NB)]
        B_KC, B_VC = Buf(), Buf()
        B_init = Buf()
        op("pool", lambda e: e.memset(VS[:, :, :, 64:65], 1.0), w=B_VS)
        op("pool", lambda e: e.memset(VC[:], 0.0), w=[B_VC])
        op("pool", lambda e: e.memset(VC[:, :, :, 64:65], 1.0), w=[B_VC])
        op("pool", lambda e: e.memset(KC[:], 0.0), w=[B_KC])
        for per in range(S // 4096 if S >= 4096 else 1):
            n = min(4096, S)
            dma("pool", lambda e, per=per, n=n: e.dma_start(out=KA[0][64:128, per * 4096:per * 4096 + n], in_=c_erow[64:128, 0:n]),
                B_init, w=[B_init] + B_KA[0])
            dma("pool", lambda e, per=per, n=n: e.dma_start(out=KA[1][0:64, per * 4096:per * 4096 + n], in_=c_erow[0:64, 0:n]),
                B_init, w=[B_init] + B_KA[1])
        eA = ExitStack()
        SH1 = load_mod(eA, "SH1", 0)
        A1 = load_mod(eA, "A1", 1)

        def rms_mod(xt_ap, hb_ap, A, Bm, npart, bufs_x, bufs_h, scr, ss, B_scr):
            op("act", lambda e: e.activation(out=scr[0:npart, :], in_=xt_ap, func=AF.Square, accum_out=ss[0:npart, :]),
               r=bufs_x, w=[B_scr])
            rstd_from_ss(ss[0:npart, :], [B_scr], 1.0 / 1024)
            op("dve", lambda e: e.scalar_tensor_tensor(out=scr[0:npart, :], in0=xt_ap, scalar=ss[0:npart, 0:1],
                                                       in1=A[0:npart, :], op0=ALU.mult, op1=ALU.mult),
               r=bufs_x + [B_scr, B_mods], w=[B_scr])
            op("dve", lambda e: e.tensor_tensor(out=hb_ap, in0=scr[0:npart, :], in1=Bm[0:npart, :], op=ALU.add),
               r=[B_scr, B_mods], w=bufs_h)

        w_in_v = w_in.rearrange("(kt p) n -> p kt n", p=128)

        with ExitStack() as e0:
            wq = sb(e0, "wq", [128, 8, 512], BF16)
            wg = sb(e0, "wg", [128, 8, 24], BF16)
            wc = sb(e0, "wc", [128, 8, 1536], BF16)
            B_w0 = Buf()
            for kt in range(8):
                dma("pool", lambda e, kt=kt: e.dma_start(out=wq[:, kt, :], in_=w_in_v[:, kt, 0:512]), B_w0, w=[B_w0])
                dma("pool", lambda e, kt=kt: e.dma_start(out=wg[:, kt, :], in_=w_in_v[:, kt, 1280:1304]), B_w0, w=[B_w0])
                dma("pool", lambda e, kt=kt: e.dma_start(out=wc[:, kt, :], in_=w_in_v[:, kt, 1304:2840]), B_w0, w=[B_w0])
            convw = sb(e0, "convw", [128, 12])
            onwc = sb(e0, "onwc", [128, 4])
            bdm = sb(e0, "bdm", [128, 128])
            padf = sb(e0, "padf", [128, 1])
            B_c0 = Buf()
            for t_, s_ in ((convw, conv_wl), (onwc, onw_c), (bdm, c_bd), (padf, c_pad)):
                dma("sp", lambda e, t_=t_, s_=s_: e.dma_start(out=t_[:], in_=s_), B_c0, w=[B_c0])

            def make_a0(sl):
                xt = sb(e0, "xt0_%d" % sl, [128, 1024])
                xp = sb(e0, "xp0_%d" % sl, [2, 1024])
                scr = sb(e0, "scr0_%d" % sl, [128, 1024])
                ss = sb(e0, "ss0_%d" % sl, [128, 1])
                hb = sb(e0, "hb0_%d" % sl, [128, 1024], BF16)
                hbp = sb(e0, "hbp0_%d" % sl, [2, 1024], BF16)
                hT = sb(e0, "hT0_%d" % sl, [128, 8, 130], BF16)
                qf = sb(e0, "qf_%d" % sl, [128, 512])
                qsq = sb(e0, "qsq_%d" % sl, [128, 512])
                qss = sb(e0, "qss_%d" % sl, [128, 8])
                qp = sb(e0, "qp_%d" % sl, [128, 512], BF16)
                qTs = sb(e0, "qTs_%d" % sl, [128, 512], BF16)
                gts = sb(e0, "gts_%d" % sl, [128, 24])
                uT = sb(e0, "uT_%d" % sl, [128, 130])
                cu = sb(e0, "cu_%d" % sl, [128, 130])
                yc = sb(e0, "yc_%d" % sl, [128, 128])
                ysq = sb(e0, "ysq_%d" % sl, [128, 128])
                yrs = sb(e0, "yrs_%d" % sl, [128, 128])
                mixc = sb(e0, "mixc_%d" % sl, [128, 4, 128], BF16)
                B_xt, B_xp, B_scr, B_hb, B_hbp, B_hT = Buf(), Buf(), Buf(), Buf(), Buf(), Buf()
                B_qf, B_qsq, B_qss, B_qp, B_qTs, B_gts = Buf(), Buf(), Buf(), Buf(), Buf(), Buf()
                B_uT, B_cu, B_yc, B_ysq, B_yrs, B_mixc = Buf(), Buf(), Buf(), Buf(), Buf(), Buf()
                TP, B_TP = (tpb[:], B_tpb) if sl == 0 else (oa[0][:].bitcast(BF16), B_oa[0])
                PQ, B_PQ = (pj[:, 0:512], B_pjA) if sl == 0 else (pj[:, 512:1024], B_pjB)
                MS, B_MS = (ms[:], B_ms) if sl == 0 else (oa[1][:], B_oa[1])
                CV, B_CV = st[sl], B_st[sl]

                def body(i):
                    dma("sp", lambda e: e.dma_start(out=xt[:], in_=x_own[i * 128:(i + 1) * 128, :]), B_xt, w=[B_xt])
                    dma("sp", lambda e: e.dma_start(out=xp[:], in_=x_prev[i * 2:(i + 1) * 2, :]), B_xp, w=[B_xp])
                    rms_mod(xt[:], hb[:], A1, SH1, 128, [B_xt], [B_hb], scr, ss, B_scr)
                    rms_mod(xp[:], hbp[:], A1, SH1, 2, [B_xp], [B_hbp], scr, ss, B_scr)
                    for kt in range(8):
                        op("pe", lambda e, kt=kt: e.transpose(out=TP[:, kt * 128:(kt + 1) * 128], in_=hb[:, kt * 128:(kt + 1) * 128],
                                                              identity=identb[:]), r=[B_hb, B_cst], w=[B_TP])
                    op("act", lambda e: e.activation(out=hT[:, :, 2:130], in_=TP.rearrange("p (k t) -> p k t", k=8), func=AF.Copy),
                       r=[B_TP], w=[B_hT])
                    for kt in range(8):
                        op("pe", lambda e, kt=kt: e.transpose(out=TP[:, kt * 2:(kt + 1) * 2], in_=hbp[:, kt * 128:(kt + 1) * 128],
                                                              identity=identb[0:2, 0:2]), r=[B_hbp, B_cst], w=[B_TP])
                    op("act", lambda e: e.activation(out=hT[:, :, 0:2], in_=TP[:, 0:16].rearrange("p (k t) -> p k t", k=8), func=AF.Copy),
                       r=[B_TP], w=[B_hT])
                    for kt in range(8):
                        op("pe", lambda e, kt=kt: e.matmul(PQ, lhsT=hT[:, kt, 2:130], rhs=wq[:, kt, :],
                                                           start=(kt == 0), stop=(kt == 7)), r=[B_hT, B_w0], w=[B_PQ])
                    for kt in range(8):
                        op("pe", lambda e, kt=kt: e.matmul(MS[:, 0:24], lhsT=hT[:, kt, 2:130], rhs=wg[:, kt, :],
                                                           start=(kt == 0), stop=(kt == 7)), r=[B_hT, B_w0], w=[B_MS])
                    op("act", lambda e: e.activation(out=gts[:], in_=MS[:, 0:24], func=AF.Exp, scale=-1.0), r=[B_MS], w=[B_gts])
                    op("dve", lambda e: e.tensor_scalar_add(out=gts[:], in0=gts[:], scalar1=1.0), r=[B_gts], w=[B_gts])
                    op("dve", lambda e: e.reciprocal(out=gts[:], in_=gts[:]), r=[B_gts], w=[B_gts])
                    dma("sp", lambda e: e.dma_start(out=gates_d[i], in_=gts[:]), B_gts, r=[B_gts], w=[D_gates])
                    op("act", lambda e: e.activation(out=qf[:], in_=PQ, func=AF.Copy), r=[B_PQ], w=[B_qf])
                    op("pool", lambda e: e.tensor_tensor(out=qsq[:], in0=qf[:], in1=qf[:], op=ALU.mult), r=[B_qf], w=[B_qsq])
                    op("dve", lambda e: e.tensor_reduce(out=qss[:], in_=qsq[:].rearrange("p (h d) -> p h d", d=64), axis=AX.X, op=ALU.add),
                       r=[B_qsq], w=[B_qss])
                    rstd_from_ss(qss[:], [B_qss], 1.0 / 64)
                    op("dve", lambda e: e.tensor_tensor(out=qsq[:].rearrange("p (h d) -> p h d", d=64),
                                                        in0=qf[:].rearrange("p (h d) -> p h d", d=64),
                                                        in1=qss[:].unsqueeze(2).to_broadcast([128, 8, 64]), op=ALU.mult),
                       r=[B_qf, B_qss], w=[B_qsq])
                    op("dve", lambda e: e.tensor_tensor(out=qp[:].rearrange("p (a g d) -> p g a d", a=4, g=2, d=64),
                                                        in0=qsq[:].rearrange("p (g a d) -> p g a d", g=2, a=4, d=64),
                                                        in1=qwr[:].unsqueeze(1).unsqueeze(1).to_broadcast([128, 2, 4, 64]), op=ALU.mult),
                       r=[B_qsq, B_qwr], w=[B_qp])
                    for a in range(4):
                        op("pe", lambda e, a=a: e.transpose(out=TP[:, a * 128:(a + 1) * 128], in_=qp[:, a * 128:(a + 1) * 128],
                                                            identity=identb[:]), r=[B_qp, B_cst], w=[B_TP])
                    op("act", lambda e: e.activation(out=qTs[:], in_=TP[:, 0:512], func=AF.Copy), r=[B_TP], w=[B_qTs])
                    dma("sp", lambda e: e.dma_start(out=qT_d[i], in_=qTs[:]), B_qTs, r=[B_qTs], w=[D_qT])
                    for ct in range(4):
                        ps = CV
                        Bp = B_CV
                        for (k, col0, t0) in ((0, 0, 2), (1, 512, 0), (2, 1024, 0)):
                            nt = 130 - t0
                            for kt in range(8):
                                op("pe", lambda e, kt=kt, k=k, col0=col0, t0=t0, nt=nt: e.matmul(
                                    ps[:, k * 130:k * 130 + nt], lhsT=wc[:, kt, col0 + ct * 128: col0 + ct * 128 + 128],
                                    rhs=hT[:, kt, t0:130], start=(kt == 0), stop=(kt == 7)), r=[B_hT, B_w0], w=[Bp])
                        op("act", lambda e: e.activation(out=uT[:], in_=ps[:, 260:390], func=AF.Copy), r=[Bp], w=[B_uT])
                        op("dve", lambda e: e.tensor_tensor(out=cu[:], in0=ps[:, 130:260], in1=uT[:], op=ALU.mult), r=[Bp, B_uT], w=[B_cu])
                        if i == 0:
                            op("dve", lambda e: e.tensor_scalar(out=cu[:, 0:2], in0=cu[:, 0:2], scalar1=padf[:, 0:1], scalar2=None,
                                                                op0=ALU.mult), r=[B_cu, B_c0], w=[B_cu])
                        op("dve", lambda e: e.tensor_scalar(out=yc[:], in0=cu[:, 0:128], scalar1=convw[:, ct * 3:ct * 3 + 1], scalar2=None,
                                                            op0=ALU.mult), r=[B_cu, B_c0], w=[B_yc])
                        for k in (1, 2):
                            op("dve", lambda e, k=k: e.scalar_tensor_tensor(out=yc[:], in0=cu[:, k:k + 128],
                                                                            scalar=convw[:, ct * 3 + k:ct * 3 + k + 1], in1=yc[:],
                                                                            op0=ALU.mult, op1=ALU.add), r=[B_cu, B_c0, B_yc], w=[B_yc])
                        op("dve", lambda e: e.tensor_tensor(out=yc[:], in0=yc[:], in1=ps[:, 0:128], op=ALU.mult), r=[Bp, B_yc], w=[B_yc])
                        op("pool", lambda e: e.tensor_tensor(out=ysq[:], in0=yc[:], in1=yc[:], op=ALU.mult), r=[B_yc], w=[B_ysq])
                        op("pe", lambda e: e.matmul(MS[:, 0:128], lhsT=bdm[:], rhs=ysq[:], start=True, stop=True), r=[B_c0, B_ysq], w=[B_MS])
                        op("act", lambda e: e.activation(out=yrs[:], in_=MS[:, 0:128], func=AF.Ln, scale=1.0 / 64, bias=epsc[:]),
                           r=[B_MS, B_ones], w=[B_yrs])
                        op("act", lambda e: e.activation(out=yrs[:], in_=yrs[:], func=AF.Exp, scale=-0.5), r=[B_yrs], w=[B_yrs])
                        op("dve", lambda e: e.scalar_tensor_tensor(out=mixc[:, ct, :], in0=yc[:], scalar=onwc[:, ct:ct + 1], in1=yrs[:],
                                                                   op0=ALU.mult, op1=ALU.mult), r=[B_yc, B_yrs, B_c0], w=[B_mixc])
                    dma("sp", lambda e: e.dma_start(out=mixc_d[i], in_=mixc[:].rearrange("p c t -> p (c t)")), B_mixc, r=[B_mixc], w=[D_mixc])
                return body

            weave_slots(make_a0, 2, NOWN)
            sc.barrier()

        with ExitStack() as e1:
            e1a = ExitStack()
            e1_real = e1
            e1 = e1a
            wkv = sb(e1, "wkv", [128, 8, 768], BF16)
            B_wkv = Buf()
            for kt in range(8):
                dma("pool", lambda e, kt=kt: e.dma_start(out=wkv[:, kt, :], in_=w_in_v[:, kt, 512:1280]), B_wkv, w=[B_wkv])
            cw1 = [sb(e1, "cw1_%d" % k, [128, 16, 256], BF16) for k in range(2)]
            cw2 = [sb(e1, "cw2_%d" % k, [128, 2, 64], BF16) for k in range(2)]
            cpos = [sb(e1, "cpos_%d" % k, [128, 16, 1], BF16) for k in range(2)]
            B_cw = Buf()
            for k, (a1_, a2_, ap_) in enumerate(((cmp_k_w1, cmp_k_w2, cmp_pos_k), (cmp_v_w1, cmp_v_w2, cmp_pos_v))):
                dma("pool", lambda e, k=k, a1_=a1_: e.dma_start(out=cw1[k][:], in_=a1_.rearrange("(kt p) n -> p kt n", p=128)), B_cw, w=[B_cw])
                dma("pool", lambda e, k=k, a2_=a2_: e.dma_start(out=cw2[k][:], in_=a2_.rearrange("(kt p) n -> p kt n", p=128)), B_cw, w=[B_cw])
                dma("pool", lambda e, k=k, ap_=ap_: e.dma_start(out=cpos[k][:], in_=ap_), B_cw, w=[B_cw])
            cb1 = [sb(e1, "cb1_%d" % k, [128, 2]) for k in range(2)]
            B_cb1 = Buf()
            for k in range(2):
                for hf in range(2):
                    for kt in range(16):
                        op("pe", lambda e, k=k, hf=hf, kt=kt: e.matmul(ms[:, hf:hf + 1], lhsT=cw1[k][:, kt, hf * 128:(hf + 1) * 128],
                                                                      rhs=cpos[k][:, kt, :], start=(kt == 0), stop=(kt == 15)),
                           r=[B_cw], w=[B_ms])
                op("act", lambda e, k=k: e.activation(out=cb1[k][:], in_=ms[:, 0:2], func=AF.Copy), r=[B_ms], w=[B_cb1])
            KSd = [[[sb(e1, "KS%d%d%d" % (q, k, g), [128, 544], BF16) for g in range(2)] for k in range(2)] for q in range(2)]
            B_KSd = [Buf(), Buf()]
            for q in range(2):
                for k in range(2):
                    for g in range(2):
                        op("pool", lambda e, q=q, k=k, g=g: e.memset(KSd[q][k][g][:], 0.0), w=[B_KSd[q]])
            NSL = 2
            xt = [sb(e1, "xt1_%d" % k, [128, 1024]) for k in range(NSL)]
            scr_ = [sb(e1, "scr1_%d" % k, [128, 1024]) for k in range(NSL)]
            ss_ = [sb(e1, "ss1_%d" % k, [128, 1]) for k in range(NSL)]
            hb_ = [sb(e1, "hb1_%d" % k, [128, 1024], BF16) for k in range(NSL)]
            hT_ = [sb(e1, "hT1_%d" % k, [128, 8, 128], BF16) for k in range(NSL)]
            kvf_ = [sb(e1, "kvf_%d" % k, [128, 768]) for k in range(NSL)]
            ksq_ = [sb(e1, "ksq_%d" % k, [128, 128]) for k in range(NSL)]
            kss_ = [sb(e1, "kss_%d" % k, [128, 2]) for k in range(NSL)]
            knb_ = [sb(e1, "knb_%d" % k, [128, 128], BF16) for k in range(NSL)]
            craw_ = [sb(e1, "craw_%d" % k, [128, 2, 2, 128], BF16) for k in range(NSL)]
            kwst = [sb(e1, "kwst%d" % k, [128, 128], BF16) for k in range(2)]
            vwst = [sb(e1, "vwst%d" % k, [128, 2, 65], BF16) for k in range(2)]
            B_kwst = [Buf(), Buf()]
            B_vwst = [Buf(), Buf()]
            for k in range(2):
                op("pool", lambda e, k=k: e.memset(vwst[k][:, :, 64:65], 1.0), w=[B_vwst[k]])
            B_xt1 = [Buf() for _ in range(NSL)]
            B_scr_ = [Buf() for _ in range(NSL)]
            B_hb_ = [Buf() for _ in range(NSL)]
            B_hT_ = [Buf() for _ in range(NSL)]
            B_kvf_ = [Buf() for _ in range(NSL)]
            B_ksq_ = [Buf() for _ in range(NSL)]
            B_kss_ = [Buf() for _ in range(NSL)]
            B_knb_ = [Buf() for _ in range(NSL)]
            B_craw_ = [Buf() for _ in range(NSL)]
            TPs = [tpb[:], st[0][:].bitcast(BF16)]
            B_TPs = [B_tpb, B_st[0]]
            PAs = [pj[:, 0:512], oa[0][:, 0:512]]
            B_PAs = [B_pjA, B_oa[0]]
            PBs = [pj[:, 512:768], oa[1][:, 0:256]]
            B_PBs = [B_pjB, B_oa[1]]
            tpc = st[1][:].bitcast(BF16)
            B_tpc = B_st[1]
            kssc = sb(e1, "kssc", [128, 2])
            B_kssc = Buf()
            hid = sb(e1, "hid", [128, 2, 32], BF16)
            hidf = sb(e1, "hidf", [128, 32])
            hide = sb(e1, "hide", [128, 32])
            kcf = sb(e1, "kcf", [32, 64])
            kcb = sb(e1, "kcb", [32, 128], BF16)
            B_hid, B_hidf, B_hide, B_kcf, B_kcb = Buf(), Buf(), Buf(), Buf(), Buf()

            def run_interleaved(gens, width):
                active = []
                it = iter(gens)
                while True:
                    while len(active) < width:
                        g_ = next(it, None)
                        if g_ is None:
                            break
                        active.append(g_)
                    if not active:
                        break
                    for g_ in list(active):
                        try:
                            next(g_)
                        except StopIteration:
                            active.remove(g_)

            def proj_block(J, jj):
                j = 4 * J + jj
                sl = j % NSL
                xb_, Bx = xt[sl], B_xt1[sl]
                scr, ss, hb, hT, kvf, ksq, kss, knb, craw = scr_[sl], ss_[sl], hb_[sl], hT_[sl], kvf_[sl], ksq_[sl], kss_[sl], knb_[sl], craw_[sl]
                B_scr1, B_hb1, B_hT1, B_kvf, B_ksq, B_kss, B_knb, B_craw = (B_scr_[sl], B_hb_[sl], B_hT_[sl], B_kvf_[sl], B_ksq_[sl], B_kss_[sl],
                                                                             B_knb_[sl], B_craw_[sl])
                tp, B_tp = TPs[sl], B_TPs[sl]
                KSr = KSd[J % 2]
                B_KS = B_KSd[J % 2]
                dma("sp", lambda e: e.dma_start(out=xb_[:], in_=x_all[j * 128:(j + 1) * 128, :]), Bx, w=[Bx])
                yield
                op("act", lambda e: e.activation(out=scr[:], in_=xb_[:], func=AF.Square, accum_out=ss[:]), r=[Bx], w=[B_scr1])
                yield
                op("act", lambda e: e.activation(out=ss[:], in_=ss[:], func=AF.Ln, scale=1.0 / 1024, bias=epsc[:]), r=[B_scr1, B_ones], w=[B_scr1])
                op("act", lambda e: e.activation(out=ss[:], in_=ss[:], func=AF.Exp, scale=-0.5), r=[B_scr1], w=[B_scr1])
                yield
                op("dve", lambda e: e.scalar_tensor_tensor(out=scr[:], in0=xb_[:], scalar=ss[:, 0:1], in1=A1, op0=ALU.mult, op1=ALU.mult),
                   r=[Bx, B_scr1, B_mods], w=[B_scr1])
                yield
                op("dve", lambda e: e.tensor_tensor(out=hb[:], in0=scr[:], in1=SH1, op=ALU.add), r=[B_scr1, B_mods], w=[B_hb1])
                yield
                for kt in range(8):
                    op("pe", lambda e, kt=kt: e.transpose(out=tp[:, kt * 128:(kt + 1) * 128], in_=hb[:, kt * 128:(kt + 1) * 128],
                                                          identity=identb[:]), r=[B_hb1, B_cst], w=[B_tp])
                yield
                op("act", lambda e: e.activation(out=hT[:].rearrange("p k t -> p (k t)"), in_=tp, func=AF.Copy), r=[B_tp], w=[B_hT1])
                yield
                for (c0, n, pdst, Bp) in ((0, 512, PAs[sl], B_PAs[sl]), (512, 256, PBs[sl], B_PBs[sl])):
                    for kt in range(8):
                        op("pe", lambda e, kt=kt, c0=c0, n=n, pdst=pdst: e.matmul(pdst, lhsT=hT[:, kt, :], rhs=wkv[:, kt, c0:c0 + n],
                                                                                start=(kt == 0), stop=(kt == 7)),
                           r=[B_hT1, B_wkv], w=[Bp])
                yield
                op("act", lambda e: e.activation(out=kvf[:, 0:512], in_=PAs[sl], func=AF.Copy), r=[B_PAs[sl]], w=[B_kvf])
                op("act", lambda e: e.activation(out=kvf[:, 512:768], in_=PBs[sl], func=AF.Copy), r=[B_PBs[sl]], w=[B_kvf])
                yield
                ws2 = j % 2
                op("dve", lambda e: e.tensor_copy(out=VS[:, j, :, 0:64], in_=kvf[:, 384:512].rearrange("p (g d) -> p g d", g=2)),
                   r=[B_kvf], w=[B_VS[j]])
                op("dve", lambda e: e.tensor_copy(out=vwst[ws2][:, :, 0:64], in_=kvf[:, 640:768].rearrange("p (g d) -> p g d", g=2)),
                   r=[B_kvf], w=[B_vwst[ws2]])
                dma("sp", lambda e: e.dma_start(out=vw_d[j], in_=vwst[ws2][:].rearrange("p g d -> p (g d)")), B_vwst[ws2],
                    r=[B_vwst[ws2]], w=[D_vw[j]])
                yield
                for (col0, kw_i, which) in ((256, 1, "slc"), (512, 2, "win")):
                    src = kvf[:, col0:col0 + 128]
                    op("pool", lambda e, src=src: e.tensor_tensor(out=ksq[:], in0=src, in1=src, op=ALU.mult), r=[B_kvf], w=[B_ksq])
                    yield
                    op("dve", lambda e: e.tensor_reduce(out=kss[:], in_=ksq[:].rearrange("p (g d) -> p g d", g=2), axis=AX.X, op=ALU.add),
                       r=[B_ksq], w=[B_kss])
                    yield
                    op("act", lambda e: e.activation(out=kss[:], in_=kss[:], func=AF.Ln, scale=1.0 / 64, bias=epsc[:]), r=[B_kss, B_ones], w=[B_kss])
                    op("act", lambda e: e.activation(out=kss[:], in_=kss[:], func=AF.Exp, scale=-0.5), r=[B_kss], w=[B_kss])
                    yield
                    op("dve", lambda e, src=src: e.tensor_tensor(out=ksq[:].rearrange("p (g d) -> p g d", g=2),
                                                                 in0=src.rearrange("p (g d) -> p g d", g=2),
                                                                 in1=kss[:].unsqueeze(2).to_broadcast([128, 2, 64]), op=ALU.mult),
                       r=[B_kvf, B_kss], w=[B_ksq])
                    op("dve", lambda e, kw_i=kw_i: e.tensor_tensor(out=knb[:].rearrange("p (g d) -> p g d", g=2),
                                                                   in0=ksq[:].rearrange("p (g d) -> p g d", g=2),
                                                                   in1=kwr[:, kw_i * 64:(kw_i + 1) * 64].unsqueeze(1).to_broadcast([128, 2, 64]),
                                                                   op=ALU.mult), r=[B_ksq, B_kwr], w=[B_knb])
                    yield
                    op("pe", lambda e: e.transpose(out=tp[:, 0:128], in_=knb[:], identity=identb[:]), r=[B_knb, B_cst], w=[B_tp])
                    yield
                    if which == "slc":
                        op("act", lambda e: e.activation(out=KA[0][0:64, j * 128:(j + 1) * 128], in_=tp[0:64, 0:128], func=AF.Copy),
                           r=[B_tp], w=[B_KA[0][j]])
                        op("act", lambda e: e.activation(out=KA[1][64:128, j * 128:(j + 1) * 128], in_=tp[64:128, 0:128], func=AF.Copy),
                           r=[B_tp], w=[B_KA[1][j]])
                    else:
                        op("act", lambda e: e.activation(out=kwst[ws2][:], in_=tp[:, 0:128], func=AF.Copy), r=[B_tp], w=[B_kwst[ws2]])
                        dma("sp", lambda e: e.dma_start(out=kw_d[j], in_=kwst[ws2][:]), B_kwst[ws2], r=[B_kwst[ws2]], w=[D_kw[j]])
                    yield
                for k in range(2):
                    base = k * 128
                    op("dve", lambda e, k=k, base=base: e.tensor_copy(out=craw[:, k, 0, :], in_=kvf[:, base:base + 128]), r=[B_kvf], w=[B_craw])
                    op("dve", lambda e, k=k, base=base: e.tensor_copy(out=craw[:, k, 1, 0:64], in_=kvf[:, base + 64:base + 128]), r=[B_kvf], w=[B_craw])
                    op("dve", lambda e, k=k, base=base: e.tensor_copy(out=craw[:, k, 1, 64:128], in_=kvf[:, base:base + 64]), r=[B_kvf], w=[B_craw])
                yield
                for k in range(2):
                    for o in range(2):
                        op("pe", lambda e, k=k, o=o: e.transpose(out=tp[:, (k * 2 + o) * 128:(k * 2 + o + 1) * 128], in_=craw[:, k, o, :],
                                                                identity=identb[:]), r=[B_craw, B_cst], w=[B_tp])
                yield
                c0 = 16 + jj * 128
                for k in range(2):
                    Ta = tp[:, (k * 2) * 128:(k * 2 + 1) * 128]
                    Tb = tp[:, (k * 2 + 1) * 128:(k * 2 + 2) * 128]
                    op("act", lambda e, k=k, Ta=Ta: e.activation(out=KSr[k][0][0:64, c0:c0 + 128], in_=Ta[0:64, :], func=AF.Copy), r=[B_tp], w=[B_KS])
                    op("act", lambda e, k=k, Tb=Tb: e.activation(out=KSr[k][0][64:128, c0 - 1:c0 + 127], in_=Tb[64:128, :], func=AF.Copy), r=[B_tp], w=[B_KS])
                    op("act", lambda e, k=k, Tb=Tb: e.activation(out=KSr[k][1][0:64, c0:c0 + 128], in_=Tb[0:64, :], func=AF.Copy), r=[B_tp], w=[B_KS])
                    op("act", lambda e, k=k, Ta=Ta: e.activation(out=KSr[k][1][64:128, c0 - 1:c0 + 127], in_=Ta[64:128, :], func=AF.Copy), r=[B_tp], w=[B_KS])
                    yield

            def compress_gen(J):
                KSr = KSd[J % 2]
                B_KS = B_KSd[J % 2]
                kss, B_kss = kssc, B_kssc
                n0 = 32 * J - 1
                nlo = max(n0, 0)
                nn = 32 * J + 31 - nlo
                col_lo = 16 * (nlo - n0)
                for k in range(2):
                    for g in range(2):
                        for hf in range(2):
                            for kt in range(16):
                                rhs_ap = bass.AP(KSr[k][g][:].tensor, KSr[k][g][:, col_lo + 2 * kt:col_lo + 2 * kt + 1].offset,
                                                 [[KSr[k][g][:].ap[0][0], 128], [16, nn]])
                                op("pe", lambda e, rhs_ap=rhs_ap, k=k, hf=hf, kt=kt: e.matmul(ms[:, 0:nn], lhsT=cw1[k][:, kt, hf * 128:(hf + 1) * 128],
                                                                                            rhs=rhs_ap, start=(kt == 0), stop=(kt == 15)),
                                   r=[B_KS, B_cw], w=[B_ms])
                            yield
                            op("dve", lambda e, k=k, hf=hf: e.tensor_scalar(out=hidf[:, 0:nn], in0=ms[:, 0:nn], scalar1=cb1[k][:, hf:hf + 1], scalar2=None,
                                                                            op0=ALU.add), r=[B_ms, B_cb1], w=[B_hidf])
                            yield
                            op("act", lambda e: e.activation(out=hide[:, 0:nn], in_=hidf[:, 0:nn], func=AF.Exp, scale=-1.0), r=[B_hidf], w=[B_hide])
                            yield
                            op("dve", lambda e: e.tensor_scalar_add(out=hide[:, 0:nn], in0=hide[:, 0:nn], scalar1=1.0), r=[B_hide], w=[B_hide])
                            op("dve", lambda e: e.reciprocal(out=hide[:, 0:nn], in_=hide[:, 0:nn]), r=[B_hide], w=[B_hide])
                            op("dve", lambda e, hf=hf: e.tensor_tensor(out=hid[:, hf, 0:nn], in0=hidf[:, 0:nn], in1=hide[:, 0:nn], op=ALU.mult),
                               r=[B_hidf, B_hide], w=[B_hid])
                            yield
                        for hf in range(2):
                            op("pe", lambda e, k=k, hf=hf: e.matmul(ms[0:nn, 0:64], lhsT=hid[:, hf, 0:nn], rhs=cw2[k][:, hf, :],
                                                                    start=(hf == 0), stop=(hf == 1)), r=[B_hid, B_cw], w=[B_ms])
                        yield
                        tn, r0 = nlo // 128, nlo % 128
                        if k == 1:
                            op("act", lambda e: e.activation(out=kcb[0:nn, 0:64], in_=ms[0:nn, 0:64], func=AF.Copy), r=[B_ms], w=[B_kcb])
                            n1 = min(nn, 128 - r0)
                            dma("sp", lambda e, g=g, tn=tn, r0=r0, n1=n1: e.dma_start(out=VC[r0:r0 + n1, tn, g, 0:64], in_=kcb[0:n1, 0:64]),
                                B_kcb, r=[B_kcb], w=[B_VC])
                            if n1 < nn:
                                dma("sp", lambda e, g=g, tn=tn, n1=n1: e.dma_start(out=VC[0:nn - n1, tn + 1, g, 0:64], in_=kcb[n1:nn, 0:64]),
                                    B_kcb, r=[B_kcb], w=[B_VC])
                        else:
                            op("act", lambda e: e.activation(out=kcf[0:nn, :], in_=ms[0:nn, 0:64], func=AF.Square, accum_out=kss[0:nn, 0:1]),
                               r=[B_ms], w=[B_kcf, B_kss])
                            yield
                            rstd_from_ss(kss[0:nn, 0:1], [B_kss], 1.0 / 64)
                            yield
                            op("dve", lambda e, g=g: e.scalar_tensor_tensor(out=kcb[0:nn, g * 64:(g + 1) * 64], in0=ms[0:nn, 0:64], scalar=kss[0:nn, 0:1],
                                                                            in1=kwr[0:nn, 0:64], op0=ALU.mult, op1=ALU.mult),
                               r=[B_ms, B_kss, B_kwr], w=[B_kcb])
                            if g == 1:
                                yield
                                op("pe", lambda e: e.transpose(out=tpc[:, 0:nn], in_=kcb[0:nn, :], identity=identb[0:nn, 0:nn]), r=[B_kcb, B_cst], w=[B_tpc])
                                yield
                                op("act", lambda e: e.activation(out=KC[:, nlo:nlo + nn], in_=tpc[:, 0:nn], func=AF.Copy), r=[B_tpc], w=[B_KC])
                        yield

            pending = []
            for J in range(NOWN):
                if J > 0:
                    for k in range(2):
                        for g in range(2):
                            op("dve", lambda e, k=k, g=g: e.tensor_copy(out=KSd[J % 2][k][g][:, 0:16], in_=KSd[(J - 1) % 2][k][g][:, 512:528]),
                               r=[B_KSd[(J - 1) % 2]], w=[B_KSd[J % 2]])
                run_interleaved(pending + [proj_block(J, jj) for jj in range(4)], 3 if pending else 2)
                pending = [compress_gen(J)]
            run_interleaved(pending, 1)

            sc.barrier()
            e1a.close()
            eA.close()
            e1b_ = ExitStack()
            e1 = e1b_
            relb = sb(e1, "relb", [33, 8])
            B_relb = Buf()
            op("dve", lambda e: e.memset(relb[32:33, :], 1.0), w=[B_relb])
            dma("sp", lambda e: e.dma_start(out=relb[0:32, :], in_=rel_bias), B_relb, w=[B_relb])
            hi9 = sb(e1, "hi9", [128, 9])
            lo9 = sb(e1, "lo9", [128, 9])
            anti48 = sb(e1, "anti48", [48, 48])
            B_c1 = Buf()
            for t_, s_ in ((hi9, c_hi), (lo9, c_lo), (anti48, c_anti48)):
                dma("sp", lambda e, t_=t_, s_=s_: e.dma_start(out=t_[:], in_=s_), B_c1, w=[B_c1])
            with ExitStack() as e1t:
                oht = sb(e1t, "oht", [33, 4224])
                ftab = sb(e1t, "ftab", [8, 4224])
                ftabb = sb(e1t, "ftabb", [8, 4224], BF16)
                B_oht, B_ftab, B_ftabb = Buf(), Buf(), Buf()
                for (src, L, dst, Dd, isb) in ((c_ohs, 768, fs_d, D_fs, True), (c_ohw, 1152, fw_d, D_fw, True),
                                               (c_ohq, 880, fq_d, D_fq, False), (c_ohk, 4224, fk_d, D_fk, True)):
                    dma("sp", lambda e, src=src, L=L: e.dma_start(out=oht[:, 0:L], in_=src), B_oht, w=[B_oht])
                    for c0 in range(0, L, 512):
                        n = min(512, L - c0)
                        op("pe", lambda e, c0=c0, n=n: e.matmul(ms[0:8, 0:n], lhsT=relb[:], rhs=oht[:, c0:c0 + n], start=True, stop=True),
                           r=[B_relb, B_oht], w=[B_ms])
                        op("act", lambda e, c0=c0, n=n: e.activation(out=ftab[:, c0:c0 + n], in_=ms[0:8, 0:n], func=AF.Copy),
                           r=[B_ms], w=[B_ftab])
                    if isb:
                        op("dve", lambda e, L=L: e.tensor_copy(out=ftabb[:, 0:L], in_=ftab[:, 0:L]), r=[B_ftab], w=[B_ftabb])
                        dma("sp", lambda e, L=L, dst=dst: e.dma_start(out=dst, in_=ftabb[:, 0:L]), B_ftabb, r=[B_ftabb], w=[Dd])
                    else:
                        dma("sp", lambda e, L=L, dst=dst: e.dma_start(out=dst, in_=ftab[:, 0:L]), B_ftab, r=[B_ftab], w=[Dd])
                sc.barrier()

            def toep(dram_ap, off, pstride, rowlen):
                return bass.AP(dram_ap.tensor, dram_ap.offset + off, [[pstride, 128], [rowlen, 8], [1, 128]])

            BTs = sb(e1, "BTs", [128, 5, 8, 128], BF16)
            BTw = sb(e1, "BTw", [128, 8, 8, 128], BF16)
            B_BT = Buf()
            for tr in range(-1, 4):
                dma("sp", lambda e, tr=tr: e.dma_start(out=BTs[:, tr + 1], in_=toep(fs_d, 128 * (3 - tr), 1, 768)), B_BT, r=[D_fs], w=[B_BT])
            for tr in range(-4, 4):
                dma("sp", lambda e, tr=tr: e.dma_start(out=BTw[:, tr + 4], in_=toep(fw_d, 128 * (3 - tr), 1, 1152)), B_BT, r=[D_fw], w=[B_BT])
            bk48 = sb(e1, "bk48", [48, 8, 128])
            Bq = sb(e1, "Bq", [128, 8, 48])
            B_bk, B_Bq = Buf(), Buf()
            dma("sp", lambda e: e.dma_start(out=bk48[:], in_=bass.AP(fq_d.tensor, fq_d.offset, [[16, 48], [880, 8], [1, 128]])),
                B_bk, r=[D_fq], w=[B_bk])
            for h in range(8):
                op("pe", lambda e, h=h: e.matmul(ms[:, h * 48:(h + 1) * 48], lhsT=bk48[:, h, :], rhs=anti48[:], start=True, stop=True),
                   r=[B_bk, B_c1], w=[B_ms])
            op("act", lambda e: e.activation(out=Bq[:].rearrange("p h c -> p (h c)"), in_=ms[:, 0:384], func=AF.Copy), r=[B_ms], w=[B_Bq])

            KW = sb(e1, "KW", [128, 8, 128], BF16)
            VW = sb(e1, "VW", [128, 8, 2, 65], BF16)
            B_KW = [Buf() for _ in range(8)]
            B_VW = [Buf() for _ in range(8)]
            win_loaded = set()
            kss = sb(e1, "kss2", [128, 2])
            QAs = [[[sb(e1, "QA%d%d%d" % (q, g, c), [128, 512], BF16) for c in range(NCH)] for g in range(2)] for q in range(2)]
            B_QAs = [[[Buf() for _ in range(NCH)] for _ in range(2)] for q in range(2)]
            prs2 = sb(e1, "prs2", [128, 2])
            stepper = [None]

            def step():
                g_ = stepper[0]
                if g_ is not None:
                    try:
                        next(g_)
                    except StopIteration:
                        stepper[0] = None
            PT = [sb(e1, "PT%d" % k, [128, 512], BF16) for k in range(3)]
            B_PT = [Buf(), Buf(), Buf()]
            STR = [st[0][:], st[1][:], pj[:, 512:1024]]
            B_STR = [B_st[0], B_st[1], B_pjB]
            BTc = sb(e1, "BTc", [128, 2, 8, 128], BF16)
            B_BTc = Buf()
            Ph = sb(e1, "Ph", [128, 1024])
            prs = sb(e1, "prs", [128, 1])
            acc0_ = sb(e1, "acc0", [128, 1032])
            acc = [acc0_, acc0_]
            imp = sb(e1, "imp", [128, 256])
            scv = sb(e1, "scv", [128, NSEL])
            scw = sb(e1, "scw", [128, NSEL])
            m8 = sb(e1, "m8", [128, 8])
            thr = sb(e1, "thr", [128, 1])
            mvb = sb(e1, "mvb", [128, NCH, 2, 64], BF16)
            oT = sb(e1, "oT", [65, 512])
            obr = sb(e1, "obr", [128, 3, 2, 4, 65])
            gt = sb(e1, "gt", [128, 24])
            rinv = sb(e1, "rinv", [128, 24])
            comb = sb(e1, "comb", [128, 512])
            csq = sb(e1, "csq", [128, 512])
            css = sb(e1, "css", [128, 8])
            mixb = sb(e1, "mixb", [128, 512], BF16)
            mixT = sb(e1, "mixT", [128, 512], BF16)
            B_Ph, B_prs, B_imp, B_scv, B_scw, B_m8, B_thr, B_mvb = Buf(), Buf(), Buf(), Buf(), Buf(), Buf(), Buf(), Buf()
            B_acc0_ = Buf()
            B_acc = [B_acc0_, B_acc0_]
            B_oT, B_obr, B_gt, B_rinv, B_comb, B_csq, B_css, B_mixb, B_mixT = (Buf() for _ in range(9))
            op("pool", lambda e: e.memset(acc0_[:], 0.0), w=[B_acc0_])
            op("pool", lambda e: e.memset(mvb[:], NEG), w=[B_mvb])

            st_i = [0]
            pt_i = [0]
            oa_i = [0]

            def attend(branch, g, tiles, qrows):
                oi = oa_i[0] % 2
                oa_i[0] += 1
                n = len(tiles)
                pend = []
                npv = [0]

                def pv(item, first, last):
                    first = (npv[0] == 0)
                    npv[0] += 1
                    pti, V_ap, vbufs = item
                    op("pe", lambda e: e.matmul(oa[oi][0:65, :], lhsT=V_ap, rhs=PT[pti][:], start=first, stop=last),
                       r=vbufs + [B_PT[pti]], w=[B_oa[oi]])

                for idx, (lhsT_ap, kbufs, rhs_ap, qbufs, bias_rhs, V_ap, vbufs) in enumerate(tiles):
                    si = st_i[0] % 3
                    st_i[0] += 1
                    pti = pt_i[0] % 3
                    pt_i[0] += 1
                    op("pe", lambda e: e.matmul(STR[si], lhsT=lhsT_ap, rhs=rhs_ap, start=True, stop=(bias_rhs is None)),
                       r=kbufs + qbufs, w=[B_STR[si]])
                    if bias_rhs is not None:
                        b_ap, b_bufs = bias_rhs
                        op("pe", lambda e: e.matmul(STR[si], lhsT=antib[:], rhs=b_ap, start=False, stop=True),
                           r=[B_cst] + b_bufs, w=[B_STR[si]])
                    op("act", lambda e: e.activation(out=PT[pti][:], in_=STR[si], func=AF.Exp), r=[B_STR[si]], w=[B_PT[pti]])
                    pend.append((pti, V_ap, vbufs))
                    if len(pend) > 2:
                        pv(pend.pop(0), False, False)
                    if idx % 3 == 2:
                        step()
                while len(pend) > 1:
                    pv(pend.pop(0), False, False)
                pv(pend.pop(0), False, True)
                op("act", lambda e: e.activation(out=oT[:], in_=oa[oi][0:65, :], func=AF.Copy), r=[B_oa[oi]], w=[B_oT])
                for h in range(4):
                    op("pe", lambda e, h=h: e.transpose(out=ms[:, h * 65:(h + 1) * 65], in_=oT[:, h * 128:(h + 1) * 128],
                                                        identity=identf[0:65, 0:65]), r=[B_oT, B_cst], w=[B_ms])
                op("act", lambda e: e.activation(out=obr[:, branch, g].rearrange("p h d -> p (h d)"), in_=ms[:, 0:260], func=AF.Copy),
                   r=[B_ms], w=[B_obr])

            def prep_gen(i):
                nch = (8 * i + 7) // 64 + 1
                QA, B_QA = QAs[i % 2], B_QAs[i % 2]
                for c in range(nch):
                    dma("sp", lambda e, c=c: e.dma_start(out=QA[0][c][0:64, :], in_=qT_d[i, 0:64, :]), B_QA[0][c], r=[D_qT], w=[B_QA[0][c]])
                    dma("sp", lambda e, c=c: e.dma_start(out=QA[1][c][64:128, :], in_=qT_d[i, 64:128, :]), B_QA[1][c], r=[D_qT], w=[B_QA[1][c]])
                ncol = min(32 * i + 32, NCMP)
                for g in range(2):
                    qrow = slice(0, 64) if g == 0 else slice(64, 128)
                    for a in range(4):
                        h = g * 4 + a
                        blo = max(32 * i - 16, 0)
                        bhi = min(32 * i + 32, ncol)
                        nchk = 0
                        for c0 in range(0, ncol, 512):
                            n = min(512, ncol - c0)
                            op("pe", lambda e, c0=c0, n=n, a=a: e.matmul(pj[:, 0:n], lhsT=QA[g][0][qrow, a * 128:(a + 1) * 128],
                                                                        rhs=KC[qrow, c0:c0 + n], start=True, stop=True),
                               r=[B_QA[g][0], B_KC], w=[B_pjA])
                            lo_, hi_ = max(blo, c0), min(bhi, c0 + n)
                            if lo_ < hi_:
                                op("dve", lambda e, h=h, lo_=lo_, hi_=hi_, c0=c0: e.tensor_tensor(out=pj[:, lo_ - c0:hi_ - c0], in0=pj[:, lo_ - c0:hi_ - c0],
                                                                                              in1=Bq[:, h, lo_ - (32 * i - 16):hi_ - (32 * i - 16)], op=ALU.add),
                                   r=[B_Bq, B_pjA], w=[B_pjA])
                            op("act", lambda e, c0=c0, n=n, nchk=nchk: e.activation(out=Ph[:, c0:c0 + n], in_=pj[:, 0:n], func=AF.Exp,
                                                                                  accum_out=prs2[:, nchk:nchk + 1]),
                               r=[B_pjA], w=[B_Ph, B_prs])
                            nchk += 1
                            yield
                        if nchk == 2:
                            op("dve", lambda e: e.tensor_tensor(out=prs[:], in0=prs2[:, 0:1], in1=prs2[:, 1:2], op=ALU.add), r=[B_prs], w=[B_prs])
                        else:
                            op("dve", lambda e: e.tensor_copy(out=prs[:], in_=prs2[:, 0:1]), r=[B_prs], w=[B_prs])
                        op("dve", lambda e: e.tensor_scalar_max(out=prs[:], in0=prs[:], scalar1=1e-30), r=[B_prs], w=[B_prs])
                        op("dve", lambda e: e.reciprocal(out=prs[:], in_=prs[:]), r=[B_prs], w=[B_prs])
                        if a == 0:
                            op("dve", lambda e: e.tensor_scalar(out=acc[g][:, 4:4 + ncol], in0=Ph[:, 0:ncol], scalar1=prs[:, 0:1], scalar2=None,
                                                                op0=ALU.mult), r=[B_Ph, B_prs], w=[B_acc[g]])
                        else:
                            op("dve", lambda e: e.scalar_tensor_tensor(out=acc[g][:, 4:4 + ncol], in0=Ph[:, 0:ncol], scalar=prs[:, 0:1],
                                                                       in1=acc[g][:, 4:4 + ncol], op0=ALU.mult, op1=ALU.add),
                               r=[B_Ph, B_prs, B_acc[g]], w=[B_acc[g]])
                        yield
                    nm = 8 * i + 8
                    op("dve", lambda e: e.tensor_reduce(out=imp[:, 0:nm], in_=acc[g][:, 4:4 + 4 * nm].rearrange("p (m f) -> p m f", f=4),
                                                        axis=AX.X, op=ALU.add), r=[B_acc[g]], w=[B_imp])
                    op("dve", lambda e: e.tensor_tensor(out=imp[:, 0:nm], in0=imp[:, 0:nm],
                                                        in1=acc[g][:, 0:4 * nm].rearrange("p (m f) -> p m f", f=4)[:, :, 3], op=ALU.add),
                       r=[B_imp, B_acc[g]], w=[B_imp])
                    op("pool", lambda e: e.memset(scv[:], -1.0), w=[B_scv])
                    mlo = max(8 * i - 1, 0)
                    if mlo > 0:
                        op("dve", lambda e: e.tensor_copy(out=scv[:, 0:mlo], in_=imp[:, 0:mlo]), r=[B_imp], w=[B_scv])
                    cl = mlo - (8 * i - 1)
                    op("dve", lambda e: e.tensor_tensor(out=scv[:, mlo:nm], in0=imp[:, mlo:nm], in1=hi9[:, cl:9], op=ALU.min), r=[B_imp, B_c1], w=[B_scv])
                    op("dve", lambda e: e.tensor_tensor(out=scv[:, mlo:nm], in0=scv[:, mlo:nm], in1=lo9[:, cl:9], op=ALU.max), r=[B_c1, B_scv], w=[B_scv])
                    op("dve", lambda e: e.memset(scv[:, 0:1], 1e4), w=[B_scv])
                    yield
                    op("dve", lambda e: e.max(out=m8[:], in_=scv[:]), r=[B_scv], w=[B_m8])
                    op("dve", lambda e: e.match_replace(out=scw[:], in_to_replace=m8[:], in_values=scv[:], imm_value=-2.0), r=[B_scv, B_m8], w=[B_scw])
                    op("dve", lambda e: e.max(out=m8[:], in_=scw[:]), r=[B_scw], w=[B_m8])
                    op("dve", lambda e: e.tensor_scalar_max(out=thr[:], in0=m8[:, 7:8], scalar1=0.0), r=[B_m8], w=[B_thr])
                    op("dve", lambda e: e.tensor_scalar(out=scw[:], in0=scv[:], scalar1=thr[:, 0:1], scalar2=-NEG, op0=ALU.is_ge, op1=ALU.mult),
                       r=[B_scv, B_thr], w=[B_scw])
                    ncb = nch * 64
                    nv = min(ncb, NSEL)
                    if nv < 64:
                        op("dve", lambda e, g=g, nv=nv: e.tensor_scalar_add(out=mvb[:, 0, 1 - g, 0:nv], in0=scw[:, 0:nv], scalar1=NEG), r=[B_scw], w=[B_mvb])
                    else:
                        op("dve", lambda e, g=g, nv=nv: e.tensor_scalar_add(out=mvb[:, 0:nv // 64, 1 - g, :], in0=scw[:, 0:nv].rearrange("p (c m) -> p c m", m=64),
                                                                            scalar1=NEG), r=[B_scw], w=[B_mvb])
                    yield
                for c in range(nch):
                    op("pe", lambda e, c=c: e.transpose(out=tpb[:, c * 128:(c + 1) * 128], in_=mvb[:, c].rearrange("p g m -> p (g m)"),
                                                        identity=identb[:]), r=[B_mvb, B_cst], w=[B_tpb])
                for c in range(nch):
                    op("act", lambda e, c=c: e.activation(out=QA[1][c][0:64, :].rearrange("p (h q) -> p h q", h=4),
                                                          in_=tpb[0:64, c * 128:(c + 1) * 128].unsqueeze(1).to_broadcast([64, 4, 128]), func=AF.Copy),
                       r=[B_tpb], w=[B_QA[1][c]])
                    op("act", lambda e, c=c: e.activation(out=QA[0][c][64:128, :].rearrange("p (h q) -> p h q", h=4),
                                                          in_=tpb[64:128, c * 128:(c + 1) * 128].unsqueeze(1).to_broadcast([64, 4, 128]), func=AF.Copy),
                       r=[B_tpb], w=[B_QA[0][c]])

            stepper[0] = prep_gen(0)
            while stepper[0] is not None:
                step()
            for J in range(NOWN):
                i = J
                for t in range(max(4 * i - 4, 0), 4 * i + 4):
                    if t in win_loaded:
                        continue
                    win_loaded.add(t)
                    dma("sp", lambda e, t=t: e.dma_start(out=KW[:, t % 8, :], in_=kw_d[t]), B_KW[t % 8], r=[D_kw[t]], w=[B_KW[t % 8]])
                    dma("sp", lambda e, t=t: e.dma_start(out=VW[:, t % 8].rearrange("p g d -> p (g d)"), in_=vw_d[t]), B_VW[t % 8],
                        r=[D_vw[t]], w=[B_VW[t % 8]])
                QA, B_QA = QAs[i % 2], B_QAs[i % 2]
                dma("sp", lambda e: e.dma_start(out=gt[:], in_=gates_d[i]), B_gt, r=[D_gates], w=[B_gt])
                stepper[0] = prep_gen(i + 1) if i + 1 < NOWN else None
                i4 = i % 4
                tnl = i // 4
                dma("sp", lambda e: e.dma_start(out=BTc[:, 0], in_=toep(fk_d, 512 * i4, 16, 4224)), B_BTc, r=[D_fk], w=[B_BTc])
                if i4 == 0 and tnl >= 1:
                    dma("sp", lambda e: e.dma_start(out=BTc[:, 1], in_=toep(fk_d, 2048, 16, 4224)), B_BTc, r=[D_fk], w=[B_BTc])
                for g in range(2):
                    qrow = slice(0, 64) if g == 0 else slice(64, 128)
                    hs = slice(g * 4, g * 4 + 4)
                    tiles = []
                    for tn in range(tnl + 1):
                        bias = None
                        if tn == tnl:
                            bias = (BTc[:, 0, hs].rearrange("p h q -> p (h q)"), [B_BTc])
                        elif tn == tnl - 1 and i4 == 0:
                            bias = (BTc[:, 1, hs].rearrange("p h q -> p (h q)"), [B_BTc])
                        tiles.append((KC[qrow, tn * 128:(tn + 1) * 128], [B_KC], QA[g][0][qrow, :], [B_QA[g][0]], bias, VC[:, tn, g, :], [B_VC]))
                    attend(0, g, tiles, qrow)
                    tiles = []
                    for t in range(4 * i + 4):
                        tr = t - 4 * i
                        bias = None
                        if tr >= -1:
                            bias = (BTs[:, tr + 1, hs].rearrange("p h q -> p (h q)"), [B_BT])
                        c = t // 32
                        tiles.append((KA[g][:, t * 128:(t + 1) * 128], [B_KA[g][t], B_init], QA[g][c][:], [B_QA[g][c]], bias, VS[:, t, g, :], [B_VS[t]]))
                    attend(1, g, tiles, None)
                    tiles = []
                    for tr in range(-4, 4):
                        t = 4 * i + tr
                        if t < 0:
                            continue
                        bias = (BTw[:, tr + 4, hs].rearrange("p h q -> p (h q)"), [B_BT])
                        tiles.append((KW[qrow, t % 8, :], [B_KW[t % 8]], QA[g][0][qrow, :], [B_QA[g][0]], bias, VW[:, t % 8, g, :], [B_VW[t % 8]]))
                    attend(2, g, tiles, qrow)
                while stepper[0] is not None:
                    step()
                op("dve", lambda e: e.tensor_scalar_max(out=rinv[:].rearrange("p (h b) -> p b h", b=3),
                                                        in0=obr[:, :, :, :, 64].rearrange("p b g a -> p b (g a)"), scalar1=1e-30), r=[B_obr], w=[B_rinv])
                op("dve", lambda e: e.reciprocal(out=rinv[:], in_=rinv[:]), r=[B_rinv], w=[B_rinv])
                op("dve", lambda e: e.tensor_tensor(out=rinv[:], in0=rinv[:], in1=gt[:], op=ALU.mult), r=[B_rinv, B_gt], w=[B_rinv])
                for br in range(3):
                    src = obr[:, br, :, :, 0:64].rearrange("p g a d -> p (g a) d")
                    wv = rinv[:].rearrange("p (h b) -> p h b", b=3)[:, :, br].unsqueeze(2).to_broadcast([128, 8, 64])
                    if br == 0:
                        op("dve", lambda e, src=src, wv=wv: e.tensor_tensor(out=comb[:].rearrange("p (h d) -> p h d", d=64), in0=src, in1=wv, op=ALU.mult),
                           r=[B_obr, B_rinv], w=[B_comb])
                    else:
                        op("dve", lambda e, src=src, wv=wv: e.tensor_tensor(out=csq[:].rearrange("p (h d) -> p h d", d=64), in0=src, in1=wv, op=ALU.mult),
                           r=[B_obr, B_rinv], w=[B_csq])
                        op("pool", lambda e: e.tensor_tensor(out=comb[:], in0=comb[:], in1=csq[:], op=ALU.add), r=[B_csq, B_comb], w=[B_comb])
                op("pool", lambda e: e.tensor_tensor(out=csq[:], in0=comb[:], in1=comb[:], op=ALU.mult), r=[B_comb], w=[B_csq])
                op("dve", lambda e: e.tensor_reduce(out=css[:], in_=csq[:].rearrange("p (h d) -> p h d", d=64), axis=AX.X, op=ALU.add), r=[B_csq], w=[B_css])
                rstd_from_ss(css[:], [B_css], 1.0 / 64)
                op("dve", lambda e: e.tensor_tensor(out=csq[:].rearrange("p (h d) -> p h d", d=64), in0=comb[:].rearrange("p (h d) -> p h d", d=64),
                                                    in1=css[:].unsqueeze(2).to_broadcast([128, 8, 64]), op=ALU.mult), r=[B_comb, B_css], w=[B_csq])
                op("dve", lambda e: e.tensor_tensor(out=mixb[:], in0=csq[:], in1=onar[:], op=ALU.mult), r=[B_csq, B_kwr], w=[B_mixb])
                for a in range(4):
                    op("pe", lambda e, a=a: e.transpose(out=tpb[:, a * 128:(a + 1) * 128], in_=mixb[:, a * 128:(a + 1) * 128], identity=identb[:]),
                       r=[B_mixb, B_cst], w=[B_tpb])
                op("act", lambda e: e.activation(out=mixT[:], in_=tpb[:, 0:512], func=AF.Copy), r=[B_tpb], w=[B_mixT])
                dma("sp", lambda e: e.dma_start(out=mixa_d[i], in_=mixT[:]), B_mixT, r=[B_mixT], w=[D_mixa])
            sc.barrier()
            e1b_.close()
        eKV.close()

        widx = sb(es, "widx", [128, NBLK], I32)
        B_widx = Buf()
        dest = sb(es, "dest", [128, NOWN, 2], I32)
        wts = sb(es, "wts", [128, NOWN, 2])
        B_dest, B_wts = Buf(), Buf()
        with ExitStack() as e2:
            G1 = load_mod(e2, "G1", 2)
            SH2 = load_mod(e2, "SH2", 3)
            A2 = load_mod(e2, "A2", 4)
            wo = sb(e2, "wo", [128, 8, 1024], BF16)
            wr = sb(e2, "wr", [128, 8, 72], BF16)
            B_wo = Buf()
            w_out_v = w_out.rearrange("(kt p) n -> p kt n", p=128)
            w_rt_v = w_rt.rearrange("(kt p) n -> p kt n", p=128)
            for kt in range(8):
                dma("pool", lambda e, kt=kt: e.dma_start(out=wo[:, kt, :], in_=w_out_v[:, kt, :]), B_wo, w=[B_wo])
                dma("pool", lambda e, kt=kt: e.dma_start(out=wr[:, kt, :], in_=w_rt_v[:, kt, :]), B_wo, w=[B_wo])
            triu = sb(e2, "triu", [128, 128], BF16)
            onesb = sb(e2, "onesb", [128, 128], BF16)
            OH = sb(e2, "OH", [128, NOWN * 2, 64])
            rk = sb(e2, "rk", [128, NOWN * 2])
            h2_d = dscr("h2_d", [NOWN, 128, 1024], BF16)
            D_h2 = Buf()
            B_OH = Buf()
            dma("pool", lambda e: e.dma_start(out=triu[:], in_=c_triu), B_c2, w=[B_c2])
            op("dve", lambda e: e.memset(onesb[:], 1.0), w=[B_c2])
            run = sb(e2, "run", [128, 64])
            B_run = Buf()
            op("dve", lambda e: e.memset(run[:], 0.0), w=[B_run])
            OHS = sb(e2, "OHS", [128, NOWN, 64], BF16)
            def make_a2(sl):
                mT = sb(e2, "mT_%d" % sl, [128, 8, 128], BF16)
                xt = sb(e2, "xt2_%d" % sl, [128, 1024])
                x1 = sb(e2, "x1_%d" % sl, [128, 1024])
                scr = sb(e2, "scr2_%d" % sl, [128, 1024])
                ss = sb(e2, "ss2_%d" % sl, [128, 1])
                h2b = sb(e2, "h2b_%d" % sl, [128, 1024], BF16)
                h2T = sb(e2, "h2T_%d" % sl, [128, 1024], BF16)
                lg = sb(e2, "lg_%d" % sl, [128, 72])
                gm = sb(e2, "gm_%d" % sl, [128, 8])
                ohg = sb(e2, "ohg_%d" % sl, [128, 8])
                eg = sb(e2, "eg_%d" % sl, [128, 8])
                gs = sb(e2, "gs_%d" % sl, [128, 1])
                esel = sb(e2, "esel_%d" % sl, [128, 64])
                ein = sb(e2, "ein_%d" % sl, [128, 8])
                em8 = sb(e2, "em8_%d" % sl, [128, 8])
                ohe = sb(e2, "ohe_%d" % sl, [128, 2, 8])
                oh64 = sb(e2, "oh64_%d" % sl, [128, 2, 64])
                ohs = sb(e2, "ohs_%d" % sl, [128, 64], BF16)
                slot = sb(e2, "slot_%d" % sl, [128, 64])
                dsf = sb(e2, "dsf_%d" % sl, [128, 2])
                wk = sb(e2, "wk_%d" % sl, [128, 2])
                tmp64 = sb(e2, "tmp64_%d" % sl, [128, 64])
                B_mT, B_xt2, B_x1, B_scr2, B_h2b, B_h2T, B_lg = (Buf() for _ in range(7))
                B_rt = Buf()
                TP, B_TP = (tpb[:], B_tpb) if sl == 0 else (st[0][:].bitcast(BF16), B_st[0])
                PJ = (pj[:, 0:512], pj[:, 512:1024]) if sl == 0 else (oa[0][:], oa[1][:])
                B_PJ = (B_pjA, B_pjB) if sl == 0 else (B_oa[0], B_oa[1])
                MS, B_MS = (ms[:], B_ms) if sl == 0 else (st[1][:], B_st[1])

                def body(i):
                    dma("sp", lambda e: e.dma_start(out=mT[:, 0:4, :].rearrange("p c t -> p (c t)"), in_=mixa_d[i]), B_mT, r=[D_mixa], w=[B_mT])
                    dma("sp", lambda e: e.dma_start(out=mT[:, 4:8, :].rearrange("p c t -> p (c t)"), in_=mixc_d[i]), B_mT, r=[D_mixc], w=[B_mT])
                    dma("sp", lambda e: e.dma_start(out=xt[:], in_=x_own[i * 128:(i + 1) * 128, :]), B_xt2, w=[B_xt2])
                    for hf, Bp in ((0, B_PJ[0]), (1, B_PJ[1])):
                        for kt in range(8):
                            op("pe", lambda e, kt=kt, hf=hf: e.matmul(PJ[hf], lhsT=mT[:, kt, :], rhs=wo[:, kt, hf * 512:(hf + 1) * 512],
                                                                      start=(kt == 0), stop=(kt == 7)), r=[B_mT, B_wo], w=[Bp])
                        op("dve", lambda e, hf=hf: e.tensor_tensor(out=x1[:, hf * 512:(hf + 1) * 512], in0=PJ[hf],
                                                                   in1=G1[:, hf * 512:(hf + 1) * 512], op=ALU.mult), r=[Bp, B_mods], w=[B_x1])
                    op("dve", lambda e: e.tensor_tensor(out=x1[:], in0=x1[:], in1=xt[:], op=ALU.add), r=[B_x1, B_xt2], w=[B_x1])
                    dma("sp", lambda e: e.dma_start(out=x1_d[i * 128:(i + 1) * 128, :], in_=x1[:]), B_x1, r=[B_x1], w=[D_x1])
                    rms_mod(x1[:], h2b[:], A2, SH2, 128, [B_x1], [B_h2b], scr, ss, B_scr2)
                    for kt in range(8):
                        op("pe", lambda e, kt=kt: e.transpose(out=TP[:, kt * 128:(kt + 1) * 128], in_=h2b[:, kt * 128:(kt + 1) * 128], identity=identb[:]),
                           r=[B_h2b, B_cst], w=[B_TP])
                    op("act", lambda e: e.activation(out=h2T[:], in_=TP, func=AF.Copy), r=[B_TP], w=[B_h2T])
                    for kt in range(8):
                        op("pe", lambda e, kt=kt: e.matmul(MS[:, 0:72], lhsT=h2T[:, kt * 128:(kt + 1) * 128], rhs=wr[:, kt, :], start=(kt == 0), stop=(kt == 7)),
                           r=[B_h2T, B_wo], w=[B_MS])
                    R = [B_rt]
                    op("dve", lambda e: e.tensor_tensor(out=lg[:], in0=MS[:, 0:72], in1=brt[:], op=ALU.add), r=[B_MS, B_c2], w=R)
                    op("dve", lambda e: e.max(out=gm[:], in_=lg[:, 0:8]), r=R, w=R)
                    op("dve", lambda e: e.tensor_scalar(out=ohg[:], in0=lg[:, 0:8], scalar1=gm[:, 0:1], scalar2=None, op0=ALU.is_ge), r=R, w=R)
                    op("dve", lambda e: e.tensor_scalar(out=eg[:], in0=lg[:, 0:8], scalar1=gm[:, 0:1], scalar2=None, op0=ALU.subtract), r=R, w=R)
                    op("act", lambda e: e.activation(out=eg[:], in_=eg[:], func=AF.Exp, accum_out=gs[:]), r=R, w=R)
                    op("dve", lambda e: e.reciprocal(out=gs[:], in_=gs[:]), r=R, w=R)
                    op("dve", lambda e: e.tensor_tensor(out=esel[:].rearrange("p (g x) -> p g x", x=8), in0=lg[:, 8:72].rearrange("p (g x) -> p g x", x=8),
                                                        in1=ohg[:].unsqueeze(2).to_broadcast([128, 8, 8]), op=ALU.mult), r=R, w=R)
                    op("dve", lambda e: e.tensor_reduce(out=ein[:], in_=esel[:].rearrange("p (g x) -> p x g", x=8), axis=AX.X, op=ALU.add), r=R, w=R)
                    op("dve", lambda e: e.max(out=em8[:], in_=ein[:]), r=R, w=R)
                    for k in range(2):
                        op("dve", lambda e, k=k: e.tensor_scalar(out=ohe[:, k, :], in0=ein[:], scalar1=em8[:, k:k + 1], scalar2=None, op0=ALU.is_equal), r=R, w=R)
                    op("dve", lambda e: e.tensor_tensor(out=wk[:, 0:1], in0=em8[:, 1:2], in1=em8[:, 0:1], op=ALU.subtract), r=R, w=R)
                    op("act", lambda e: e.activation(out=wk[:, 0:1], in_=wk[:, 0:1], func=AF.Exp), r=R, w=R)
                    op("dve", lambda e: e.tensor_scalar_add(out=wk[:, 0:1], in0=wk[:, 0:1], scalar1=1.0), r=R, w=R)
                    op("dve", lambda e: e.reciprocal(out=wk[:, 0:1], in_=wk[:, 0:1]), r=R, w=R)
                    op("dve", lambda e: e.tensor_scalar(out=wk[:, 1:2], in0=wk[:, 0:1], scalar1=-1.0, scalar2=1.0, op0=ALU.mult, op1=ALU.add), r=R, w=R)
                    op("dve", lambda e: e.tensor_scalar(out=wts[:, i, :], in0=wk[:], scalar1=gs[:, 0:1], scalar2=None, op0=ALU.mult), r=R, w=R + [B_wts])
                    for k in range(2):
                        op("dve", lambda e, k=k: e.tensor_tensor(out=oh64[:, k, :].rearrange("p (g x) -> p g x", x=8),
                                                                 in0=ohg[:].unsqueeze(2).to_broadcast([128, 8, 8]),
                                                                 in1=ohe[:, k, :].unsqueeze(1).to_broadcast([128, 8, 8]), op=ALU.mult), r=R, w=R)
                    op("dve", lambda e: e.tensor_tensor(out=OHS[:, i, :], in0=oh64[:, 0, :], in1=oh64[:, 1, :], op=ALU.add), r=R, w=R + [B_OH])
                    for k in range(2):
                        op("pool", lambda e, k=k: e.tensor_copy(out=OH[:, 2 * i + k, :], in_=oh64[:, k, :]), r=R, w=[B_OH])
                    dma("sp", lambda e: e.dma_start(out=h2_d[i], in_=h2b[:]), B_h2b, r=[B_h2b], w=[D_h2])
                return body

            weave_slots(make_a2, 2, NOWN)
            slot = sb(e2, "slot_p", [128, 64])
            tmp64 = sb(e2, "tmp64_p", [128, 64])
            R = [Buf()]
            for i in range(NOWN):
                op("pe", lambda e: e.matmul(ms[:, 128:192], lhsT=triu[:], rhs=OHS[:, i, :], start=True, stop=True), r=[B_OH, B_c2], w=[B_ms])
                op("dve", lambda e: e.tensor_tensor(out=slot[:], in0=ms[:, 128:192], in1=run[:], op=ALU.add), r=[B_ms, B_run], w=R)
                op("pe", lambda e: e.matmul(ms[:, 192:256], lhsT=onesb[:], rhs=OHS[:, i, :], start=True, stop=True), r=[B_OH, B_c2], w=[B_ms])
                op("dve", lambda e: e.tensor_tensor(out=run[:], in0=run[:], in1=ms[:, 192:256], op=ALU.add), r=[B_ms, B_run], w=[B_run])
                for k in range(2):
                    op("dve", lambda e, k=k: e.tensor_tensor(out=tmp64[:], in0=slot[:], in1=OH[:, 2 * i + k, :], op=ALU.mult), r=R + [B_OH], w=R)
                    op("dve", lambda e, k=k: e.tensor_reduce(out=rk[:, 2 * i + k:2 * i + k + 1], in_=tmp64[:], axis=AX.X, op=ALU.add), r=R, w=R + [B_OH])
            cnt = run
            pe_a = sb(e2, "pe_a", [128, 64])
            pe_b = sb(e2, "pe_b", [128, 64])
            padd = sb(e2, "padd", [128, 64])
            pst = sb(e2, "pst", [128, 64])
            R2 = [Buf()]
            blkc = sb(e2, "blkc", [128, NBLK])
            pidx = sb(e2, "pidx", [128, 1])
            dma("sp", lambda e: e.dma_start(out=blkc[:], in_=c_blk), B_c2, w=[B_c2])
            dma("sp", lambda e: e.dma_start(out=pidx[:], in_=c_pidx), B_c2, w=[B_c2])
            cmp3 = sb(e2, "cmp3", [128, NBLK, 64])
            cmpk = cmp3[:].rearrange("p b e -> p (b e)")[:, 0:64 * NOWN].rearrange("p (e k) -> p e k", k=NOWN)
            op("dve", lambda e: e.tensor_tensor(out=cmpk, in0=cnt[:].unsqueeze(2).to_broadcast([128, 64, NOWN]),
                                                in1=blkc[:, 0:NOWN].unsqueeze(1).to_broadcast([128, 64, NOWN]), op=ALU.is_gt), r=[B_run, B_c2], w=R2)
            op("dve", lambda e: e.tensor_reduce(out=padd[:], in_=cmpk, axis=AX.X, op=ALU.add), r=R2, w=R2)
            op("dve", lambda e: e.tensor_scalar_mul(out=padd[:], in0=padd[:], scalar1=128.0), r=R2, w=R2)
            op("dve", lambda e: e.tensor_copy(out=pe_a[:], in_=padd[:]), r=R2, w=R2)
            cur, oth = pe_a, pe_b
            for sft in (1, 2, 4, 8, 16, 32):
                op("dve", lambda e, cur=cur, oth=oth, sft=sft: e.tensor_copy(out=oth[:, 0:sft], in_=cur[:, 0:sft]), r=R2, w=R2)
                op("dve", lambda e, cur=cur, oth=oth, sft=sft: e.tensor_tensor(out=oth[:, sft:64], in0=cur[:, sft:64], in1=cur[:, 0:64 - sft], op=ALU.add),
                   r=R2, w=R2)
                cur, oth = oth, cur
            pend_ = cur
            op("dve", lambda e: e.tensor_tensor(out=pst[:], in0=pend_[:], in1=padd[:], op=ALU.subtract), r=R2, w=R2)
            ber = sb(e2, "ber", [128, NBLK])
            bpv = sb(e2, "bpv", [128, NBLK])
            idxf = sb(e2, "idxf", [128, NBLK])
            op("dve", lambda e: e.tensor_tensor(out=cmp3[:], in0=pend_[:].unsqueeze(1).to_broadcast([128, NBLK, 64]),
                                                in1=blkc[:].unsqueeze(2).to_broadcast([128, NBLK, 64]), op=ALU.is_le), r=R2 + [B_c2], w=R2)
            op("dve", lambda e: e.tensor_reduce(out=ber[:], in_=cmp3[:], axis=AX.X, op=ALU.add), r=R2, w=R2)
            op("dve", lambda e: e.tensor_scalar_min(out=ber[:], in0=ber[:], scalar1=63.0), r=R2, w=R2)
            op("dve", lambda e: e.memset(bpv[:, 0:2], -1.0), r=R2, w=R2)
            op("dve", lambda e: e.tensor_copy(out=bpv[:, 2:NBLK], in_=ber[:, 0:NBLK - 2]), r=R2, w=R2)
            op("dve", lambda e: e.tensor_tensor(out=bpv[:], in0=bpv[:], in1=ber[:], op=ALU.is_equal), r=R2, w=R2)
            op("dve", lambda e: e.tensor_scalar(out=idxf[:], in0=ber[:], scalar1=128.0, scalar2=pidx[:, 0:1], op0=ALU.mult, op1=ALU.add), r=R2 + [B_c2], w=R2)
            op("dve", lambda e: e.scalar_tensor_tensor(out=idxf[:], in0=bpv[:], scalar=1.0e6, in1=idxf[:], op0=ALU.mult, op1=ALU.add), r=R2, w=R2)
            op("dve", lambda e: e.tensor_copy(out=widx[:], in_=idxf[:]), r=R2, w=[B_widx])
            ohp = cmp3[:, 0:NOWN * 2, :] if NBLK >= NOWN * 2 else None
            op("dve", lambda e: e.tensor_tensor(out=ohp, in0=OH[:], in1=pst[:].unsqueeze(1).to_broadcast([128, NOWN * 2, 64]), op=ALU.mult),
               r=R2 + [B_OH], w=R2)
            op("dve", lambda e: e.tensor_reduce(out=idxf[:, 0:NOWN * 2], in_=ohp, axis=AX.X, op=ALU.add), r=R2, w=R2)
            op("dve", lambda e: e.tensor_tensor(out=idxf[:, 0:NOWN * 2], in0=idxf[:, 0:NOWN * 2], in1=rk[:], op=ALU.add), r=R2 + [B_OH], w=R2)
            op("dve", lambda e: e.tensor_copy(out=dest[:].rearrange("p i k -> p (i k)"), in_=idxf[:, 0:NOWN * 2]), r=R2, w=[B_dest])
            h2sc = [sb(e2, "h2sc%d" % q, [128, 1024], BF16) for q in range(2)]
            B_h2sc = [Buf(), Buf()]
            for i in range(NOWN):
                q = i % 2
                dma("sp", lambda e: e.dma_start(out=h2sc[q][:], in_=h2_d[i]), B_h2sc[q], r=[D_h2], w=[B_h2sc[q]])
                for k in range(2):
                    dma("pool", lambda e, k=k: e.indirect_dma_start(out=xdisp_d[:, :], out_offset=bass.IndirectOffsetOnAxis(ap=dest[:, i, k:k + 1], axis=0),
                                                                    in_=h2sc[q][:], in_offset=None), B_h2sc[q], r=[B_h2sc[q], B_dest], w=[D_xd])
            sc.barrier()

        with ExitStack() as e3:
            Wf = [[sb(e3, "Wf%d_%d" % (m, q), [128, 4096]) for m in range(3)] for q in range(2)]
            B_Wf = [[Buf(), Buf(), Buf()] for q in range(2)]
            Wb = [[sb(e3, "Wb%d_%d" % (m, k), [128, 4096], BF16) for m in range(3)] for k in range(2)]
            B_Wb = [[Buf(), Buf(), Buf()] for _ in range(2)]
            wsrc = (w1, w3, w2)
            bnd_reg = nc.gpsimd.alloc_register('bnd')
            nc.gpsimd.reg_mov(bnd_reg, 64 * 128 - 1)
            cast_eng = ("act", "act", "dve")
            TPm = [tpb[:], ms[:].bitcast(BF16)]
            B_TPm = [B_tpb, B_ms]
            H1p = [st[0][:], oa[0][:]]
            B_H1p = [B_st[0], B_oa[0]]
            H3p = [st[1][:], oa[1][:]]
            B_H3p = [B_st[1], B_oa[1]]

            def make_moe(sl):
                xe = sb(e3, "xe%d" % sl, [128, 1024], BF16)
                xeT = sb(e3, "xeT%d" % sl, [128, 8, 128], BF16)
                hs = sb(e3, "hs%d" % sl, [128, 512])
                h1e = sb(e3, "h1e%d" % sl, [128, 512])
                actb = sb(e3, "actb%d" % sl, [128, 512], BF16)
                aT = sb(e3, "aT%d" % sl, [128, 4, 128], BF16)
                yo = sb(e3, "yo%d" % sl, [128, 1024], BF16)
                B_xe, B_xeT, B_hs, B_h1e, B_actb, B_aT, B_yo = (Buf() for _ in range(7))
                tp, B_tp = TPm[sl], B_TPm[sl]
                h1p, B_h1p, h3p, B_h3p = H1p[sl], B_H1p[sl], H3p[sl], B_H3p[sl]

                def body(b):
                    p = b % 2
                    while b >= 2 and not body_done[b - 2]:
                        weave_yield()
                    dma("sp", lambda e: e.dma_start(out=xe[:], in_=xdisp_d[b * 128:(b + 1) * 128, :]), B_xe, r=[D_xd], w=[B_xe])
                    for kt in range(8):
                        op("pe", lambda e, kt=kt: e.transpose(out=tp[:, kt * 128:(kt + 1) * 128], in_=xe[:, kt * 128:(kt + 1) * 128], identity=identb[:]),
                           r=[B_xe, B_cst], w=[B_tp])
                    op("dve", lambda e: e.tensor_copy(out=xeT[:].rearrange("p k t -> p (k t)"), in_=tp), r=[B_tp], w=[B_xeT])
                    while not cast_issued[b]:
                        weave_yield()
                    W1b = Wb[p][0][:].rearrange("p (k f) -> p k f", k=8)
                    W3b = Wb[p][1][:].rearrange("p (k f) -> p k f", k=8)
                    W2b = Wb[p][2][:].rearrange("p (k f) -> p k f", k=4)
                    for (Wm, mi, dst, Bd) in ((W1b, 0, h1p, B_h1p), (W3b, 1, h3p, B_h3p)):
                        for kt in range(8):
                            op("pe", lambda e, kt=kt, Wm=Wm, dst=dst: e.matmul(dst, lhsT=xeT[:, kt, :], rhs=Wm[:, kt, :], start=(kt == 0), stop=(kt == 7)),
                               r=[B_Wb[p][mi], B_xeT], w=[Bd])
                    op("act", lambda e: e.activation(out=h1e[:], in_=h1p, func=AF.Exp, scale=-1.0), r=[B_h1p], w=[B_h1e])
                    op("act", lambda e: e.activation(out=hs[:], in_=h1p, func=AF.Copy), r=[B_h1p], w=[B_hs])
                    op("dve", lambda e: e.tensor_scalar_add(out=h1e[:], in0=h1e[:], scalar1=1.0), r=[B_h1e], w=[B_h1e])
                    op("dve", lambda e: e.reciprocal(out=h1e[:], in_=h1e[:]), r=[B_h1e], w=[B_h1e])
                    op("dve", lambda e: e.tensor_tensor(out=hs[:], in0=hs[:], in1=h3p, op=ALU.mult), r=[B_hs, B_h3p], w=[B_hs])
                    op("dve", lambda e: e.tensor_tensor(out=actb[:], in0=hs[:], in1=h1e[:], op=ALU.mult), r=[B_hs, B_h1e], w=[B_actb])
                    for ft in range(4):
                        op("pe", lambda e, ft=ft: e.transpose(out=tp[:, ft * 128:(ft + 1) * 128], in_=actb[:, ft * 128:(ft + 1) * 128], identity=identb[:]),
                           r=[B_actb, B_cst], w=[B_tp])
                    op("dve", lambda e: e.tensor_copy(out=aT[:].rearrange("p k t -> p (k t)"), in_=tp[:, 0:512]), r=[B_tp], w=[B_aT])
                    while pj_lock[0]:
                        weave_yield()
                    pj_lock[0] = True
                    for hf, Bp in ((0, B_pjA), (1, B_pjB)):
                        for ft in range(4):
                            op("pe", lambda e, ft=ft, hf=hf: e.matmul(pj[:, hf * 512:(hf + 1) * 512], lhsT=aT[:, ft, :],
                                                                      rhs=W2b[:, ft, hf * 512:(hf + 1) * 512], start=(ft == 0), stop=(ft == 3)),
                               r=[B_aT, B_Wb[p][2]], w=[Bp])
                        op("act", lambda e, hf=hf: e.activation(out=yo[:, hf * 512:(hf + 1) * 512], in_=pj[:, hf * 512:(hf + 1) * 512], func=AF.Copy),
                           r=[Bp], w=[B_yo])
                    pj_lock[0] = False
                    comp_done[b] = True
                    dma("sp", lambda e: e.dma_start(out=ydisp_d[b * 128:(b + 1) * 128, :], in_=yo[:]), B_yo, r=[B_yo], w=[D_yd])
                    body_done[b] = True
                return body

            body_done = [False] * NBLK
            cast_issued = [False] * NBLK
            comp_done = [False] * NBLK
            pj_lock = [False]

            def weights_task():
                for b in range(NBLK):
                    p = b % 2
                    for m in range(3):
                        dma("pool", lambda e, m=m: e.indirect_dma_start(out=Wf[p][m][:], out_offset=None, in_=wsrc[m][:, :],
                                                                        in_offset=bass.IndirectOffsetOnAxis(ap=widx[:, b:b + 1], axis=0),
                                                                        bounds_check=bnd_reg, oob_is_err=False),
                            B_Wf[p][m], r=[B_widx], w=[B_Wf[p][m]])
                    while b >= 2 and not comp_done[b - 2]:
                        weave_yield()
                    for m in range(3):
                        if cast_eng[m] == "act":
                            op("act", lambda e, m=m: e.activation(out=Wb[p][m][:], in_=Wf[p][m][:], func=AF.Copy), r=[B_Wf[p][m]], w=[B_Wb[p][m]])
                        else:
                            op(cast_eng[m], lambda e, m=m: e.tensor_copy(out=Wb[p][m][:], in_=Wf[p][m][:]), r=[B_Wf[p][m]], w=[B_Wb[p][m]])
                    cast_issued[b] = True

            moe_bodies = [make_moe(0), make_moe(1)]
            assert nc.sbuf_bytes_remaining >= 20000, nc.sbuf_bytes_remaining
            weave([weights_task] + [(lambda b=b: moe_bodies[b % 2](b)) for b in range(NBLK)], 3)
            sc.barrier()

        with ExitStack() as e4:
            G2 = load_mod(e4, "G2", 5)
            def make_c(sl):
                y0 = sb(e4, "y0_%d" % sl, [128, 1024], BF16)
                y1 = sb(e4, "y1_%d" % sl, [128, 1024], BF16)
                xr = sb(e4, "xr_%d" % sl, [128, 1024])
                ob = sb(e4, "ob_%d" % sl, [128, 1024])
                B_y0, B_y1, B_xr, B_ob = Buf(), Buf(), Buf(), Buf()

                def body(i):
                    dma("pool", lambda e: e.indirect_dma_start(out=y0[:], out_offset=None, in_=ydisp_d[:, :],
                                                               in_offset=bass.IndirectOffsetOnAxis(ap=dest[:, i, 0:1], axis=0)), B_y0, r=[D_yd, B_dest], w=[B_y0])
                    dma("pool", lambda e: e.indirect_dma_start(out=y1[:], out_offset=None, in_=ydisp_d[:, :],
                                                               in_offset=bass.IndirectOffsetOnAxis(ap=dest[:, i, 1:2], axis=0)), B_y1, r=[D_yd, B_dest], w=[B_y1])
                    dma("sp", lambda e: e.dma_start(out=xr[:], in_=x1_d[i * 128:(i + 1) * 128, :]), B_xr, r=[D_x1], w=[B_xr])
                    op("dve", lambda e: e.tensor_scalar(out=ob[:], in0=y0[:], scalar1=wts[:, i, 0:1], scalar2=None, op0=ALU.mult), r=[B_y0, B_wts], w=[B_ob])
                    op("dve", lambda e: e.scalar_tensor_tensor(out=ob[:], in0=y1[:], scalar=wts[:, i, 1:2], in1=ob[:], op0=ALU.mult, op1=ALU.add),
                       r=[B_y1, B_wts, B_ob], w=[B_ob])
                    op("dve", lambda e: e.tensor_tensor(out=ob[:], in0=ob[:], in1=G2, op=ALU.mult), r=[B_ob, B_mods], w=[B_ob])
                    op("dve", lambda e: e.tensor_tensor(out=ob[:], in0=ob[:], in1=xr[:], op=ALU.add), r=[B_ob, B_xr], w=[B_ob])
                    dma("sp", lambda e: e.dma_start(out=out[i * 128:(i + 1) * 128, :], in_=ob[:]), B_ob, r=[B_ob], w=[])
                return body

            weave_slots(make_c, 2, NOWN)
            sc.barrier()
    return nc


def host_consts(S, C, r):
    c = {}
    c["c_ident"] = np.eye(128, dtype=np.float32)
    c["c_anti"] = np.eye(128, dtype=np.float32)[::-1].copy()
    c["c_anti48"] = np.eye(48, dtype=np.float32)[::-1].copy()
    bd = np.zeros((128, 128), np.float32)
    bd[:64, :64] = 1
    bd[64:, 64:] = 1
    c["c_bd"] = bd
    c["c_triu"] = np.triu(np.ones((128, 128), np.float32), 1)
    er = np.zeros((128, 4096), np.float32)
    k = np.arange(4096)
    er[(k // 64) % 64, k] = 1.0
    er[64 + (k // 64) % 64, k] = 1.0
    c["c_erow"] = er
    v = np.arange(768)
    c["c_ohs"] = oh_table(v - 511 + 128 * r)
    v = np.arange(1152)
    c["c_ohw"] = oh_table(v - 511 + 128 * r, 0, 512)
    w = np.arange(880)
    c["c_ohq"] = oh_table(w + 128 * r - 527)
    w = np.arange(4224)
    c["c_ohk"] = oh_table(w + 128 * r - 2063)
    hi = np.full((128, 9), 3.0e38, np.float32)
    lo = np.full((128, 9), -1.0, np.float32)
    ql = np.arange(128)
    for cc in range(9):
        m_rel = cc - 1
        cur = 2 * r + (ql >= 64)
        forced = (m_rel == cur) | (m_rel == cur - 1)
        invalid = m_rel > cur
        lo[forced, cc] = 1e4
        hi[invalid, cc] = -1.0
    c["c_hi"] = hi
    c["c_lo"] = lo
    c["c_pad"] = np.full((128, 1), 0.0 if r == 0 else 1.0, np.float32)
    NBLK = (S // 512 * 256 + 64 * 127) // 128
    c["c_blk"] = np.tile((np.arange(NBLK, dtype=np.float32) * 128)[None, :], (128, 1))
    c["c_pidx"] = np.arange(128, dtype=np.float32).reshape(128, 1)
    return c


def kernel(x, c, w_ada, b_ada, norm1_w, w_in, q_norm_w, k_norm_w, cmp_pos_k, cmp_pos_v,
           cmp_k_w1, cmp_k_w2, cmp_v_w1, cmp_v_w2, conv_w, out_norm_w, w_out, rel_bias,
           norm2_w, w_group, b_group, w_expert, b_expert, w1, w3, w2, _C=None):
    f = lambda a: np.ascontiguousarray(np.asarray(a, dtype=np.float32))
    x = f(x)
    B, S, D = x.shape
    NB = S // 128
    NOWN = NB // 4
    C = _C if _C is not None else (256 if S >= 8192 else 128)
    nc = build(S, C)

    def pos_lay(p):
        p = f(p)[0]
        return np.ascontiguousarray(p.reshape(16, 2, 64).transpose(1, 2, 0).reshape(128, 16, 1))

    shared = {
        "w_ada": f(w_ada)[0], "b_ada": f(b_ada), "norm1_w": f(norm1_w), "w_in": f(w_in)[0],
        "q_norm_w": f(q_norm_w), "k_norm_w": f(k_norm_w).reshape(1, 192),
        "cmp_pos_k": pos_lay(cmp_pos_k), "cmp_pos_v": pos_lay(cmp_pos_v),
        "cmp_k_w1": f(cmp_k_w1)[0], "cmp_k_w2": f(cmp_k_w2)[0], "cmp_v_w1": f(cmp_v_w1)[0], "cmp_v_w2": f(cmp_v_w2)[0],
        "conv_wl": np.ascontiguousarray(f(conv_w)[0].reshape(3, 4, 128).transpose(2, 1, 0).reshape(128, 12)),
        "onw_c": np.ascontiguousarray(f(out_norm_w)[0, 512:].reshape(4, 128).T),
        "onw_a": np.ascontiguousarray(f(out_norm_w)[:, :512]),
        "w_out": f(w_out)[0], "rel_bias": f(rel_bias), "norm2_w": f(norm2_w),
        "w_rt": np.ascontiguousarray(np.concatenate([f(w_group)[0], f(w_expert)[0]], axis=1)),
        "b_rt": np.ascontiguousarray(np.concatenate([f(b_group), f(b_expert)], axis=1)),
        "w1": np.ascontiguousarray(f(w1)[0].reshape(64, 8, 128, 512).transpose(0, 2, 1, 3).reshape(64 * 128, 4096)),
        "w3": np.ascontiguousarray(f(w3)[0].reshape(64, 8, 128, 512).transpose(0, 2, 1, 3).reshape(64 * 128, 4096)),
        "w2": np.ascontiguousarray(f(w2)[0].reshape(64, 4, 128, 1024).transpose(0, 2, 1, 3).reshape(64 * 128, 4096)),
    }
    cf = f(c)
    in_maps = []
    for core in range(8):
        b, r = core // 4, core % 4
        xb = x[b]
        blocks = xb.reshape(NB, 128, D)
        own = np.ascontiguousarray(blocks[r::4].reshape(NOWN * 128, D))
        prev = np.zeros((NOWN, 2, D), np.float32)
        for i in range(NOWN):
            j = 4 * i + r
            if j > 0:
                prev[i] = xb[128 * j - 2:128 * j]
        m = dict(shared)
        m.update(host_consts(S, C, r))
        m["x_all"] = xb
        m["x_own"] = own
        m["x_prev"] = prev.reshape(NOWN * 2, D)
        m["c_lay"] = np.ascontiguousarray(cf[b].reshape(8, 128).T)
        in_maps.append(m)
    res = run_bass_kernel_spmd(nc, in_maps, core_ids=list(range(8)))
    outp = np.zeros((B, NB, 128, D), np.float32)
    for core in range(8):
        b, r = core // 4, core % 4
        outp[b, r::4] = np.asarray(res.results[core]["out"]).reshape(NOWN, 128, D)
    return outp.reshape(B, S, D)
```
